# Optimizing a Trainium2 kernel written in Bass

```python
import math
import jax, jax.numpy as jnp
from jax import lax
import numpy as np

D_MODEL = 1024
BATCH = 4
SEQ = 4096
DEPTH = 2

N_A_LAYERS = (DEPTH + 1) // 2
N_B_LAYERS = DEPTH // 2

CHUNK = 64
Q_BLOCK = 128
MEM_LEN = 256
MIX_W = D_MODEL
MEM_W = D_MODEL // 4
MEM_HEADS = 4
MEM_HD = MEM_W // MEM_HEADS
TOK_W = MIX_W - MEM_W
DA_DH = 64
DA_HEADS = TOK_W // (2 * DA_DH)
DA_IN = 3 * TOK_W + MEM_W
SSM_HD = 64
SSM_HEADS = TOK_W // SSM_HD
SSM_GROUPS = 2
SSM_STATE = 128
SSM_CONV_K = 4
SSM_CONV_DIM = TOK_W + 2 * SSM_GROUPS * SSM_STATE
SSM_IN = TOK_W + SSM_CONV_DIM + SSM_HEADS + MEM_W
FFN_DIM = 128 * ((8 * D_MODEL // 3 + 127) // 128)
N_EXPERTS = 8
TOP_K = 2
EXPERT_DIM = 7 * D_MODEL // 2
EPS = 1e-6

kernel_name = "hybrid_diffattn_ssd_moe_streaming"


def rms_norm(x, g):
    xf = x.astype(jnp.float32)
    y = xf * lax.rsqrt(jnp.mean(xf * xf, axis=-1, keepdims=True) + EPS)
    return (y * g.astype(jnp.float32)).astype(x.dtype)


def swiglu(h, w_gate, w_up, w_down):
    return (jax.nn.silu(h @ w_gate) * (h @ w_up)) @ w_down


def diff_attention(q, k, v, qn_g, kn_g, lam, sub_g, lambda_init):
    b, s = q.shape[:2]
    q = rms_norm(q, qn_g)
    k = rms_norm(k, kn_g)
    scale = DA_DH ** -0.5
    nqb = s // Q_BLOCK
    q_blocks = jnp.swapaxes(q.reshape(b, nqb, Q_BLOCK, DA_HEADS, 2, DA_DH), 0, 1)
    pos_chunk = jnp.arange(s) // CHUNK
    q_chunk = pos_chunk.reshape(nqb, Q_BLOCK)

    def one_block(args):
        q_blk, qc = args
        sc = jnp.einsum('bqhid,bkhid->bhiqk', q_blk, k).astype(jnp.float32) * scale
        mask = pos_chunk[None, :] <= qc[:, None]
        sc = jnp.where(mask, sc, -jnp.inf)
        p = jax.nn.softmax(sc, axis=-1)
        a = p[:, :, 0] - lam * p[:, :, 1]
        return jnp.einsum('bhqk,bkhe->bqhe', a.astype(v.dtype), v)

    o = lax.map(one_block, (q_blocks, q_chunk))
    o = jnp.swapaxes(o, 0, 1).reshape(b, s, DA_HEADS, 2 * DA_DH)
    o = rms_norm(o, sub_g) * (1.0 - lambda_init)
    return o.reshape(b, s, TOK_W)


def memory_attention(mq, mem_n, w_kv, qn_g, kn_g):
    b, s = mq.shape[:2]
    m = mem_n.shape[1]
    q = rms_norm(mq.reshape(b, s, MEM_HEADS, MEM_HD), qn_g)
    kv = mem_n @ w_kv
    k = rms_norm(kv[..., :MEM_W].reshape(b, m, MEM_HEADS, MEM_HD), kn_g)
    v = kv[..., MEM_W:].reshape(b, m, MEM_HEADS, MEM_HD)
    sc = jnp.einsum('bqhd,bkhd->bhqk', q, k).astype(jnp.float32) * (MEM_HD ** -0.5)
    p = jax.nn.softmax(sc, axis=-1)
    o = jnp.einsum('bhqk,bkhd->bqhd', p.astype(v.dtype), v)
    return o.reshape(b, s, MEM_W)


def causal_depthwise_conv(x, w):
    kw, c = w.shape
    return lax.conv_general_dilated(x, w[:, None, :].astype(x.dtype), window_strides=(1,),
                                    padding=[(kw - 1, 0)], dimension_numbers=('NWC', 'WIO', 'NWC'),
                                    feature_group_count=c)


def ssd(x, dt, a, bm, cm):
    b, s, h, p = x.shape
    g, n = bm.shape[2], bm.shape[3]
    hg = h // g
    nc = s // CHUNK
    f32 = jnp.float32
    x = x.astype(f32).reshape(b, nc, CHUNK, g, hg, p)
    dt = dt.reshape(b, nc, CHUNK, g, hg)
    bm = bm.astype(f32).reshape(b, nc, CHUNK, g, n)
    cm = cm.astype(f32).reshape(b, nc, CHUNK, g, n)
    cum = jnp.cumsum(dt * a.reshape(g, hg), axis=2)
    tri = jnp.tril(jnp.ones((CHUNK, CHUNK), dtype=bool))
    seg = cum[:, :, :, None] - cum[:, :, None, :]
    decay = jnp.exp(jnp.where(tri[:, :, None, None], seg, -jnp.inf))
    cb = jnp.einsum('bctgn,bcsgn->bctsg', cm, bm)
    w = cb[..., None] * decay * dt[:, :, None]
    y_diag = jnp.einsum('bctsgh,bcsghp->bctghp', w, x)
    to_end = jnp.exp(cum[:, :, -1:] - cum) * dt
    states = jnp.einsum('bclgn,bclgh,bclghp->bcghpn', bm, to_end, x)
    chunk_decay = jnp.exp(cum[:, :, -1])

    def step(carry, inp):
        st, dec = inp
        return carry * dec[..., None, None] + st, carry

    h0 = jnp.zeros((b, g, hg, p, n), f32)
    _, h_in = lax.scan(step, h0, (jnp.moveaxis(states, 1, 0), jnp.moveaxis(chunk_decay, 1, 0)))
    h_in = jnp.moveaxis(h_in, 0, 1)
    y_off = jnp.einsum('bclgn,bcghpn,bclgh->bclghp', cm, h_in, jnp.exp(cum))
    return (y_diag + y_off).reshape(b, s, h, p)


def mamba2_mixer(z, xbc, dt_raw, conv_w, conv_b, dt_bias, a_log, d_skip, norm_g):
    b, s = z.shape[:2]
    xbc = jax.nn.silu(causal_depthwise_conv(xbc, conv_w) + conv_b)
    xs = xbc[..., :TOK_W].reshape(b, s, SSM_HEADS, SSM_HD)
    bm = xbc[..., TOK_W:TOK_W + SSM_GROUPS * SSM_STATE].reshape(b, s, SSM_GROUPS, SSM_STATE)
    cm = xbc[..., TOK_W + SSM_GROUPS * SSM_STATE:].reshape(b, s, SSM_GROUPS, SSM_STATE)
    dt = jax.nn.softplus(dt_raw.astype(jnp.float32) + dt_bias.astype(jnp.float32))
    a = -jnp.exp(a_log.astype(jnp.float32))
    y = ssd(xs, dt, a, bm, cm)
    y = y + d_skip.astype(jnp.float32)[:, None] * xs.astype(jnp.float32)
    y = y.reshape(b, s, TOK_W) * jax.nn.silu(z.astype(jnp.float32))
    y = rms_norm(y.reshape(b, s, SSM_GROUPS, TOK_W // SSM_GROUPS),
                 norm_g.reshape(SSM_GROUPS, TOK_W // SSM_GROUPS)).reshape(b, s, TOK_W)
    return y.astype(z.dtype)


def moe_swiglu(h, w_router, w_gate, w_up, w_down):
    logits = (h @ w_router).astype(jnp.float32)
    top_val, top_idx = lax.top_k(logits, TOP_K)
    top_w = jax.nn.softmax(top_val, axis=-1)
    gates = jnp.sum(jax.nn.one_hot(top_idx, N_EXPERTS, dtype=jnp.float32) * top_w[..., None], axis=-2)
    out = jnp.zeros(h.shape, jnp.float32)
    for e in range(N_EXPERTS):
        out = out + gates[..., e:e + 1] * swiglu(h, w_gate[e], w_up[e], w_down[e])
    return out.astype(h.dtype)


def setup_inputs(seed: int = 0) -> dict:
    key = jax.random.key(seed)
    ks = iter(jax.random.split(key, 48))
    f32 = jnp.float32

    def nrm(shape, scale):
        return jax.random.normal(next(ks), shape, f32) * scale

    def gain(shape):
        return 1.0 + nrm(shape, 0.02)

    NA, NB = N_A_LAYERS, N_B_LAYERS
    dt0 = jnp.exp(jax.random.uniform(next(ks), (NB, SSM_HEADS), f32,
                                     minval=math.log(1e-3), maxval=math.log(1e-1)))
    dt_bias = dt0 + jnp.log(-jnp.expm1(-dt0))
    a_log = jnp.log(jax.random.uniform(next(ks), (NB, SSM_HEADS), f32, minval=1.0, maxval=16.0))
    return {
        "x": nrm((BATCH, SEQ, D_MODEL), 1.0),
        "mem": nrm((BATCH, MEM_LEN, D_MODEL), 1.0),
        "ln1_g": gain((DEPTH, D_MODEL)),
        "ln2_g": gain((DEPTH, D_MODEL)),
        "mem_norm_g": gain((D_MODEL,)),
        "w_out": nrm((DEPTH, MIX_W, D_MODEL), MIX_W ** -0.5),
        "mem_w_kv": nrm((DEPTH, D_MODEL, 2 * MEM_W), D_MODEL ** -0.5),
        "mem_qn_g": gain((DEPTH, MEM_HD)),
        "mem_kn_g": gain((DEPTH, MEM_HD)),
        "da_w_in": nrm((NA, D_MODEL, DA_IN), D_MODEL ** -0.5),
        "da_qn_g": gain((NA, DA_DH)),
        "da_kn_g": gain((NA, DA_DH)),
        "da_lq1": nrm((NA, DA_DH), 0.1),
        "da_lk1": nrm((NA, DA_DH), 0.1),
        "da_lq2": nrm((NA, DA_DH), 0.1),
        "da_lk2": nrm((NA, DA_DH), 0.1),
        "da_sub_g": gain((NA, 2 * DA_DH)),
        "ssm_w_in": nrm((NB, D_MODEL, SSM_IN), D_MODEL ** -0.5),
        "ssm_conv_w": nrm((NB, SSM_CONV_K, SSM_CONV_DIM), SSM_CONV_K ** -0.5),
        "ssm_conv_b": nrm((NB, SSM_CONV_DIM), 0.02),
        "ssm_dt_bias": dt_bias,
        "ssm_a_log": a_log,
        "ssm_d": gain((NB, SSM_HEADS)),
        "ssm_norm_g": gain((NB, TOK_W)),
        "ffn_w_gate": nrm((NA, D_MODEL, FFN_DIM), D_MODEL ** -0.5),
        "ffn_w_up": nrm((NA, D_MODEL, FFN_DIM), D_MODEL ** -0.5),
        "ffn_w_down": nrm((NA, FFN_DIM, D_MODEL), FFN_DIM ** -0.5),
        "moe_w_router": nrm((NB, D_MODEL, N_EXPERTS), D_MODEL ** -0.5),
        "moe_w_gate": nrm((NB, N_EXPERTS, D_MODEL, EXPERT_DIM), D_MODEL ** -0.5),
        "moe_w_up": nrm((NB, N_EXPERTS, D_MODEL, EXPERT_DIM), D_MODEL ** -0.5),
        "moe_w_down": nrm((NB, N_EXPERTS, EXPERT_DIM, D_MODEL), EXPERT_DIM ** -0.5),
    }


def reference(x, mem, ln1_g, ln2_g, mem_norm_g, w_out, mem_w_kv, mem_qn_g, mem_kn_g,
              da_w_in, da_qn_g, da_kn_g, da_lq1, da_lk1, da_lq2, da_lk2, da_sub_g,
              ssm_w_in, ssm_conv_w, ssm_conv_b, ssm_dt_bias, ssm_a_log, ssm_d, ssm_norm_g,
              ffn_w_gate, ffn_w_up, ffn_w_down,
              moe_w_router, moe_w_gate, moe_w_up, moe_w_down):
    b, s = x.shape[:2]
    mem_n = rms_norm(mem, mem_norm_g)
    for i in range(DEPTH):
        j = i // 2
        h = rms_norm(x, ln1_g[i])
        if i % 2 == 0:
            u = h @ da_w_in[j]
            q = u[..., :TOK_W].reshape(b, s, DA_HEADS, 2, DA_DH)
            k = u[..., TOK_W:2 * TOK_W].reshape(b, s, DA_HEADS, 2, DA_DH)
            v = u[..., 2 * TOK_W:3 * TOK_W].reshape(b, s, DA_HEADS, 2 * DA_DH)
            mq = u[..., 3 * TOK_W:]
            lambda_init = 0.8 - 0.6 * math.exp(-0.3 * i)
            lam = (jnp.exp(jnp.sum(da_lq1[j].astype(jnp.float32) * da_lk1[j].astype(jnp.float32)))
                   - jnp.exp(jnp.sum(da_lq2[j].astype(jnp.float32) * da_lk2[j].astype(jnp.float32)))
                   + lambda_init)
            tok = diff_attention(q, k, v, da_qn_g[j], da_kn_g[j], lam, da_sub_g[j], lambda_init)
        else:
            u = h @ ssm_w_in[j]
            z = u[..., :TOK_W]
            xbc = u[..., TOK_W:TOK_W + SSM_CONV_DIM]
            dt_raw = u[..., TOK_W + SSM_CONV_DIM:TOK_W + SSM_CONV_DIM + SSM_HEADS]
            mq = u[..., TOK_W + SSM_CONV_DIM + SSM_HEADS:]
            tok = mamba2_mixer(z, xbc, dt_raw, ssm_conv_w[j], ssm_conv_b[j], ssm_dt_bias[j],
                               ssm_a_log[j], ssm_d[j], ssm_norm_g[j])
        mo = memory_attention(mq, mem_n, mem_w_kv[i], mem_qn_g[i], mem_kn_g[i])
        mixed = jnp.concatenate([tok.astype(h.dtype), mo.astype(h.dtype)], axis=-1)
        x = x + (mixed @ w_out[i]).astype(x.dtype)
        h = rms_norm(x, ln2_g[i])
        if i % 2 == 0:
            x = x + swiglu(h, ffn_w_gate[j], ffn_w_up[j], ffn_w_down[j]).astype(x.dtype)
        else:
            x = x + moe_swiglu(h, moe_w_router[j], moe_w_gate[j], moe_w_up[j], moe_w_down[j]).astype(x.dtype)
    return x
```

```python
import math
from contextlib import ExitStack

import numpy as np
import concourse.bass as bass
import concourse.mybir as mybir
from concourse.bass_utils import run_bass_kernel_spmd

F32 = mybir.dt.float32
BF16 = mybir.dt.bfloat16
AF = mybir.ActivationFunctionType
ALU = mybir.AluOpType
AX = mybir.AxisListType

D = 1024
NCH = 8
SEQH = 2048
TT = 512
NTT = SEQH // TT
EPS = 1e-6
FFN_DIM = 2816
EXPERT_DIM = 3584
N_EXPERTS = 8
MEM_LEN = 256
TOK_W = 768
NEG = -30000.0


class _Op:
    __slots__ = ("eng", "fn", "deps", "eidx", "is_dma", "dkey", "dval", "signal", "tick", "region")


class Sched:
    ENGS = ("pe", "act", "dve", "pool", "sp")

    def __init__(self, nc):
        self.nc = nc
        self.eng_ops = {e: [] for e in self.ENGS}
        self.last_w = {}
        self.readers = {}
        self.dma_tot = {}
        self.last_dma = {}
        self.barrier_deps = []
        self.cur_region = None
        self.reg_keys = []
        self.cur_regs = {}

    def add(self, eng, fn, reads=(), writes=(), dma_key=None):
        op = _Op()
        op.eng = eng
        op.fn = fn
        op.is_dma = dma_key is not None
        op.dkey = dma_key
        op.signal = False
        op.tick = 0
        op.dval = 0
        op.eidx = len(self.eng_ops[eng])
        op.region = self.cur_region
        assert not (op.is_dma and op.region is not None and eng not in ("pool", "sp"))
        deps = {}

        def need(P, dval=None):
            if P is None or P is op:
                return
            if P.is_dma:
                deps[id(P)] = (P, self.dma_tot[P.dkey] if dval is None else dval)
                return
            if P.eng == eng:
                if eng == "pe":
                    return
                if (not op.is_dma) and (op.eidx - P.eidx) > 3:
                    cnt = 0
                    lst = self.eng_ops[eng]
                    for qi in range(len(lst) - 1, P.eidx, -1):
                        o = lst[qi]
                        if o.region is None or o.region == op.region:
                            cnt += 1
                            if cnt >= 3:
                                break
                    if cnt >= 3:
                        return
            deps[id(P)] = (P, 0)

        for (P, bval) in self.barrier_deps:
            need(P, bval)
        for t in reads:
            need(self.last_w.get(t))
        for t in writes:
            need(self.last_w.get(t))
            for r in self.readers.get(t, ()):
                need(r)
        for t in reads:
            self.readers.setdefault(t, []).append(op)
        for t in writes:
            self.last_w[t] = op
            self.readers[t] = []
        if op.is_dma:
            self.dma_tot[dma_key] = self.dma_tot.get(dma_key, 0) + 16
            op.dval = self.dma_tot[dma_key]
            self.last_dma[dma_key] = op
        op.deps = list(deps.values())
        self.eng_ops[eng].append(op)
        return op

    def barrier(self):
        deps = []
        for e in self.ENGS:
            for op in reversed(self.eng_ops[e]):
                if not op.is_dma:
                    deps.append((op, None))
                    break
        fz = getattr(self, "freeze_weights", False)
        deps.extend((P, self.dma_tot[P.dkey] if (fz and str(P.dkey)[:2] in ("wg", "wu", "wd")) else None)
                    for P in self.last_dma.values())
        self.barrier_deps = deps

    def emit(self):
        nc = self.nc
        for e in self.ENGS:
            for op in self.eng_ops[e]:
                for (P, _v) in op.deps:
                    if not P.is_dma:
                        P.signal = True
        for e in self.ENGS:
            c = 0
            for op in self.eng_ops[e]:
                if op.signal:
                    c += 1
                    op.tick = c
        with ExitStack() as st:
            esem = {e: st.enter_context(nc.semaphore("sem_" + e)) for e in self.ENGS}
            dsem = {k: st.enter_context(nc.semaphore("dsem_%d" % i)) for i, k in enumerate(self.dma_tot)}
            block = st.enter_context(nc.Block())

            def run(ename, eng):
                seen = {}
                regs = {}
                if ename in ("pe", "act", "dve", "pool", "sp"):
                    for rk in self.reg_keys:
                        regs[rk] = eng.alloc_register("r_%s_%s" % (ename, rk))
                self.cur_regs = regs

                def emit_op(op):
                    for (P, v) in op.deps:
                        if P.is_dma:
                            key = ("d", P.dkey)
                            sem = dsem[P.dkey]
                            val = v
                        else:
                            key = ("e", P.eng)
                            sem = esem[P.eng]
                            val = P.tick
                        if seen.get(key, 0) < val:
                            eng.wait_ge(sem, val)
                            seen[key] = val
                    inst = op.fn(eng)
                    if op.is_dma:
                        inst.then_inc(dsem[op.dkey], 16)
                    elif op.signal:
                        inst.then_inc(esem[ename], 1)

                ops = self.eng_ops[ename]
                i = 0
                tick_before = 0
                while i < len(ops):
                    reg = ops[i].region
                    j = i
                    while j < len(ops) and ops[j].region == reg:
                        j += 1
                    group = ops[i:j]
                    nsig = sum(1 for op in group if op.signal and not op.is_dma)
                    if reg is None:
                        for op in group:
                            emit_op(op)
                    else:
                        rk, thr = reg
                        with eng.If_lt(regs[rk], thr + 1):
                            if nsig:
                                if tick_before > 0:
                                    eng.wait_ge(esem[ename], tick_before)
                                eng.nop().then_inc(esem[ename], nsig)
                            else:
                                eng.nop()
                            dk = {}
                            for op in group:
                                if op.is_dma:
                                    first, n = dk.get(op.dkey, (op.dval - 16, 0))
                                    dk[op.dkey] = (first, n + 1)
                            for key, (first, n) in dk.items():
                                if first > 0:
                                    eng.wait_ge(dsem[key], first)
                                eng.nop().then_inc(dsem[key], 16 * n)
                        with eng.Else():
                            saved = dict(seen)
                            for op in group:
                                emit_op(op)
                            seen.clear()
                            seen.update(saved)
                    tick_before += nsig
                    i = j

            @block.tensor
            def _(eng):
                run("pe", eng)

            @block.scalar
            def _(eng):
                run("act", eng)

            @block.vector
            def _(eng):
                run("dve", eng)

            @block.gpsimd
            def _(eng):
                run("pool", eng)

            @block.sync
            def _(eng):
                run("sp", eng)


class Mem:
    def __init__(self, nc, sched):
        self.nc = nc
        self.s = sched
        self.off = 16512
        self.n = 0
        self.limit = 229376

    def alloc(self, name, shape, dtype):
        size = 1
        for d in shape[1:]:
            size *= d
        size *= 2 if dtype == BF16 else 4
        size = (size + 63) // 64 * 64
        self.n += 1
        t = self.nc.alloc_sbuf_tensor_at("%s_%d" % (name, self.n), list(shape), dtype, offset=self.off)
        self.off += size
        assert self.off <= self.limit, "SBUF overflow at %s: %d" % (name, self.off)
        return t

    def overlay(self, name, shape, dtype, off):
        self.n += 1
        return self.nc.alloc_sbuf_tensor_at("%s_%d" % (name, self.n), list(shape), dtype, offset=off)

    def mark(self):
        return self.off

    def release(self, mark):
        self.off = mark
        self.s.barrier()


class K:
    def __init__(self, nc):
        self.nc = nc
        self.s = Sched(nc)
        self.m = Mem(nc, self.s)
        self.st = ExitStack()
        self.psall = self.st.enter_context(nc.psum_tensor("psall", [128, 4096], F32))
        self.ps = [self.psall[:, i * 512:(i + 1) * 512] for i in range(8)]
        self.uid = 0

    def mm(self, out, pairs, reads, writes):
        n = len(pairs)

        def fn(eng, out=out, pairs=pairs, n=n):
            inst = None
            for i, (l, r) in enumerate(pairs):
                inst = eng.matmul(out, l, r, start=(i == 0), stop=(i == n - 1))
            return inst
        return self.s.add("pe", fn, reads, writes)

    def mm1(self, out, lhsT, rhs, start, stop, reads, writes):
        return self.s.add("pe", lambda eng, o=out, l=lhsT, r=rhs, a=start, b=stop: eng.matmul(o, l, r, start=a, stop=b),
                          reads, writes)

    def tr(self, out, in_, ident, reads, writes):
        return self.s.add("pe", lambda eng, o=out, i=in_, d=ident: eng.transpose(o, i, d), reads, writes)

    def act(self, out, in_, func, reads, writes, bias=None, scale=1.0, accum_out=None, eng="act"):
        def fn(e, out=out, in_=in_, func=func, bias=bias, scale=scale, accum_out=accum_out):
            kw = {}
            if bias is not None:
                kw["bias"] = bias
            if accum_out is not None:
                kw["accum_out"] = accum_out
            return e.activation(out=out, in_=in_, func=func, scale=scale, **kw)
        return self.s.add(eng, fn, reads, writes)

    def tt(self, out, in0, in1, op, reads, writes, eng="dve"):
        return self.s.add(eng, lambda e, o=out, a=in0, b=in1, p=op: e.tensor_tensor(o, a, b, p), reads, writes)

    def ts(self, out, in0, s1, s2, op0, op1, reads, writes, eng="dve"):
        def fn(e, o=out, a=in0, s1=s1, s2=s2, op0=op0, op1=op1):
            if op1 is None:
                return e.tensor_scalar(o, a, s1, None, op0)
            return e.tensor_scalar(o, a, s1, s2, op0, op1)
        return self.s.add(eng, fn, reads, writes)

    def stt(self, out, in0, scalar, in1, op0, op1, reads, writes, eng="dve"):
        return self.s.add(eng, lambda e, o=out, a=in0, sc=scalar, b=in1, p0=op0, p1=op1:
                          e.scalar_tensor_tensor(o, a, sc, b, p0, p1), reads, writes)

    def copy(self, out, in_, reads, writes, eng="dve"):
        if eng == "act":
            return self.s.add("act", lambda e, o=out, i=in_: e.copy(o, i), reads, writes)
        return self.s.add(eng, lambda e, o=out, i=in_: e.tensor_copy(o, i), reads, writes)

    def memset(self, ap, val, writes, eng="pool"):
        return self.s.add(eng, lambda e, a=ap, v=val: e.memset(a, v), (), writes)

    def dma(self, queue, out, in_, reads, writes, key):
        return self.s.add(queue, lambda e, o=out, i=in_: e.dma_start(out=o, in_=i), reads, writes, dma_key=key)

    def wdma(self, out, in_, tok, key):
        n = getattr(self, "_wn", 0)
        self._wn = n + 1
        reads = [("wdma", n - 2)] if n >= 2 else []
        return self.dma("pool", out, in_, reads, [tok, ("wdma", n)], key)

    @staticmethod
    def xtok(name, tile512, cs=range(NCH)):
        return [(name, c, tile512 * 4 + j) for c in cs for j in range(4)]

    def eps_ap(self, val):
        key = float(val)
        if key not in self.epsc:
            i = len(self.epsc)
            self.memset(self.epst[:, i:i + 1], key, ["epsc"], eng="dve")
            self.epsc[key] = self.epst[:, i:i + 1]
        return self.epsc[key]

    def setup_consts(self, cst):
        m = self.m
        self.c_f32 = m.alloc("cf32", [128, CONST_W], F32)
        self.dma("sp", self.c_f32[:], cst[:, :], (), ["cf32"], "cst")
        self.ident = self.c_f32[:, 0:128]
        self.ones_f = self.c_f32[:, 128:256]
        self.utri = self.c_f32[:, 256:384]
        self.maskneg = self.c_f32[:, 384:512]
        self.bd_f = self.c_f32[:, 512:640]
        self.sel = self.c_f32[0:8, 640:640 + 8 * 128]
        self.negutri = self.c_f32[:, 1664:1792]
        self.epst = m.alloc("epst", [128, 16], F32)
        self.epsc = {}
        self.c_bf = m.alloc("cbf", [128, 512], BF16)
        self.copy(self.c_bf[:, 0:128], self.ident, ["cf32"], ["cbf"])
        self.copy(self.c_bf[:, 128:256], self.ones_f, ["cf32"], ["cbf"])
        self.copy(self.c_bf[:, 256:384], self.bd_f, ["cf32"], ["cbf"])
        self.ident_b = self.c_bf[:, 0:128]
        self.ones_b = self.c_bf[:, 128:256]
        self.bd_b = self.c_bf[:, 256:384]
        self.copy(self.c_bf[:, 384:512], self.utri, ["cf32"], ["cbf"])
        self.utri_b = self.c_bf[:, 384:512]

    def load_xT(self, x_dram, xT, name, ntok=SEQH):
        m = self.m
        mk = m.mark()
        stg = [m.alloc("xstg", [128, D], F32) for _ in range(2)]
        for i in range(ntok // 128):
            sb = stg[i % 2]
            tk = "xstg%d" % (i % 2)
            self.dma("sp", sb[:], x_dram[i * 128:(i + 1) * 128, :], (), [tk], tk)
            for c in range(NCH):
                pb = self.ps[(i * NCH + c) % 2]
                pk = "ps%d" % ((i * NCH + c) % 2)
                self.tr(pb[:, 0:128], sb[:, c * 128:(c + 1) * 128], self.ident, [tk, "cf32"], [pk])
                eng = "act" if c % 2 == 0 else "dve"
                self.copy(xT[:, c, i * 128:(i + 1) * 128], pb[:, 0:128], [pk], [(name, c, i)], eng=eng)
        m.release(mk)

    def store_xT(self, xT, y_dram, name, ntok=SEQH, final=True):
        m = self.m
        mk = m.mark()
        stg = [m.alloc("ystg", [128, D], F32) for _ in range(2)]
        for i in range(ntok // 128):
            sb = stg[i % 2]
            tk = "ystg%d" % (i % 2)
            for c in range(NCH):
                pb = self.ps[(i * NCH + c) % 2]
                pk = "ps%d" % ((i * NCH + c) % 2)
                self.tr(pb[:, 0:128], xT[:, c, i * 128:(i + 1) * 128], self.ident, [(name, c, i), "cf32"], [pk])
                eng = "act" if c % 2 == 0 else "dve"
                self.copy(sb[:, c * 128:(c + 1) * 128], pb[:, 0:128], [pk], [tk], eng=eng)
            self.dma("sp", y_dram[i * 128:(i + 1) * 128, :], sb[:], [tk], [("yout", i)], "yout")
        if final:
            self.s.add("sp", lambda e: e.nop(), [("yout", i) for i in range(ntok // 128)], ())
        m.release(mk)

    def rmsnorm_T(self, xT, xname, g32, hT, hname, tsl, tile_id, sq, sqname, rstd, rname, ps_bank=6, htile=None):
        n = tsl.stop - tsl.start
        if htile is None:
            htile = tile_id
        pb = self.ps[ps_bank]
        pk = "ps%d" % ps_bank
        self.act(sq[:, :, 0:n], xT[:, :, tsl], AF.Square, self.xtok(xname, tile_id), [sqname])
        self.mm(pb[:, 0:n], [(self.ones_b, sq[:, c, 0:n]) for c in range(NCH)], [sqname, "cbf"], [pk])
        self.act(rstd[:, 0:n], pb[:, 0:n], AF.Ln, [pk, "epsc"], [rname], bias=self.eps_ap(D * EPS))
        self.act(rstd[:, 0:n], rstd[:, 0:n], AF.Exp, [rname], [rname], scale=-0.5)
        for c in range(NCH):
            self.stt(hT[:, c, 0:n] if hT.shape[2] == n else hT[:, c, tsl], xT[:, c, tsl], g32[:, c:c + 1], rstd[:, 0:n],
                     ALU.mult, ALU.mult, self.xtok(xname, tile_id, [c]) + [rname, "par"], [(hname, c, htile)],
                     eng="dve")

    def ffn_chunks(self, wg_d, wu_d, wd_d, F, gate_bc=None, gname=None, pre=None):
        FC = 512
        wgv = wg_d.rearrange("(c p) f -> p c f", p=128)
        wuv = wu_d.rearrange("(c p) f -> p c f", p=128)
        wdv = wd_d.rearrange("(s p) d -> p s d", p=128)
        out = []
        for fc in range((F + FC - 1) // FC):
            f0 = fc * FC
            fw = min(FC, F - f0)
            out.append(dict(wgv=wgv, wuv=wuv, wdv=wdv, f0=f0, fw=fw, gate_bc=gate_bc, gname=gname,
                            pre=pre if fc == 0 else None))
        return out

    def ffn_run(self, xT, xname, h2T, hname, chunks):
        nch = len(chunks)

        def load(ci):
            ch = chunks[ci]
            slot = ci % 2
            wg, wu, wd = self.wbuf[slot]
            kg, ku, kd = ("wg%d" % slot, "wu%d" % slot, "wd%d" % slot)
            f0, fw = ch["f0"], ch["fw"]
            nfs = fw // 128
            self.wdma(wg[:, :, 0:fw], ch["wgv"][:, :, f0:f0 + fw], kg, kg)
            self.wdma(wu[:, :, 0:fw], ch["wuv"][:, :, f0:f0 + fw], ku, ku)
            self.wdma(wd[:, 0:nfs, :], ch["wdv"][:, f0 // 128:f0 // 128 + nfs, :], kd, kd)

        units = [(ci, tt) for ci in range(nch) for tt in range(NTT)]

        def GU(ui):
            ci, tt = units[ui]
            ch = chunks[ci]
            if tt == 0 and ch["pre"] is not None:
                ch["pre"]()
            slot = ci % 2
            wg, wu, wd = self.wbuf[slot]
            kg, ku = "wg%d" % slot, "wu%d" % slot
            nfs = ch["fw"] // 128
            tsl = slice(tt * TT, (tt + 1) * TT)
            ab = ui % 2
            for fs in range(nfs):
                j = self.cnt % 2
                self.cnt += 1
                pg, pu = self.ps[j], self.ps[2 + j]
                kpg, kpu = "ps%d" % j, "ps%d" % (2 + j)
                hrd = [(hname, c, tt) for c in range(NCH)]
                self.mm(pg[:, :], [(wg[:, c, fs * 128:(fs + 1) * 128], h2T[:, c, tsl]) for c in range(NCH)], [kg] + hrd, [kpg])
                self.mm(pu[:, :], [(wu[:, c, fs * 128:(fs + 1) * 128], h2T[:, c, tsl]) for c in range(NCH)], [ku] + hrd, [kpu])
                sg = self.sgbuf[j]
                ksg = "sg%d" % j
                self.act(sg[:, :], pg[:, :], AF.Silu, [kpg], [ksg])
                if ch["gate_bc"] is not None:
                    self.tt(sg[:, :], sg[:, :], ch["gate_bc"][:, tsl], ALU.mult, [ksg, ch["gname"]], [ksg], eng="dve")
                self.tt(self.actbuf[ab][:, fs, :], pu[:, :], sg[:, :], ALU.mult, [kpu, ksg], [("actT", ab, fs)])

        def DN(ui):
            ci, tt = units[ui]
            ch = chunks[ci]
            slot = ci % 2
            wd = self.wbuf[slot][2]
            kd = "wd%d" % slot
            nfs = ch["fw"] // 128
            tsl = slice(tt * TT, (tt + 1) * TT)
            ab = ui % 2
            abuf = self.actbuf[ab]
            kab = [("actT", ab, fs) for fs in range(nfs)]
            for ds in range(NCH):
                j = self.cnt2 % 3
                self.cnt2 += 1
                pd = self.ps[4 + j]
                kpd = "ps%d" % (4 + j)
                self.mm(pd[:, :], [(wd[:, fs, ds * 128:(ds + 1) * 128], abuf[:, fs, :]) for fs in range(nfs)], [kd] + kab, [kpd])
                self.tt(xT[:, ds, tsl], pd[:, :], xT[:, ds, tsl], ALU.add, [kpd] + self.xtok(xname, tt, [ds]), self.xtok(xname, tt, [ds]))

        load(0)
        if nch > 1:
            load(1)
        GU(0)
        for ui in range(len(units)):
            if ui + 1 < len(units):
                GU(ui + 1)
            DN(ui)
            ci, tt = units[ui]
            if tt == NTT - 1 and ci + 2 < nch:
                load(ci + 2)

    def ffn(self, xT, xname, h2T, hname, wg_d, wu_d, wd_d, F, gate_bc=None, gname=None):
        self.ffn_run(xT, xname, h2T, hname, self.ffn_chunks(wg_d, wu_d, wd_d, F, gate_bc, gname))

    def ffn_bufs(self):
        m = self.m
        self.wbuf = [(m.alloc("wg", [128, NCH, 512], BF16), m.alloc("wu", [128, NCH, 512], BF16),
                      m.alloc("wd", [128, 4, D], BF16)) for _ in range(2)]
        self.sgbuf = [m.alloc("sg", [128, TT], F32) for _ in range(2)]
        self.actbuf = [m.alloc("actT", [128, 4, TT], BF16) for _ in range(2)]
        self.wslot = 0
        self.cnt = 0
        self.cnt2 = 0
        self.acnt = 0


CONST_W = 640 + 8 * 128 + 128


def make_consts():
    c = np.zeros((128, CONST_W), np.float32)
    c[:, 0:128] = np.eye(128, dtype=np.float32)
    c[:, 128:256] = 1.0
    r = np.arange(128)
    c[:, 256:384] = (r[:, None] <= r[None, :]).astype(np.float32)
    c[:, 384:512] = np.where(r[:, None] <= r[None, :], 0.0, NEG).astype(np.float32)
    bd = np.zeros((128, 128), np.float32)
    bd[:64, :64] = 1.0
    bd[64:, 64:] = 1.0
    c[:, 512:640] = bd
    for e in range(8):
        c[e, 640 + e * 128:640 + (e + 1) * 128] = 1.0
    c[:, 1664:1792] = -c[:, 256:384]
    return c


def pack_pp(v):
    return np.ascontiguousarray(np.asarray(v, np.float32).reshape(NCH, 128).T)


def build_ffn0():
    nc = bass.Bass("TRN2", target_bir_lowering=False)
    x = nc.dram_tensor("x", [SEQH, D], F32, kind="ExternalInput").ap()
    cst = nc.dram_tensor("cst", [128, CONST_W], F32, kind="ExternalInput").ap()
    par = nc.dram_tensor("par", [128, 8], F32, kind="ExternalInput").ap()
    wg = nc.dram_tensor("wg", [D, FFN_DIM], F32, kind="ExternalInput").ap()
    wu = nc.dram_tensor("wu", [D, FFN_DIM], F32, kind="ExternalInput").ap()
    wd = nc.dram_tensor("wd", [FFN_DIM, D], F32, kind="ExternalInput").ap()
    y = nc.dram_tensor("y", [SEQH, D], F32, kind="ExternalOutput").ap()
    k = K(nc)
    with k.st:
        m = k.m
        k.setup_consts(cst)
        xT = m.alloc("xT", [128, NCH, SEQH], F32)
        g = m.alloc("g", [128, 8], F32)
        g32 = m.alloc("g32", [128, 8], F32)
        k.dma("sp", g[:], par[:, :], (), ["graw"], "par")
        k.ts(g32[:], g[:], 32.0, None, ALU.mult, None, ["graw"], ["par"])
        k.load_xT(x, xT, "xT")
        h2T = m.alloc("h2T", [128, NCH, SEQH], BF16)
        mk = m.mark()
        sq = m.alloc("sq", [128, NCH, TT], BF16)
        rstd = m.alloc("rstd", [128, TT], F32)
        for tt in range(NTT):
            k.rmsnorm_T(xT, "xT", g32, h2T, "h2T", slice(tt * TT, (tt + 1) * TT), tt, sq, "sq", rstd, "rstd")
        m.release(mk)
        k.ffn_bufs()
        k.ffn(xT, "xT", h2T, "h2T", wg, wu, wd, FFN_DIM)
        k.store_xT(xT, y, "xT")
        k.s.emit()
    return nc


_cache = {}


def run_ffn0(x_shards, ln2_g0, wg, wu, wd):
    if "ffn0" not in _cache:
        _cache["ffn0"] = build_ffn0()
    nc = _cache["ffn0"]
    cst = make_consts()
    par = pack_pp(ln2_g0)
    in_maps = [{"x": np.ascontiguousarray(xs), "cst": cst, "par": par, "wg": wg, "wu": wu, "wd": wd} for xs in x_shards]
    res = run_bass_kernel_spmd(nc, in_maps, core_ids=list(range(8)))
    return [r["y"] for r in res.results]


def moe_block(k, xT, xname, h2T, hname, wr_d, wg_d, wu_d, wd_d):
    m = k.m
    NS = SEQH // 128
    wr = m.alloc("wr", [128, NCH, 8], BF16)
    k.dma("pool", wr[:], wr_d.rearrange("(c p) e -> p c e", p=128), (), ["wr"], "wr")
    lg = m.alloc("lg", [128, NS, 8], F32)
    pb = k.ps[7]
    for i in range(NS):
        k.mm(pb[:, i * 8:(i + 1) * 8], [(h2T[:, c, i * 128:(i + 1) * 128], wr[:, c, :]) for c in range(NCH)],
             ["wr"] + [(hname, c, i // 4) for c in range(NCH)], ["ps7"])
    k.copy(lg[:].rearrange("p a b -> p (a b)"), pb[:, 0:NS * 8], ["ps7"], ["lg"], eng="act")
    mk = m.mark()
    m1 = m.alloc("m1", [128, NS], F32)
    m2 = m.alloc("m2", [128, NS], F32)
    eq1 = m.alloc("eq1", [128, NS, 8], F32)
    eq2 = m.alloc("eq2", [128, NS, 8], F32)
    lg2 = m.alloc("lg2", [128, NS, 8], F32)
    w1 = m.alloc("w1", [128, NS], F32)
    w2 = m.alloc("w2", [128, NS], F32)
    gates = m.alloc("gates", [128, NS, 8], F32)

    def bc(t):
        return t[:, :].unsqueeze(2).broadcast_to([128, NS, 8])

    s = k.s
    s.add("dve", lambda e: e.tensor_reduce(m1[:, :], lg[:, :, :], AX.X, ALU.max), ["lg"], ["m1"])
    k.tt(eq1[:], lg[:], bc(m1), ALU.is_equal, ["lg", "m1"], ["eq1"])
    k.stt(lg2[:], eq1[:], -1e30, lg[:], ALU.mult, ALU.add, ["eq1", "lg"], ["lg2"])
    s.add("dve", lambda e: e.tensor_reduce(m2[:, :], lg2[:, :, :], AX.X, ALU.max), ["lg2"], ["m2"])
    k.tt(eq2[:], lg2[:], bc(m2), ALU.is_equal, ["lg2", "m2"], ["eq2"])
    k.tt(w1[:], m1[:], m2[:], ALU.subtract, ["m1", "m2"], ["w1"])
    k.act(w1[:], w1[:], AF.Sigmoid, ["w1"], ["w1"])
    k.ts(w2[:], w1[:], -1.0, 1.0, ALU.mult, ALU.add, ["w1"], ["w2"])
    k.tt(eq1[:], eq1[:], bc(w1), ALU.mult, ["eq1", "w1"], ["eq1"])
    k.tt(eq2[:], eq2[:], bc(w2), ALU.mult, ["eq2", "w2"], ["eq2"])
    k.tt(gates[:], eq1[:], eq2[:], ALU.add, ["eq1", "eq2"], ["gates"])
    gT = m.alloc("gT", [8, SEQH], F32)
    for t4 in range(NTT):
        pbk = k.ps[6]
        for j in range(4):
            i = t4 * 4 + j
            k.tr(pbk[0:8, j * 128:(j + 1) * 128], gates[:, i, :], k.ident, ["gates", "cf32"], ["ps6"])
        k.copy(gT[:, t4 * TT:(t4 + 1) * TT], pbk[0:8, :], ["ps6"], [("gT", t4)], eng="act")
    gbc = [m.alloc("gbc", [128, SEQH], F32) for _ in range(2)]
    k.ffn_bufs()

    def make_pre(e):
        def pre():
            gb = gbc[e % 2]
            gk = "gbc%d" % (e % 2)
            for t4 in range(NTT):
                pbk = k.ps[7]
                pkk = "ps7"
                k.mm(pbk[:, :], [(k.sel[:, e * 128:(e + 1) * 128], gT[:, t4 * TT:(t4 + 1) * TT])], [("gT", t4), "cf32"], [pkk])
                k.copy(gb[:, t4 * TT:(t4 + 1) * TT], pbk[:, :], [pkk], [gk], eng="act")
        return pre

    chunks = []
    for e in range(N_EXPERTS):
        chunks += k.ffn_chunks(wg_d[e], wu_d[e], wd_d[e], EXPERT_DIM, gate_bc=gbc[e % 2], gname="gbc%d" % (e % 2),
                               pre=make_pre(e))
    k.ffn_run(xT, xname, h2T, hname, chunks)


CAP = SEQH
ST = 512
NSLT = CAP // ST
I32 = mybir.dt.int32


def hc_zero_fill(k, zt, hc_d):
    k.memset(zt[:, :], 0.0, ["zt"], eng="pool")
    for j in range(N_EXPERTS * CAP // 128):
        k.dma("sp", hc_d[j * 128:(j + 1) * 128, :], zt[:, :], ["zt"], ["hcz"], "hcz")


def moe_sparse(k, xT, xname, xT_off, g32, wr_d, wg_d, wu_d, wd_d, y_dram, hc_d, yc_d, cnt_d):
    m = k.m
    s = k.s
    NS = SEQH // 128
    NFC = EXPERT_DIM // 512
    k.ffn_bufs()
    chunks = []
    for e in range(N_EXPERTS):
        wgv = wg_d[e].rearrange("(c p) f -> p c f", p=128)
        wuv = wu_d[e].rearrange("(c p) f -> p c f", p=128)
        wdv = wd_d[e].rearrange("(s p) d -> p s d", p=128)
        for fc in range(NFC):
            chunks.append(dict(e=e, fc=fc, f0=fc * 512, wgv=wgv, wuv=wuv, wdv=wdv))
    nch = len(chunks)

    def load(ci):
        ch = chunks[ci]
        slot = ci % 2
        wg, wu, wd = k.wbuf[slot]
        kg, ku, kd = ("wg%d" % slot, "wu%d" % slot, "wd%d" % slot)
        f0 = ch["f0"]
        k.wdma(wg[:, :, :], ch["wgv"][:, :, f0:f0 + 512], kg, kg)
        k.wdma(wu[:, :, :], ch["wuv"][:, :, f0:f0 + 512], ku, ku)
        k.wdma(wd[:, :, :], ch["wdv"][:, f0 // 128:f0 // 128 + 4, :], kd, kd)

    load(0)
    load(1)
    d1i = m.alloc("d1i", [128, NS], I32)
    d2i = m.alloc("d2i", [128, NS], I32)
    w1 = m.alloc("w1", [128, NS], F32)
    w2 = m.alloc("w2", [128, NS], F32)
    cnti = m.alloc("cnti", [128, 8], I32)
    stg_off = [m.off, m.off + 8192]
    stg = [m.alloc("stg", [128, 4, D], BF16) for _ in range(2)]
    h2T_off = m.off
    h2T = m.alloc("h2T", [128, NCH, SEQH], BF16)
    hname = "h2T"
    mkA = m.mark()
    sq = m.overlay("sq", [128, NCH, TT], BF16, stg_off[0])
    rstd = m.overlay("rstd", [128, TT], F32, stg_off[1])
    sst = [m.overlay("sst", [128, D], BF16, stg_off[1] + 2048 * (1 + j)) for j in range(2)]
    for tt in range(NTT):
        k.rmsnorm_T(xT, xname, g32, h2T, hname, slice(tt * TT, (tt + 1) * TT), tt, sq, "sq", rstd, "rstd")
    wr = m.alloc("wr", [128, NCH, 8], BF16)
    wrf = m.alloc("wrf", [128, NCH, 8], F32)
    k.dma("sp", wrf[:], wr_d.rearrange("(c p) e -> p c e", p=128), (), ["wrf"], "wrf")
    k.copy(wr[:], wrf[:], ["wrf"], ["wr"])
    lg = m.alloc("lg", [128, NS, 8], F32)
    pb = k.ps[7]
    for i in range(NS):
        k.mm(pb[:, i * 8:(i + 1) * 8], [(h2T[:, c, i * 128:(i + 1) * 128], wr[:, c, :]) for c in range(NCH)],
             ["wr"] + [(hname, c, i // 4) for c in range(NCH)], ["ps7"])
    k.copy(lg[:].rearrange("p a b -> p (a b)"), pb[:, 0:NS * 8], ["ps7"], ["lg"], eng="act")
    m1 = m.alloc("m1", [128, NS], F32)
    m2 = m.alloc("m2", [128, NS], F32)
    eq1 = m.alloc("eq1", [128, NS, 8], F32)
    eq2 = m.alloc("eq2", [128, NS, 8], F32)
    lg2 = m.alloc("lg2", [128, NS, 8], F32)
    mask = m.alloc("mask", [128, NS, 8], F32)
    maskb = m.alloc("maskb", [128, NS, 8], BF16)
    it = m.alloc("it", [128, 2, NS, 8], F32)
    off = m.alloc("off", [128, NS, 8], F32)
    ebase = m.alloc("ebase", [128, NS, 8], F32)
    rank = m.alloc("rank", [128, NS, 8], F32)
    tmp = m.alloc("rtmp", [128, NS, 8], F32)
    d1f = m.alloc("d1f", [128, NS], F32)
    d2f = m.alloc("d2f", [128, NS], F32)
    cntf = m.alloc("cntf", [128, 8], F32)

    def bc(t):
        return t[:, :].unsqueeze(2).broadcast_to([128, NS, 8])

    s.add("dve", lambda e: e.tensor_reduce(m1[:, :], lg[:, :, :], AX.X, ALU.max), ["lg"], ["m1"])
    k.tt(eq1[:], lg[:], bc(m1), ALU.is_equal, ["lg", "m1"], ["eq1"])
    k.stt(lg2[:], eq1[:], -1e30, lg[:], ALU.mult, ALU.add, ["eq1", "lg"], ["lg2"])
    s.add("dve", lambda e: e.tensor_reduce(m2[:, :], lg2[:, :, :], AX.X, ALU.max), ["lg2"], ["m2"])
    k.tt(eq2[:], lg2[:], bc(m2), ALU.is_equal, ["lg2", "m2"], ["eq2"])
    k.tt(w1[:], m1[:], m2[:], ALU.subtract, ["m1", "m2"], ["w1"])
    k.act(w1[:], w1[:], AF.Sigmoid, ["w1"], ["w1"])
    k.ts(w2[:], w1[:], -1.0, 1.0, ALU.mult, ALU.add, ["w1"], ["w2"])
    k.tt(mask[:], eq1[:], eq2[:], ALU.add, ["eq1", "eq2"], ["mask"])
    k.copy(maskb[:], mask[:], ["mask"], ["maskb"])
    for e in range(N_EXPERTS):
        k.memset(ebase[:, :, e:e + 1], float(e * CAP), ["ebase"], eng="dve")
    mb2 = maskb[:].rearrange("p a b -> p (a b)")
    k.mm(k.ps[6][:, 0:128], [(k.utri_b, mb2)], ["maskb", "cbf"], ["ps6"])
    k.mm(k.ps[6][:, 128:256], [(k.ones_b, mb2)], ["maskb", "cbf"], ["ps6"])
    k.copy(it[:].rearrange("p t a b -> p (t a b)"), k.ps[6][:, 0:256], ["ps6"], ["it"], eng="act")
    k.memset(off[:, 0, :], 0.0, [("off", 0)], eng="dve")
    for i in range(1, NS):
        k.tt(off[:, i, :], off[:, i - 1, :], it[:, 1, i - 1, :], ALU.add, [("off", i - 1), "it"], [("off", i)])
    k.tt(cntf[:, :], off[:, NS - 1, :], it[:, 1, NS - 1, :], ALU.add, [("off", NS - 1), "it"], ["cntf"])
    k.copy(cnti[:, :], cntf[:, :], ["cntf"], ["cnti"])
    k.dma("sp", cnt_d[0:1, :], cnti[0:1, :], ["cnti"], ["cntd"], "cntd")
    offr = [("off", i) for i in range(NS)]
    k.tt(rank[:], it[:, 0, :, :], mask[:], ALU.subtract, ["it", "mask"], ["rank"])
    k.tt(rank[:], rank[:], off[:], ALU.add, ["rank"] + offr, ["rank"])
    k.tt(rank[:], rank[:], ebase[:], ALU.add, ["rank", "ebase"], ["rank"])
    k.tt(tmp[:], rank[:], eq1[:], ALU.mult, ["rank", "eq1"], ["rtmp"])
    s.add("dve", lambda e: e.tensor_reduce(d1f[:, :], tmp[:, :, :], AX.X, ALU.add), ["rtmp"], ["d1f"])
    k.copy(d1i[:, :], d1f[:, :], ["d1f"], ["d1i"])
    k.tt(tmp[:], rank[:], eq2[:], ALU.mult, ["rank", "eq2", "d1f"], ["rtmp"])
    s.add("dve", lambda e: e.tensor_reduce(d2f[:, :], tmp[:, :, :], AX.X, ALU.add), ["rtmp"], ["d2f"])
    k.copy(d2i[:, :], d2f[:, :], ["d2f"], ["d2i"])
    s.reg_keys = list(range(N_EXPERTS))
    for en in ("pe", "act", "dve", "pool", "sp"):
        for e in range(N_EXPERTS):
            s.add(en, lambda eng, e=e: eng.reg_load(s.cur_regs[e], cnt_d[0:1, e:e + 1]), ["cntd"], ())
    for i in range(NS):
        j = i % 2
        pbf = k.ps[j].bitcast(BF16)
        pk = "ps%d" % j
        for c in range(NCH):
            k.tr(pbf[:, c * 128:(c + 1) * 128], h2T[:, c, i * 128:(i + 1) * 128], k.ident_b,
                 [(hname, c, i // 4), "cbf"], [pk])
        tk = "sst%d" % j
        k.copy(sst[j][:, :], pbf[:, 0:1024], [pk], [tk], eng="act" if j == 0 else "dve")
        for a, di in enumerate((d1i, d2i)):
            s.add("pool", lambda eng, di=di, i=i, j=j: eng.indirect_dma_start(
                out=hc_d[:, :], out_offset=bass.IndirectOffsetOnAxis(ap=di[:, i:i + 1], axis=0),
                in_=sst[j][:, :], in_offset=None),
                [tk, "hcz", "d1i", "d2i"], [("hcs", i, a)], dma_key="hcs")
    hcs_tokens = [("hcs", i, a) for i in range(NS) for a in range(2)]
    k.store_xT(xT, y_dram, xname, final=False)
    s.freeze_weights = True
    m.release(mkA)
    yacc = m.overlay("yacc", [128, CAP // 128, D], F32, xT_off)
    hTe = m.overlay("hTe", [128, NCH, CAP], BF16, h2T_off)

    def prep(e, tts, region=None):
        for tt in tts:
            sb = stg[tt % 2]
            tk = "stg%d" % (tt % 2)
            r0 = e * CAP + tt * ST
            s.cur_region = region
            k.dma("sp", sb[:, :, :], hc_d[r0:r0 + ST, :].rearrange("(b p) d -> p b d", p=128), hcs_tokens, [tk], tk)
            for blk in range(4):
                pbf = k.ps[7].bitcast(BF16)
                for c in range(NCH):
                    k.tr(pbf[:, c * 128:(c + 1) * 128], sb[:, blk, c * 128:(c + 1) * 128], k.ident_b, [tk, "cbf"], ["ps7"])
                col = (tt * 4 + blk) * 128
                k.copy(hTe[:, :, col:col + 128], pbf[:, 0:1024].rearrange("p (c q) -> p c q", c=NCH), ["ps7"],
                       [("hTe", tt * 4 + blk)], eng="act")
            s.cur_region = None

    MAINW = 640
    main_tiles = [(0, 512, 0), (512, 128, 1)]
    rare_tiles = [(640, 384, 1), (1024, 512, 0), (1536, 512, 1)]

    def GU(ci, tile, region):
        ch = chunks[ci]
        t0, tw, ab = tile
        s.cur_region = region
        slot = ci % 2
        wg, wu, wd = k.wbuf[slot]
        kg, ku = "wg%d" % slot, "wu%d" % slot
        tsl = slice(t0, t0 + tw)
        hrd = [("hTe", b_) for b_ in range(t0 // 128, (t0 + tw) // 128)]
        for fs in range(4):
            j = k.cnt % 2
            k.cnt += 1
            pg, pu = k.ps[j], k.ps[2 + j]
            kpg, kpu = "ps%d" % j, "ps%d" % (2 + j)
            k.mm(pg[:, 0:tw], [(wg[:, c, fs * 128:(fs + 1) * 128], hTe[:, c, tsl]) for c in range(NCH)], [kg] + hrd, [kpg])
            k.mm(pu[:, 0:tw], [(wu[:, c, fs * 128:(fs + 1) * 128], hTe[:, c, tsl]) for c in range(NCH)], [ku] + hrd, [kpu])
            sg = k.sgbuf[j]
            ksg = "sg%d" % j
            k.act(sg[:, 0:tw], pg[:, 0:tw], AF.Silu, [kpg], [ksg])
            k.tt(k.actbuf[ab][:, fs, 0:tw], pu[:, 0:tw], sg[:, 0:tw], ALU.mult, [kpu, ksg], [("actT", ab, fs)])
        s.cur_region = None

    def DN(ci, tile, region):
        ch = chunks[ci]
        t0, tw, ab = tile
        s.cur_region = region
        wd = k.wbuf[ci % 2][2]
        kd = "wd%d" % (ci % 2)
        abuf = k.actbuf[ab]
        kab = [("actT", ab, fs) for fs in range(4)]
        for blk in range(tw // 128):
            gb = t0 // 128 + blk
            for dh in range(2):
                j = k.cnt2 % 3
                k.cnt2 += 1
                pd = k.ps[4 + j]
                kpd = "ps%d" % (4 + j)
                k.mm(pd[:, :], [(abuf[:, fs, blk * 128:(blk + 1) * 128], wd[:, fs, dh * 512:(dh + 1) * 512]) for fs in range(4)],
                     [kd] + kab, [kpd])
                dst = yacc[:, gb, dh * 512:(dh + 1) * 512]
                tok = ("yacc", gb, dh)
                if ch["fc"] == 0:
                    k.copy(dst, pd[:, :], [kpd], [tok])
                else:
                    k.tt(dst, pd[:, :], dst, ALU.add, [kpd, tok], [tok])
        s.cur_region = None

    def ystore(e, gb0, gb1, region=None):
        r0 = e * CAP + gb0 * 128
        s.cur_region = region
        k.dma("sp", yc_d[r0:r0 + (gb1 - gb0) * 128, :].rearrange("(b p) d -> p b d", p=128), yacc[:, gb0:gb1, :],
              [("yacc", gb, dh) for gb in range(gb0, gb1) for dh in range(2)], [("ycd", e, gb0)], "ycd")
        s.cur_region = None

    ycd_tokens = []
    prep(0, (0, 1))
    for ci in range(nch):
        e = chunks[ci]["e"]
        lastc = chunks[ci]["fc"] == NFC - 1
        GU(ci, main_tiles[0], None)
        GU(ci, main_tiles[1], None)
        if lastc and e + 1 < N_EXPERTS:
            prep(e + 1, (0, 1))
        DN(ci, main_tiles[0], None)
        DN(ci, main_tiles[1], None)
        if lastc:
            ystore(e, 0, 4)
            ystore(e, 4, 5)
            ycd_tokens += [("ycd", e, 0), ("ycd", e, 4)]
        if ci + 2 < nch:
            load(ci + 2)
    for e in range(N_EXPERTS):
        for (reg, ptiles, rtiles, stores) in (((e, MAINW), (1,), rare_tiles[0:1], ((5, 8),)),
                                              ((e, 2 * ST), (2, 3), rare_tiles[1:3], ((8, 12), (12, 16)))):
            prep(e, ptiles, region=reg)
            for fc in range(NFC):
                ci = e * NFC + fc
                s.cur_region = reg
                load(ci)
                s.cur_region = None
                for tile in rtiles:
                    GU(ci, tile, reg)
                    DN(ci, tile, reg)
            for (g0, g1) in stores:
                ystore(e, g0, g1, region=reg)
                ycd_tokens.append(("ycd", e, g0))
    mkB = m.mark()
    xs = [m.alloc("xs", [128, D], F32) for _ in range(2)]
    a1 = [m.alloc("a1", [128, D], F32) for _ in range(2)]
    a2 = [m.alloc("a2", [128, D], F32) for _ in range(2)]
    for i in range(NS):
        j = i % 2
        k.dma("sp", xs[j][:, :], y_dram[i * 128:(i + 1) * 128, :], [("yout", i)], ["xs%d" % j], "xs%d" % j)
        for (ab_, di, nm) in ((a1, d1i, "a1"), (a2, d2i, "a2")):
            s.add("pool", lambda eng, ab_=ab_, di=di, i=i, j=j: eng.indirect_dma_start(
                out=ab_[j][:, :], out_offset=None, in_=yc_d[:, :],
                in_offset=bass.IndirectOffsetOnAxis(ap=di[:, i:i + 1], axis=0)),
                ycd_tokens + ["d1i", "d2i"], ["%s%d" % (nm, j)], dma_key="%s%d" % (nm, j))
        k.stt(xs[j][:, :], a1[j][:, :], w1[:, i:i + 1], xs[j][:, :], ALU.mult, ALU.add, ["a1%d" % j, "xs%d" % j, "w1"], ["xs%d" % j])
        k.stt(xs[j][:, :], a2[j][:, :], w2[:, i:i + 1], xs[j][:, :], ALU.mult, ALU.add, ["a2%d" % j, "xs%d" % j, "w2"], ["xs%d" % j])
        k.dma("sp", y_dram[i * 128:(i + 1) * 128, :], xs[j][:, :], ["xs%d" % j], [("yfin", i)], "yfin")
    s.add("sp", lambda e: e.nop(), [("yfin", i) for i in range(NS)], ())
    m.release(mkB)


def build_moe():
    nc = bass.Bass("TRN2", target_bir_lowering=False)
    x = nc.dram_tensor("x", [SEQH, D], F32, kind="ExternalInput").ap()
    cst = nc.dram_tensor("cst", [128, CONST_W], F32, kind="ExternalInput").ap()
    par = nc.dram_tensor("par", [128, 8], F32, kind="ExternalInput").ap()
    wr = nc.dram_tensor("wr", [D, N_EXPERTS], F32, kind="ExternalInput").ap()
    wg = nc.dram_tensor("wg", [N_EXPERTS, D, EXPERT_DIM], F32, kind="ExternalInput").ap()
    wu = nc.dram_tensor("wu", [N_EXPERTS, D, EXPERT_DIM], F32, kind="ExternalInput").ap()
    wd = nc.dram_tensor("wd", [N_EXPERTS, EXPERT_DIM, D], F32, kind="ExternalInput").ap()
    y = nc.dram_tensor("y", [SEQH, D], F32, kind="ExternalOutput").ap()
    hc_d = nc.dram_tensor("hcd", [N_EXPERTS * CAP, D], BF16).ap()
    yc_d = nc.dram_tensor("ycd", [N_EXPERTS * CAP, D], F32).ap()
    cnt_d = nc.dram_tensor("cntd", [1, 8], I32).ap()
    k = K(nc)
    with k.st:
        m = k.m
        k.setup_consts(cst)
        xT_off = m.off
        xT = m.alloc("xT", [128, NCH, SEQH], F32)
        g = m.alloc("g", [128, 8], F32)
        g32 = m.alloc("g32", [128, 8], F32)
        zt = m.alloc("zt", [128, D], BF16)
        k.dma("sp", g[:], par[:, :], (), ["graw"], "par")
        k.ts(g32[:], g[:], 32.0, None, ALU.mult, None, ["graw"], ["par"])
        k.load_xT(x, xT, "xT")
        hc_zero_fill(k, zt, hc_d)
        moe_sparse(k, xT, "xT", xT_off, g32, wr, wg, wu, wd, y, hc_d, yc_d, cnt_d)
        k.s.emit()
    return nc


def run_moe(x_shards, ln2_g1, wr, wg, wu, wd, trace=False):
    if "moe" not in _cache:
        _cache["moe"] = build_moe()
    nc = _cache["moe"]
    cst = make_consts()
    par = pack_pp(ln2_g1)
    in_maps = [{"x": np.ascontiguousarray(xs), "cst": cst, "par": par, "wr": wr, "wg": wg, "wu": wu, "wd": wd}
               for xs in x_shards]
    res = run_bass_kernel_spmd(nc, in_maps, core_ids=list(range(8)), trace=trace)
    return [r["y"] for r in res.results], res


PA_W = 24 + 256


def pack_par_attn(inp, layer, flag):
    p = np.zeros((128, PA_W), np.float32)
    p[:, 0:8] = pack_pp(inp["ln1_g"][layer])
    p[:, 8:16] = pack_pp(inp["mem_norm_g"])
    p[:, 16] = np.tile(np.asarray(inp["da_qn_g"][0], np.float32), 2)
    p[:, 17] = np.tile(np.asarray(inp["da_kn_g"][0], np.float32), 2)
    p[:, 18] = np.tile(np.asarray(inp["mem_qn_g"][layer], np.float32), 2)
    p[:, 19] = np.tile(np.asarray(inp["mem_kn_g"][layer], np.float32), 2)
    p[:, 20] = np.asarray(inp["da_sub_g"][0], np.float32)
    p[:, 21] = flag
    p[:, 22] = NEG if flag == 0 else 0.0
    lv = np.concatenate([np.asarray(inp[n][0], np.float32) for n in ("da_lq1", "da_lk1", "da_lq2", "da_lk2")])
    p[:, 24:24 + 256] = lv[None, :]
    return p


def headnorm(k, raw, P, n, gain, ones_bf, out, reads, writes, tmp):
    sq, ksq, rs, krs, pb, kpb = tmp["sq"], tmp["ksq"], tmp["rs"], tmp["krs"], tmp["pb"], tmp["kpb"]
    k.act(sq[0:P, 0:n], raw, AF.Square, reads, [ksq])
    k.mm(pb[0:P, 0:n], [(ones_bf, sq[0:P, 0:n])], [ksq, "cbf"], [kpb])
    k.act(rs[0:P, 0:n], pb[0:P, 0:n], AF.Ln, [kpb, "epsc"], [krs], bias=k.eps_ap(64 * EPS)[0:P, :])
    k.act(rs[0:P, 0:n], rs[0:P, 0:n], AF.Exp, [krs], [krs], scale=-0.5)
    k.stt(out, raw, gain, rs[0:P, 0:n], ALU.mult, ALU.mult, list(reads) + [krs, "gains"], writes)


def mem_kv_setup(k, mem_d, wkv_d, gmem32, gmk8, KmT, Vm, lname):
    m = k.m
    mk = m.mark()
    memT = m.alloc("memT", [128, NCH, MEM_LEN], F32)
    k.load_xT(mem_d, memT, "memT" + lname, ntok=MEM_LEN)
    mnT = m.alloc("mnT", [128, NCH, MEM_LEN], BF16)
    sq = m.alloc("msq", [128, NCH, MEM_LEN], BF16)
    rstd = m.alloc("mrstd", [128, MEM_LEN], F32)
    pb = k.ps[6]
    rd = [("memT" + lname, c, i) for c in range(NCH) for i in range(2)]
    k.act(sq[:, :, :], memT[:, :, :], AF.Square, rd, ["msq"])
    k.mm(pb[:, 0:MEM_LEN], [(k.ones_b, sq[:, c, :]) for c in range(NCH)], ["msq", "cbf"], ["ps6"])
    k.act(rstd[:, :], pb[:, 0:MEM_LEN], AF.Ln, ["ps6", "epsc"], ["mrstd"], bias=k.eps_ap(D * EPS))
    k.act(rstd[:, :], rstd[:, :], AF.Exp, ["mrstd"], ["mrstd"], scale=-0.5)
    for c in range(NCH):
        k.stt(mnT[:, c, :], memT[:, c, :], gmem32[:, c:c + 1], rstd[:, :], ALU.mult, ALU.mult,
              rd + ["mrstd", "gains"], [("mnT", c)])
    wkv = m.alloc("wkv", [128, NCH, 512], BF16)
    k.dma("pool", wkv[:], wkv_d.rearrange("(c p) f -> p c f", p=128), (), ["wkv"], "wkv")
    tmp = dict(sq=m.alloc("hsq", [128, 512], BF16), ksq="hsq", rs=m.alloc("hrs", [128, 512], F32), krs="hrs",
               pb=k.ps[7], kpb="ps7")
    mn_rd = [("mnT", c) for c in range(NCH)]
    for hm in range(4):
        pr = k.ps[hm % 2]
        kpr = "ps%d" % (hm % 2)
        k.mm(pr[0:64, 0:MEM_LEN], [(wkv[:, c, hm * 64:(hm + 1) * 64], mnT[:, c, :]) for c in range(NCH)],
             ["wkv"] + mn_rd, [kpr])
        headnorm(k, pr[0:64, 0:MEM_LEN], 64, MEM_LEN, gmk8[0:64, :], k.ones_b[0:64, 0:64], KmT[0:64, hm, :],
                 [kpr], [("KmT" + lname, hm)], tmp)
    for mt in range(2):
        pr = k.ps[2 + mt]
        kpr = "ps%d" % (2 + mt)
        k.mm(pr[:, 0:256], [(mnT[:, c, mt * 128:(mt + 1) * 128], wkv[:, c, 256:512]) for c in range(NCH)],
             ["wkv"] + mn_rd, [kpr])
        k.copy(Vm[:, mt, :], pr[:, 0:256], [kpr], [("Vm" + lname, mt)], eng="act")
    m.release(mk)


def attn_inproj(k, x_tiles, xT_own, g32, win, gq8, gk8, gmq8, QT, mqT, kscr, vscr, ctx0):
    m = k.m
    mk = m.mark()
    xtmp = m.alloc("xtmp", [128, NCH, TT], F32) if any(o is None for (_x, o) in x_tiles) else None
    hT = m.alloc("hT", [128, NCH, TT], BF16)
    sq = m.alloc("sq", [128, NCH, TT], BF16)
    rstd = m.alloc("rstd", [128, TT], F32)
    tmps = [dict(sq=m.alloc("hsq", [128, 512], BF16), ksq="hsq%d" % j, rs=m.alloc("hrs", [128, 512], F32), krs="hrs%d" % j,
                 pb=k.ps[7 - j], kpb="ps%d" % (7 - j)) for j in range(2)]
    hn = [0]

    def tmp_next():
        hn[0] += 1
        return tmps[hn[0] % 2]
    kt_sb = [m.alloc("ktsb", [128, TT], BF16) for _ in range(2)]
    vt_sb = [m.alloc("vtsb", [128, TOK_W], BF16) for _ in range(2)]
    stg = [m.alloc("xstg", [128, D], F32) for _ in range(2)]
    cnt = 0
    vcnt = 0
    for ti, (xd, own) in enumerate(x_tiles):
        g = ctx0 + ti
        if own is None:
            dst, dname, dtile, dsl = xtmp, "xtmp", 0, slice(0, TT)
        else:
            dst, dname, dtile, dsl = xT_own, "xT", own, slice(own * TT, (own + 1) * TT)
        for i in range(4):
            sb = stg[i % 2]
            tk = "xstg%d" % (i % 2)
            k.dma("sp", sb[:], xd[i * 128:(i + 1) * 128, :], (), [tk], tk)
            for c in range(NCH):
                pb = k.ps[(i * NCH + c) % 2]
                pk = "ps%d" % ((i * NCH + c) % 2)
                k.tr(pb[:, 0:128], sb[:, c * 128:(c + 1) * 128], k.ident, [tk, "cf32"], [pk])
                k.copy(dst[:, c, dsl.start + i * 128:dsl.start + (i + 1) * 128], pb[:, 0:128], [pk],
                       [(dname, c, dtile * 4 + i)], eng="act" if c % 2 == 0 else "dve")
        k.rmsnorm_T(dst, dname, g32, hT, "hT", dsl, dtile, sq, "sq", rstd, "rstd", htile=0)
        h_rd = [("hT", c, 0) for c in range(NCH)]
        for h in range(6):
            pr = k.ps[2 + cnt % 2]
            kpr = "ps%d" % (2 + cnt % 2)
            k.mm(pr[:, :], [(win[:, c, 768 + h * 128:768 + (h + 1) * 128], hT[:, c, :]) for c in range(NCH)],
                 ["win"] + h_rd, [kpr])
            kb = kt_sb[cnt % 2]
            kkb = "ktsb%d" % (cnt % 2)
            cnt += 1
            headnorm(k, pr[:, :], 128, TT, gk8, k.bd_b, kb[:, :], [kpr], [kkb], tmp_next())
            k.dma("sp", kscr[h, :, g * TT:(g + 1) * TT], kb[:, :], [kkb], [("kscr", h, g)], "kscr")
        for i in range(4):
            vb = vt_sb[vcnt % 2]
            kvb = "vtsb%d" % (vcnt % 2)
            vcnt += 1
            for (c0, cw, bank) in ((0, 512, 4), (512, 256, 5)):
                pr = k.ps[bank]
                kpr = "ps%d" % bank
                k.mm(pr[:, 0:cw], [(hT[:, c, i * 128:(i + 1) * 128], win[:, c, 1536 + c0:1536 + c0 + cw])
                                   for c in range(NCH)], ["win"] + h_rd, [kpr])
                k.copy(vb[:, c0:c0 + cw], pr[:, 0:cw], [kpr], [kvb], eng="act" if bank == 4 else "dve")
            k.dma("sp", vscr.rearrange("h p t e -> p h t e")[:, :, g * 4 + i, :],
                  vb[:, :].rearrange("p (h e) -> p h e", e=128), [kvb], [("vscr", g * 4 + i)], "vscr")
        if own is None:
            continue
        for h in range(6):
            pr = k.ps[2 + cnt % 2]
            kpr = "ps%d" % (2 + cnt % 2)
            cnt += 1
            k.mm(pr[:, :], [(win[:, c, h * 128:(h + 1) * 128], hT[:, c, :]) for c in range(NCH)], ["win"] + h_rd, [kpr])
            headnorm(k, pr[:, :], 128, TT, gq8, k.bd_b, QT[:, h, dsl], [kpr], [("QT", h, own)], tmp_next())
        for hm in range(4):
            pr = k.ps[2 + cnt % 2]
            kpr = "ps%d" % (2 + cnt % 2)
            cnt += 1
            k.mm(pr[0:64, :], [(win[:, c, 2304 + hm * 64:2304 + (hm + 1) * 64], hT[:, c, :]) for c in range(NCH)],
                 ["win"] + h_rd, [kpr])
            headnorm(k, pr[0:64, :], 64, TT, gmq8[0:64, :], k.ones_b[0:64, 0:64], mqT[0:64, hm, dsl], [kpr],
                     [("mqT", hm, own)], tmp_next())
    m.release(mk)


def attn_core(k, QT, tokT, kscr, vscr, neglam, subgs, flagb, nprev):
    m = k.m
    mk = m.mark()
    nctx = nprev + NTT
    nkt = nctx * 4
    kbuf = [m.alloc("kbuf", [128, nctx * TT], BF16) for _ in range(2)]
    vbuf = [m.alloc("vbuf", [128, nkt, 128], BF16) for _ in range(2)]
    pb = [m.alloc("pp", [128, 2, TT], BF16) for _ in range(2)]
    acc = m.alloc("acc", [128, 2, TT], F32)
    rs1 = m.alloc("rs1", [128, TT], F32)
    rs2 = m.alloc("rs2", [128, TT], F32)
    t1 = m.alloc("t1", [128, TT], F32)
    t2 = m.alloc("t2", [128, TT], F32)
    sqb = m.alloc("asq", [128, TT], BF16)
    rsn = m.alloc("rsn", [128, TT], F32)
    steps = []
    for h in range(6):
        for qt in range(NTT):
            kts = [(g, 0, True) for g in range(nprev * 4)] + [(nprev * 4 + j, 0, False) for j in range(qt * 4)] + \
                  [(nprev * 4 + qt * 4 + j, 128 * j, False) for j in range(4)]
            for idx, (g, n0, isprev) in enumerate(kts):
                steps.append((h, qt, idx, len(kts), g, n0, isprev))
    loaded = set()

    def load_head(h):
        if h in loaded or h >= 6:
            return
        loaded.add(h)
        kb, vb = kbuf[h % 2], vbuf[h % 2]
        kkb, kvb = "kbuf%d" % (h % 2), "vbuf%d" % (h % 2)
        k.dma("sp", kb[:, :], kscr[h, :, 0:nctx * TT], [("kscr", h, g) for g in range(nctx)], [kkb], kkb)
        k.dma("sp", vb[:, :, :], vscr[h, :, 0:nkt, :], [("vscr", g) for g in range(nkt)], [kvb], kvb)

    def S(i):
        h, qt, idx, nk, g, n0, isprev = steps[i]
        load_head(h)
        b = i % 2
        kb = kbuf[h % 2]
        kkb = "kbuf%d" % (h % 2)
        k.mm1(k.ps[2 * b][:, n0:TT], kb[0:64, g * 128:(g + 1) * 128], QT[0:64, h, qt * TT + n0:(qt + 1) * TT], True, True,
              [kkb, ("QT", h, qt)], ["ps%d" % (2 * b)])
        k.mm1(k.ps[2 * b + 1][:, n0:TT], kb[64:128, g * 128:(g + 1) * 128], QT[64:128, h, qt * TT + n0:(qt + 1) * TT], True, True,
              [kkb, ("QT", h, qt)], ["ps%d" % (2 * b + 1)])

    load_head(0)
    S(0)
    for i in range(len(steps)):
        h, qt, idx, nk, g, n0, isprev = steps[i]
        if i + 1 < len(steps):
            S(i + 1)
        if idx == 0 and qt == 0:
            load_head(h + 1)
        b = i % 2
        vb = vbuf[h % 2]
        kvb = "vbuf%d" % (h % 2)
        P = pb[b]
        kp = "pp%d" % b
        sview = k.psall[:, 2 * b * 512:(2 * b + 2) * 512].rearrange("p (a c) -> p a c", c=512)
        diag = n0 > 0 or (g >= nprev * 4 + qt * 4)
        bias = flagb if isprev else None
        rdb = ["gains"] if isprev else []
        k.act(P[:, :, n0:TT], sview[:, :, n0:TT], AF.Exp, ["ps%d" % (2 * b), "ps%d" % (2 * b + 1)] + rdb, [kp],
              bias=bias, scale=0.125)
        if diag:
            k.memset(P[64:128, :, n0:n0 + 64], 0.0, [kp], eng="pool")
        if idx == 0:
            k.copy(acc[:, 0, :], P[:, 0, :], [kp], ["acc0"], eng="dve")
            k.copy(acc[:, 1, :], P[:, 1, :], [kp], ["acc1"], eng="pool")
        else:
            k.tt(acc[:, 0, n0:TT], P[:, 0, n0:TT], acc[:, 0, n0:TT], ALU.add, [kp, "acc0"], ["acc0"], eng="dve")
            k.tt(acc[:, 1, n0:TT], P[:, 1, n0:TT], acc[:, 1, n0:TT], ALU.add, [kp, "acc1"], ["acc1"], eng="pool")
        k.mm1(k.ps[4][:, n0:TT], vb[:, g, :], P[:, 0, n0:TT], idx == 0, idx == nk - 1, [kvb, kp], ["ps4"])
        k.mm1(k.ps[5][:, n0:TT], vb[:, g, :], P[:, 1, n0:TT], idx == 0, idx == nk - 1, [kvb, kp], ["ps5"])
        if idx != nk - 1:
            continue
        qsl = slice(qt * TT, (qt + 1) * TT)
        k.mm(k.ps[6][:, :], [(k.ones_f, acc[:, 0, :])], ["acc0", "cf32"], ["ps6"])
        k.mm(k.ps[7][:, :], [(k.ones_f, acc[:, 1, :])], ["acc1", "cf32"], ["ps7"])
        k.s.add("dve", lambda e, o=rs1[:, :], i_=k.ps[6][:, :]: e.reciprocal(o, i_), ["ps6"], ["rs1"])
        k.s.add("dve", lambda e, o=rs2[:, :], i_=k.ps[7][:, :]: e.reciprocal(o, i_), ["ps7"], ["rs2"])
        k.tt(t1[:, :], k.ps[4][:, :], rs1[:, :], ALU.mult, ["ps4", "rs1"], ["t1"])
        k.tt(t2[:, :], k.ps[5][:, :], rs2[:, :], ALU.mult, ["ps5", "rs2"], ["t2"])
        k.stt(t1[:, :], t2[:, :], neglam, t1[:, :], ALU.mult, ALU.add, ["t1", "t2", "gains"], ["t1"])
        k.act(sqb[:, :], t1[:, :], AF.Square, ["t1"], ["asq"])
        k.mm(k.ps[6][:, :], [(k.ones_b, sqb[:, :])], ["asq", "cbf"], ["ps6"])
        k.act(rsn[:, :], k.ps[6][:, :], AF.Ln, ["ps6", "epsc"], ["rsn"], bias=k.eps_ap(128 * EPS))
        k.act(rsn[:, :], rsn[:, :], AF.Exp, ["rsn"], ["rsn"], scale=-0.5)
        k.stt(tokT[:, h, qsl], t1[:, :], subgs, rsn[:, :], ALU.mult, ALU.mult, ["t1", "rsn", "gains"],
              [("tokT", h, qt), ("QT", h, qt)])
    m.release(mk)


def mem_attn(k, mqT, memT, KmT, Vm, lname):
    m = k.m
    mk = m.mark()
    p1 = [m.alloc("pm", [128, TT], BF16) for _ in range(2)]
    rs1 = m.alloc("rsm", [128, TT], F32)
    it = 0
    for qt in range(NTT):
        qsl = slice(qt * TT, (qt + 1) * TT)
        for hm in range(4):
            for mt in range(2):
                b = it % 2
                it += 1
                s1 = k.ps[b]
                ks1 = "ps%d" % b
                P1 = p1[b]
                kp1 = "pm_%d" % b
                k.mm1(s1[:, :], KmT[0:64, hm, mt * 128:(mt + 1) * 128], mqT[0:64, hm, qsl], True, True,
                      [("KmT" + lname, hm), ("mqT", hm, qt)], [ks1])
                k.act(P1[:, :], s1[:, :], AF.Exp, [ks1], [kp1], scale=0.125)
                k.mm1(k.ps[4][0:64, :], Vm[:, mt, hm * 64:(hm + 1) * 64], P1[:, :], mt == 0, mt == 1,
                      [("Vm" + lname, mt), kp1], ["ps4"])
                k.mm1(k.ps[5][0:64, :], k.ones_b[:, 0:64], P1[:, :], mt == 0, mt == 1, ["cbf", kp1], ["ps5"])
            k.s.add("dve", lambda e, o=rs1[0:64, :], i=k.ps[5][0:64, :]: e.reciprocal(o, i), ["ps5"], ["rsm"])
            k.tt(memT[0:64, hm, qsl], k.ps[4][0:64, :], rs1[0:64, :], ALU.mult, ["ps4", "rsm"],
                 [("memT", hm, qt), ("mqT", hm, qt)])
    m.release(mk)


def attn_outproj(k, xT, xname, tokT, tokname, nk, memT, wo_d, lname):
    m = k.m
    mk = m.mark()
    wo = m.alloc("wo", [128, 6, D], BF16)
    wom = m.alloc("wom", [64, 4, D], BF16)
    k.dma("pool", wo[:], wo_d[0:768, :].rearrange("(c p) d -> p c d", p=128), (), ["wo"], "wo")
    k.dma("pool", wom[:], wo_d[768:1024, :].rearrange("(h p) d -> p h d", p=64), (), ["wom"], "wom")
    cnt = 0
    for qt in range(NTT):
        qsl = slice(qt * TT, (qt + 1) * TT)
        rd = [(tokname, kc, qt) for kc in range(nk)] + [("memT", hm, qt) for hm in range(4)]
        for ds in range(NCH):
            pb = k.ps[cnt % 2]
            kpb = "ps%d" % (cnt % 2)
            cnt += 1
            pairs = [(wo[:, kc, ds * 128:(ds + 1) * 128], tokT[:, kc, qsl]) for kc in range(nk)] + \
                    [(wom[0:64, hm, ds * 128:(ds + 1) * 128], memT[0:64, hm, qsl]) for hm in range(4)]
            k.mm(pb[:, :], pairs, ["wo", "wom"] + rd, [kpb])
            k.tt(xT[:, ds, qsl], pb[:, :], xT[:, ds, qsl], ALU.add, [kpb] + k.xtok(xname, qt, [ds]), k.xtok(xname, qt, [ds]))
    m.release(mk)


def attn_gains(k, par_d):
    m = k.m
    par = m.alloc("par", [128, PA_W], F32)
    k.dma("sp", par[:], par_d[:, :], (), ["parraw"], "par")
    gn = m.alloc("gains", [128, 32], F32)
    k.ts(gn[:, 0:16], par[:, 0:16], 32.0, None, ALU.mult, None, ["parraw"], ["par"])
    k.ts(gn[:, 16:20], par[:, 16:20], 8.0, None, ALU.mult, None, ["parraw"], ["gains"])
    lam_init = 0.8 - 0.6 * math.exp(-0.3 * 0)
    k.ts(gn[:, 20:21], par[:, 20:21], float(math.sqrt(128.0) * (1.0 - lam_init)), None, ALU.mult, None, ["parraw"], ["g20"])
    k.copy(gn[:, 21:23], par[:, 21:23], ["parraw"], ["g21"])
    pr = m.alloc("lprod", [128, 2, 64], F32)
    k.tt(pr[:, 0, :], par[:, 24:88], par[:, 88:152], ALU.mult, ["parraw"], ["lprod0"])
    k.tt(pr[:, 1, :], par[:, 152:216], par[:, 216:280], ALU.mult, ["parraw"], ["lprod1"])
    k.s.add("dve", lambda e: e.tensor_reduce(gn[:, 24:26], pr[:, :, :], AX.X, ALU.add), ["lprod0", "lprod1"], ["g24"])
    k.act(gn[:, 24:26], gn[:, 24:26], AF.Exp, ["g24"], ["g24"])
    k.tt(gn[:, 26:27], gn[:, 25:26], gn[:, 24:25], ALU.subtract, ["g24"], ["g26"])
    k.ts(gn[:, 27:28], gn[:, 26:27], float(-lam_init), None, ALU.add, None, ["g26"], ["g27"])
    k.copy(gn[:, 28:29], gn[:, 27:28], ["g27", "g20", "g21", "par", "gains"], ["gains"])
    return dict(g32=gn[:, 0:8], gmem32=gn[:, 8:16], gq8=gn[:, 16:17], gk8=gn[:, 17:18], gmq8=gn[:, 18:19],
                gmk8=gn[:, 19:20], subgs=gn[:, 20:21], flag=gn[:, 21:22], flagb=gn[:, 22:23], neglam=gn[:, 27:28])


def build_attn0():
    nc = bass.Bass("TRN2", target_bir_lowering=False)
    xo = nc.dram_tensor("xo", [SEQH, D], F32, kind="ExternalInput").ap()
    xp = nc.dram_tensor("xp", [SEQH, D], F32, kind="ExternalInput").ap()
    mem = nc.dram_tensor("mem", [MEM_LEN, D], F32, kind="ExternalInput").ap()
    cst = nc.dram_tensor("cst", [128, CONST_W], F32, kind="ExternalInput").ap()
    par = nc.dram_tensor("par", [128, PA_W], F32, kind="ExternalInput").ap()
    win_d = nc.dram_tensor("win", [D, 2560], F32, kind="ExternalInput").ap()
    wkv_d = nc.dram_tensor("wkv", [D, 512], F32, kind="ExternalInput").ap()
    wo_d = nc.dram_tensor("wo", [D, D], F32, kind="ExternalInput").ap()
    y = nc.dram_tensor("y", [SEQH, D], F32, kind="ExternalOutput").ap()
    kscr = nc.dram_tensor("kscr", [6, 128, 2 * SEQH], BF16).ap()
    vscr = nc.dram_tensor("vscr", [6, 128, 32, 128], BF16).ap()
    k = K(nc)
    with k.st:
        m = k.m
        k.setup_consts(cst)
        gd = attn_gains(k, par)
        xT = m.alloc("xT", [128, NCH, SEQH], F32)
        KmT = m.alloc("KmT", [64, 4, MEM_LEN], BF16)
        Vm = m.alloc("Vm", [128, 2, 256], BF16)
        mem_kv_setup(k, mem, wkv_d, gd["gmem32"], gd["gmk8"], KmT, Vm, "0")
        QT = m.alloc("QT", [128, 6, SEQH], BF16)
        mqT = m.alloc("mqT", [64, 4, SEQH], BF16)
        mk1 = m.mark()
        win = m.alloc("win", [128, NCH, 2560], BF16)
        winv = win_d.rearrange("(c p) f -> p c f", p=128)
        for j in range(5):
            k.dma("pool", win[:, :, j * 512:(j + 1) * 512], winv[:, :, j * 512:(j + 1) * 512], (), ["win"], "win")
        tiles = [(xp[i * TT:(i + 1) * TT, :], None) for i in range(NTT)] + [(xo[i * TT:(i + 1) * TT, :], i) for i in range(NTT)]
        attn_inproj(k, tiles, xT, gd["g32"], win, gd["gq8"], gd["gk8"], gd["gmq8"], QT, mqT, kscr, vscr, 0)
        m.release(mk1)
        tokT = m.alloc("tokT", [128, 6, SEQH], BF16)
        memT = m.alloc("memTo", [64, 4, SEQH], BF16)
        attn_core(k, QT, tokT, kscr, vscr, gd["neglam"], gd["subgs"], gd["flagb"], NTT)
        mem_attn(k, mqT, memT, KmT, Vm, "0")
        attn_outproj(k, xT, "xT", tokT, "tokT", 6, memT, wo_d, "0")
        k.store_xT(xT, y, "xT")
        k.s.emit()
    return nc


def run_attn0(inp, xo_shards, xp_shards, flags, mems, trace=False):
    if "attn0" not in _cache:
        _cache["attn0"] = build_attn0()
    nc = _cache["attn0"]
    cst = make_consts()
    in_maps = []
    for i in range(8):
        in_maps.append({"xo": np.ascontiguousarray(xo_shards[i]), "xp": np.ascontiguousarray(xp_shards[i]),
                        "mem": np.ascontiguousarray(mems[i]), "cst": cst, "par": pack_par_attn(inp, 0, flags[i]),
                        "win": np.asarray(inp["da_w_in"][0]), "wkv": np.asarray(inp["mem_w_kv"][0]),
                        "wo": np.asarray(inp["w_out"][0])})
    res = run_bass_kernel_spmd(nc, in_maps, core_ids=list(range(8)), trace=trace)
    return [r["y"] for r in res.results], res


PC_W = 880
TC = 256
NTC = SEQH // TC
SSM_IN = 2316


def pack_par_ssd(inp, flag):
    p = np.zeros((128, PC_W), np.float32)
    p[:, 0:8] = pack_pp(inp["ln1_g"][1])
    p[:, 8:16] = pack_pp(inp["mem_norm_g"])
    p[:, 16] = np.tile(np.asarray(inp["mem_qn_g"][1], np.float32), 2)
    p[:, 17] = np.tile(np.asarray(inp["mem_kn_g"][1], np.float32), 2)
    p[:, 18] = flag
    cw = np.asarray(inp["ssm_conv_w"][0], np.float32)
    p[:, 20:60] = cw.reshape(4, 10, 128).transpose(2, 1, 0).reshape(128, 40)
    p[:, 60:70] = np.asarray(inp["ssm_conv_b"][0], np.float32).reshape(10, 128).T
    p[:, 70:82] = np.asarray(inp["ssm_dt_bias"][0], np.float32)[None, :]
    p[:, 82:94] = np.asarray(inp["ssm_a_log"][0], np.float32)[None, :]
    p[:, 94:106] = np.asarray(inp["ssm_d"][0], np.float32)[None, :]
    p[:, 106:874] = np.asarray(inp["ssm_norm_g"][0], np.float32)[None, :]
    return p


def ssd_gains(k, par_d):
    m = k.m
    par = m.alloc("parc", [128, PC_W], F32)
    k.dma("sp", par[:], par_d[:, :], (), ["parraw"], "par")
    gn = m.alloc("gainc", [128, 48], F32)
    k.ts(gn[:, 0:16], par[:, 0:16], 32.0, None, ALU.mult, None, ["parraw"], ["par"])
    k.ts(gn[:, 16:18], par[:, 16:18], 8.0, None, ALU.mult, None, ["parraw"], ["gains"])
    k.act(gn[:, 20:32], par[:, 82:94], AF.Exp, ["parraw"], ["g20"])
    k.ts(gn[:, 20:32], gn[:, 20:32], -1.0, None, ALU.mult, None, ["g20"], ["g20"])
    k.copy(gn[:, 32:33], par[:, 18:19], ["parraw", "g20", "par", "gains"], ["gains"])
    return dict(g32=gn[:, 0:8], gmem32=gn[:, 8:16], gmq8=gn[:, 16:17], gmk8=gn[:, 17:18], a_bc=gn[:, 20:32],
                flag=gn[:, 32:33], cw=par[:, 20:60], cb=par[:, 60:70], dtb=par[:, 70:82], dsk=par[:, 94:106],
                ng=par[:, 106:874])


def ssd_pass(k, xT, xname, gd, win, own, H, Hbf, halo, tokT, mqT):
    m = k.m
    mk = m.mark()
    h_off = m.mark()
    dabc = m.alloc("dabc", [128, 12, 128], F32)
    hT = m.overlay("hT1", [128, NCH, TC], BF16, h_off)
    sq_off = m.mark()
    expE = m.alloc("expE", [128, 12, 128], F32)
    sq = m.overlay("sq1", [128, NCH, TC], BF16, sq_off)
    ytmp = m.overlay("ytmp", [128, TOK_W], F32, sq_off)
    rstd = m.alloc("rstd1", [128, TC], F32)
    raw = [m.alloc("raw", [128, TC + 3], F32) for _ in range(2)]
    cacc = [m.alloc("cacc", [128, TC], F32) for _ in range(2)]
    xbcT = m.alloc("xbcT", [128, 10, TC], BF16)
    dtt = m.alloc("dtt", [128, 2, 12], F32)
    dAt = m.alloc("dAt", [128, 2, 12], F32)
    xB = m.alloc("xB", [128, 1024], BF16)
    cs = m.alloc("cs", [128, 24], F32)
    ed = m.alloc("ed", [128, 24], F32)
    wend = m.alloc("wend", [128, 12], F32)
    xend = m.alloc("xend", [128, TOK_W], BF16)
    tmp = dict(sq=m.alloc("hsq", [128, 384], BF16), ksq="hsq", rs=m.alloc("hrs", [128, 384], F32), krs="hrs",
               pb=k.ps[7], kpb="ps7")
    if own:
        zs = m.alloc("zs", [128, 2, TOK_W], BF16)
        cb = m.alloc("cb", [128, 2, 128], F32)
        Wt = m.alloc("Wt", [128, 12, 128], BF16)
        xdt = m.alloc("xdt", [128, TOK_W], BF16)
        xD = m.alloc("xD", [128, TOK_W], BF16)
        yn = m.alloc("yn", [128, TOK_W], BF16)
        ss = m.alloc("ss", [128, 4], F32)
        junk = tmp["rs"]
    cw, cbias = gd["cw"], gd["cb"]
    ps = k.ps
    rc = 0
    for ti in range(NTC):
        tsl = slice(ti * TC, (ti + 1) * TC)
        xrd = [(xname, c, ti * 2 + j) for c in range(NCH) for j in range(2)]
        k.act(sq[:, :, :], xT[:, :, tsl], AF.Square, xrd, ["sq1", "ytmp"] + [("expE", q) for q in range(3)])
        k.mm(ps[6][:, 0:TC], [(k.ones_b, sq[:, c, :]) for c in range(NCH)], ["sq1", "cbf"], ["ps6"])
        k.act(rstd[:, :], ps[6][:, 0:TC], AF.Ln, ["ps6", "epsc"], ["rstd1"], bias=k.eps_ap(D * EPS))
        k.act(rstd[:, :], rstd[:, :], AF.Exp, ["rstd1"], ["rstd1"], scale=-0.5)
        for c in range(NCH):
            k.stt(hT[:, c, :], xT[:, c, tsl], gd["g32"][:, c:c + 1], rstd[:, :], ALU.mult, ALU.mult,
                  [(xname, c, ti * 2), (xname, c, ti * 2 + 1), "rstd1", "par"], [("hT1", c), "dabc"])
        h_rd = [("hT1", c) for c in range(NCH)]
        for cc in range(10):
            b = rc % 2
            rc += 1
            pr, kpr = ps[b], "ps%d" % b
            rw, krw = raw[b], "raw%d" % b
            ca, kca = cacc[b], "cacc%d" % b
            k.mm(pr[:, 0:TC], [(win[:, c, 768 + cc * 128:768 + (cc + 1) * 128], hT[:, c, :]) for c in range(NCH)],
                 ["win1"] + h_rd, [kpr])
            k.copy(rw[:, 3:TC + 3], pr[:, 0:TC], [kpr], [krw], eng="act")
            k.copy(rw[:, 0:3], halo[:, cc, :], [("halo", cc)], [krw], eng="pool")
            k.copy(halo[:, cc, :], rw[:, TC:TC + 3], [krw], [("halo", cc)], eng="pool")
            k.ts(ca[:, :], rw[:, 0:TC], cw[:, cc * 4:cc * 4 + 1], cbias[:, cc:cc + 1], ALU.mult, ALU.add,
                 [krw, "parraw"], [kca])
            for j in range(1, 4):
                k.stt(ca[:, :], rw[:, j:j + TC], cw[:, cc * 4 + j:cc * 4 + j + 1], ca[:, :], ALU.mult, ALU.add,
                      [krw, kca, "parraw"], [kca])
            k.act(xbcT[:, cc, :], ca[:, :], AF.Silu, [kca], [("xbcT", cc)])
        for j in range(2):
            k.mm(ps[2][:, j * 12:(j + 1) * 12], [(hT[:, c, j * 128:(j + 1) * 128], win[:, c, 2048:2060]) for c in range(NCH)],
                 ["win1"] + h_rd, ["ps2"])
        k.tt(dtt[:, :, :], ps[2][:, 0:24].rearrange("p (a b) -> p a b", b=12),
             gd["dtb"].unsqueeze(1).broadcast_to([128, 2, 12]), ALU.add, ["ps2", "parraw"], ["dtt"])
        k.act(dtt[:, :, :], dtt[:, :, :], AF.Exp, ["dtt"], ["dtt"])
        k.act(dtt[:, :, :], dtt[:, :, :], AF.Ln, ["dtt", "epsc"], ["dtt"], bias=k.eps_ap(1.0))
        k.tt(dAt[:, :, :], dtt[:, :, :], gd["a_bc"].unsqueeze(1).broadcast_to([128, 2, 12]), ALU.mult, ["dtt", "gains"], ["dAt"])
        if own:
            for j in range(2):
                for (c0, cwid, bank) in ((0, 512, 3), (512, 256, 4)):
                    k.mm(ps[bank][:, 0:cwid], [(hT[:, c, j * 128:(j + 1) * 128], win[:, c, c0:c0 + cwid]) for c in range(NCH)],
                         ["win1"] + h_rd, ["ps%d" % bank])
                    k.act(zs[:, j, c0:c0 + cwid], ps[bank][:, 0:cwid], AF.Silu, ["ps%d" % bank], [("zs", j)])
            for hm in range(4):
                k.mm(ps[5][0:64, 0:TC], [(win[:, c, 2060 + hm * 64:2060 + (hm + 1) * 64], hT[:, c, :]) for c in range(NCH)],
                     ["win1"] + h_rd, ["ps5"])
                headnorm(k, ps[5][0:64, 0:TC], 64, TC, gd["gmq8"][0:64, :], k.ones_b[0:64, 0:64], mqT[0:64, hm, tsl],
                         ["ps5"], [("mqT", hm, ti // 2)], tmp)
        for w in range(2):
            wsl = slice(w * 128, (w + 1) * 128)
            gw = ti * 2 + w
            pbf = ps[0].bitcast(BF16)
            for cc in range(8):
                k.tr(pbf[:, cc * 128:(cc + 1) * 128], xbcT[:, cc, wsl], k.ident_b, [("xbcT", cc), "cbf"], ["ps0"])
            k.copy(xB[:, :], pbf[:, 0:1024], ["ps0"], ["xB"], eng="act")
            k.mm(ps[1][:, 0:12], [(k.utri, dAt[:, w, :])], ["dAt", "cf32"], ["ps1"])
            k.mm(ps[1][:, 12:24], [(k.ones_f, dAt[:, w, :])], ["dAt", "cf32"], ["ps1"])
            k.copy(cs[:, :], ps[1][:, 0:24], ["ps1"], ["cs"], eng="dve")
            k.act(ed[:, :], ps[1][:, 0:24], AF.Exp, ["ps1"], ["ed"])
            k.tt(wend[:, :], cs[:, 12:24], cs[:, 0:12], ALU.subtract, ["cs"], ["wend"])
            k.act(wend[:, :], wend[:, :], AF.Exp, ["wend"], ["wend"])
            k.tt(wend[:, :], wend[:, :], dtt[:, w, :], ALU.mult, ["wend", "dtt"], ["wend"])
            x3 = xB[:, 0:TOK_W].rearrange("p (h d) -> p h d", d=64)
            k.tt(xend[:, :].rearrange("p (h d) -> p h d", d=64), x3, wend[:, :].unsqueeze(2).broadcast_to([128, 12, 64]),
                 ALU.mult, ["xB", "wend"], ["xend"])
            if own:
                k.tt(xdt[:, :].rearrange("p (h d) -> p h d", d=64), x3, dtt[:, w, :].unsqueeze(2).broadcast_to([128, 12, 64]),
                     ALU.mult, ["xB", "dtt"], ["xdt"], eng="pool")
                k.tt(xD[:, :].rearrange("p (h d) -> p h d", d=64), x3, gd["dsk"].unsqueeze(2).broadcast_to([128, 12, 64]),
                     ALU.mult, ["xB", "parraw"], ["xD"], eng="pool")
                k.copy(dabc[:, :, :], dAt[:, w, :].unsqueeze(2).broadcast_to([128, 12, 128]), ["dAt"], ["dabc"] + [("hT1", c) for c in range(NCH)], eng="pool")
                for h in range(12):
                    bank = 2 + h // 4
                    o = ps[bank][:, (h % 4) * 128:(h % 4 + 1) * 128]
                    kb = "ps%d" % bank
                    k.mm1(o, dabc[:, h, :], k.utri, True, False, ["dabc", "cf32"], [kb])
                    k.mm1(o, k.negutri, dabc[:, h, :], False, False, ["dabc", "cf32"], [kb])
                    k.mm1(o, k.ident, k.maskneg, False, True, ["cf32"], [kb])
                for q in range(3):
                    k.act(expE[:, q * 4:(q + 1) * 4, :].rearrange("p a b -> p (a b)"), ps[2 + q][:, :], AF.Exp,
                          ["ps%d" % (2 + q), "sq1", "ytmp"], [("expE", q)])
                for g in range(2):
                    k.mm1(ps[1][:, 256 + g * 128:256 + (g + 1) * 128], xbcT[:, 6 + g, wsl], xbcT[:, 8 + g, wsl], True, True,
                          [("xbcT", 6 + g), ("xbcT", 8 + g)], ["ps1"])
                k.copy(cb[:, :, :].rearrange("p a b -> p (a b)"), ps[1][:, 256:512], ["ps1"], ["cb"], eng="dve")
                for g in range(2):
                    k.tt(Wt[:, 6 * g:6 * g + 6, :], expE[:, 6 * g:6 * g + 6, :], cb[:, g, :].unsqueeze(1).broadcast_to([128, 6, 128]),
                         ALU.mult, [("expE", q) for q in range(3)] + ["cb"], [("Wt", g)])
                k.mm1(ps[2][:, 0:512], k.ident_b, xD[:, 0:512], True, False, ["xD", "cbf"], ["ps2"])
                for h in range(8):
                    k.mm1(ps[2][:, h * 64:(h + 1) * 64], Wt[:, h, :], xdt[:, h * 64:(h + 1) * 64], False, h == 7,
                          [("Wt", h // 6), "xdt"], ["ps2"])
                k.mm1(ps[3][:, 0:256], k.ident_b, xD[:, 512:768], True, False, ["xD", "cbf"], ["ps3"])
                for h in range(8, 12):
                    k.mm1(ps[3][:, (h - 8) * 64:(h - 7) * 64], Wt[:, h, :], xdt[:, h * 64:(h + 1) * 64], False, h == 11,
                          [("Wt", h // 6), "xdt"], ["ps3"])
                for g in range(2):
                    k.mm1(ps[5 + g][:, 0:384], xbcT[:, 8 + g, wsl], Hbf[:, g * 384:(g + 1) * 384], True, True,
                          [("xbcT", 8 + g), "Hbf"], ["ps%d" % (5 + g)])
                for g in range(2):
                    k.tt(ytmp[:, g * 384:(g + 1) * 384].rearrange("p (h d) -> p h d", d=64),
                         ps[5 + g][:, 0:384].rearrange("p (h d) -> p h d", d=64),
                         ed[:, 6 * g:6 * g + 6].unsqueeze(2).broadcast_to([128, 6, 64]), ALU.mult, ["ps%d" % (5 + g), "ed"], ["ytmp", "sq1"] + [("expE", q) for q in range(3)])
                k.tt(ytmp[:, 0:512], ps[2][:, 0:512], ytmp[:, 0:512], ALU.add, ["ps2", "ytmp"], ["ytmp"])
                k.tt(ytmp[:, 512:768], ps[3][:, 0:256], ytmp[:, 512:768], ALU.add, ["ps3", "ytmp"], ["ytmp"])
                k.tt(ytmp[:, :], ytmp[:, :], zs[:, w, :], ALU.mult, ["ytmp", ("zs", w)], ["ytmp"])
                for g in range(2):
                    k.act(junk[:, :], ytmp[:, g * 384:(g + 1) * 384], AF.Square, ["ytmp"], ["hrs", ("ss", g)],
                          accum_out=ss[:, g:g + 1])
                k.act(ss[:, 2:4], ss[:, 0:2], AF.Ln, [("ss", 0), ("ss", 1), "epsc"], ["ss2"], bias=k.eps_ap(EPS), scale=1.0 / 384.0)
                k.act(ss[:, 2:4], ss[:, 2:4], AF.Exp, ["ss2"], ["ss2"], scale=-0.5)
                for g in range(2):
                    k.stt(yn[:, g * 384:(g + 1) * 384], ytmp[:, g * 384:(g + 1) * 384], ss[:, 2 + g:3 + g],
                          gd["ng"][:, g * 384:(g + 1) * 384], ALU.mult, ALU.mult, ["ytmp", "ss2", "parraw"], [("yn", g)])
                pbf4 = ps[4].bitcast(BF16)
                for kc in range(6):
                    k.tr(pbf4[:, kc * 128:(kc + 1) * 128], yn[:, kc * 128:(kc + 1) * 128], k.ident_b,
                         [("yn", kc // 3), "cbf"], ["ps4"])
                k.copy(tokT[:, :, gw * 128:(gw + 1) * 128], pbf4[:, 0:768].rearrange("p (a b) -> p a b", b=128), ["ps4"],
                       [("tokT1", kc, gw // 4) for kc in range(6)], eng="act")
            for g in range(2):
                k.mm1(ps[7 - g][:, 0:384], xB[:, 768 + g * 128:768 + (g + 1) * 128], xend[:, g * 384:(g + 1) * 384], True, True,
                      ["xB", "xend"], ["ps%d" % (7 - g)])
            for g in range(2):
                Hg = H[:, g * 384:(g + 1) * 384].rearrange("p (h d) -> p h d", d=64)
                k.tt(Hg, Hg, ed[:, 12 + 6 * g:12 + 6 * g + 6].unsqueeze(2).broadcast_to([128, 6, 64]), ALU.mult,
                     ["H", "ed", "Hbf"], ["H"])
                k.tt(H[:, g * 384:(g + 1) * 384], ps[7 - g][:, 0:384], H[:, g * 384:(g + 1) * 384], ALU.add,
                     ["ps%d" % (7 - g), "H"], ["H"])
            k.copy(Hbf[:, :], H[:, :], ["H"], ["Hbf"], eng="pool")
    m.release(mk)


def build_ssd():
    nc = bass.Bass("TRN2", target_bir_lowering=False)
    xo = nc.dram_tensor("xo", [SEQH, D], F32, kind="ExternalInput").ap()
    xp = nc.dram_tensor("xp", [SEQH, D], F32, kind="ExternalInput").ap()
    mem = nc.dram_tensor("mem", [MEM_LEN, D], F32, kind="ExternalInput").ap()
    cst = nc.dram_tensor("cst", [128, CONST_W], F32, kind="ExternalInput").ap()
    par = nc.dram_tensor("par", [128, PC_W], F32, kind="ExternalInput").ap()
    win_d = nc.dram_tensor("win", [D, SSM_IN], F32, kind="ExternalInput").ap()
    wkv_d = nc.dram_tensor("wkv", [D, 512], F32, kind="ExternalInput").ap()
    wo_d = nc.dram_tensor("wo", [D, D], F32, kind="ExternalInput").ap()
    y = nc.dram_tensor("y", [SEQH, D], F32, kind="ExternalOutput").ap()
    k = K(nc)
    with k.st:
        m = k.m
        k.setup_consts(cst)
        gd = ssd_gains(k, par)
        xT = m.alloc("xT", [128, NCH, SEQH], F32)
        KmT = m.alloc("KmT", [64, 4, MEM_LEN], BF16)
        Vm = m.alloc("Vm", [128, 2, 256], BF16)
        mem_kv_setup(k, mem, wkv_d, gd["gmem32"], gd["gmk8"], KmT, Vm, "1")
        H = m.alloc("H", [128, TOK_W], F32)
        Hbf = m.alloc("Hbf", [128, TOK_W], BF16)
        halo = m.alloc("halo", [128, 10, 3], F32)
        k.memset(H[:, :], 0.0, ["H"], eng="dve")
        k.memset(Hbf[:, :], 0.0, ["Hbf"], eng="pool")
        k.memset(halo[:, :, :], 0.0, [("halo", cc) for cc in range(10)], eng="pool")
        mqT = m.alloc("mqT", [64, 4, SEQH], BF16)
        tokT = m.alloc("tokT1", [128, 6, SEQH], BF16)
        mk1 = m.mark()
        win = m.alloc("win1", [128, NCH, SSM_IN], BF16)
        winv = win_d.rearrange("(c p) f -> p c f", p=128)
        for (a, b) in ((0, 512), (512, 1024), (1024, 1536), (1536, 2048), (2048, SSM_IN)):
            k.dma("pool", win[:, :, a:b], winv[:, :, a:b], (), ["win1"], "win1")
        k.load_xT(xp, xT, "xT")
        ssd_pass(k, xT, "xT", gd, win, False, H, Hbf, halo, None, None)
        k.ts(H[:, :], H[:, :], gd["flag"], None, ALU.mult, None, ["H", "gains"], ["H"])
        k.copy(Hbf[:, :], H[:, :], ["H"], ["Hbf"], eng="pool")
        for cc in range(10):
            k.ts(halo[:, cc, :], halo[:, cc, :], gd["flag"], None, ALU.mult, None, [("halo", cc), "gains"], [("halo", cc)])
        k.s.barrier()
        k.load_xT(xo, xT, "xT")
        ssd_pass(k, xT, "xT", gd, win, True, H, Hbf, halo, tokT, mqT)
        m.release(mk1)
        memT = m.alloc("memTo", [64, 4, SEQH], BF16)
        mem_attn(k, mqT, memT, KmT, Vm, "1")
        attn_outproj(k, xT, "xT", tokT, "tokT1", 6, memT, wo_d, "1")
        k.store_xT(xT, y, "xT")
        k.s.emit()
    return nc


def run_ssd(inp, xo_shards, xp_shards, flags, mems, trace=False):
    if "ssd" not in _cache:
        _cache["ssd"] = build_ssd()
    nc = _cache["ssd"]
    cst = make_consts()
    in_maps = []
    for i in range(8):
        in_maps.append({"xo": np.ascontiguousarray(xo_shards[i]), "xp": np.ascontiguousarray(xp_shards[i]),
                        "mem": np.ascontiguousarray(mems[i]), "cst": cst, "par": pack_par_ssd(inp, flags[i]),
                        "win": np.asarray(inp["ssm_w_in"][0]), "wkv": np.asarray(inp["mem_w_kv"][1]),
                        "wo": np.asarray(inp["w_out"][1])})
    res = run_bass_kernel_spmd(nc, in_maps, core_ids=list(range(8)), trace=trace)
    return [r["y"] for r in res.results], res


def build_fused():
    nc = bass.Bass("TRN2", target_bir_lowering=False)
    dt = lambda name, shape: nc.dram_tensor(name, shape, F32, kind="ExternalInput").ap()
    xo = dt("xo", [SEQH, D])
    xp = dt("xp", [SEQH, D])
    mem = dt("mem", [MEM_LEN, D])
    cst = dt("cst", [128, CONST_W])
    parA = dt("parA", [128, PA_W])
    parC = dt("parC", [128, PC_W])
    parF = dt("parF", [128, 16])
    win0_d = dt("win0", [D, 2560])
    win1_d = dt("win1", [D, SSM_IN])
    wkv0_d = dt("wkv0", [D, 512])
    wkv1_d = dt("wkv1", [D, 512])
    wo0_d = dt("wo0", [D, D])
    wo1_d = dt("wo1", [D, D])
    fg = dt("fg", [D, FFN_DIM])
    fu = dt("fu", [D, FFN_DIM])
    fd = dt("fd", [FFN_DIM, D])
    wr = dt("wr", [D, N_EXPERTS])
    eg = dt("eg", [N_EXPERTS, D, EXPERT_DIM])
    eu = dt("eu", [N_EXPERTS, D, EXPERT_DIM])
    ed = dt("ed", [N_EXPERTS, EXPERT_DIM, D])
    y = nc.dram_tensor("y", [SEQH, D], F32, kind="ExternalOutput").ap()
    kscr = nc.dram_tensor("kscr", [6, 128, 2 * SEQH], BF16).ap()
    vscr = nc.dram_tensor("vscr", [6, 128, 32, 128], BF16).ap()
    hc_d = nc.dram_tensor("hcd", [N_EXPERTS * CAP, D], BF16).ap()
    yc_d = nc.dram_tensor("ycd", [N_EXPERTS * CAP, D], F32).ap()
    cnt_d = nc.dram_tensor("cntd", [1, 8], I32).ap()
    k = K(nc)
    with k.st:
        m = k.m
        k.setup_consts(cst)
        xT_off = m.off
        xT = m.alloc("xT", [128, NCH, SEQH], F32)
        gF = m.alloc("gF", [128, 16], F32)
        gF32 = m.alloc("gF32", [128, 16], F32)
        zt = m.alloc("zt", [128, D], BF16)
        k.dma("sp", gF[:], parF[:, :], (), ["gFraw"], "parF")
        k.ts(gF32[:], gF[:], 32.0, None, ALU.mult, None, ["gFraw"], ["par"])
        mk_layers = m.mark()
        gA = attn_gains(k, parA)
        gC = ssd_gains(k, parC)
        KmT0 = m.alloc("KmT0", [64, 4, MEM_LEN], BF16)
        Vm0 = m.alloc("Vm0", [128, 2, 256], BF16)
        KmT1 = m.alloc("KmT1", [64, 4, MEM_LEN], BF16)
        Vm1 = m.alloc("Vm1", [128, 2, 256], BF16)
        mem_kv_setup(k, mem, wkv0_d, gA["gmem32"], gA["gmk8"], KmT0, Vm0, "0")
        mem_kv_setup(k, mem, wkv1_d, gC["gmem32"], gC["gmk8"], KmT1, Vm1, "1")
        H = m.alloc("H", [128, TOK_W], F32)
        Hbf = m.alloc("Hbf", [128, TOK_W], BF16)
        halo = m.alloc("halo", [128, 10, 3], F32)
        k.memset(H[:, :], 0.0, ["H"], eng="dve")
        k.memset(Hbf[:, :], 0.0, ["Hbf"], eng="pool")
        k.memset(halo[:, :, :], 0.0, [("halo", cc) for cc in range(10)], eng="pool")

        def layer0(xd, nprev):
            mk = m.mark()
            QT = m.alloc("QT", [128, 6, SEQH], BF16)
            mqT = m.alloc("mqT", [64, 4, SEQH], BF16)
            mk1 = m.mark()
            win = m.alloc("win", [128, NCH, 2560], BF16)
            winv = win0_d.rearrange("(c p) f -> p c f", p=128)
            for j in range(5):
                k.dma("pool", win[:, :, j * 512:(j + 1) * 512], winv[:, :, j * 512:(j + 1) * 512], (), ["win"], "win")
            tiles = [(xd[i * TT:(i + 1) * TT, :], i) for i in range(NTT)]
            attn_inproj(k, tiles, xT, gA["g32"], win, gA["gq8"], gA["gk8"], gA["gmq8"], QT, mqT, kscr, vscr, nprev)
            m.release(mk1)
            mem_attn(k, mqT, mqT, KmT0, Vm0, "0")
            attn_core(k, QT, QT, kscr, vscr, gA["neglam"], gA["subgs"], gA["flagb"], nprev)
            attn_outproj(k, xT, "xT", QT, "tokT", 6, mqT, wo0_d, "0")
            m.release(mk)
            mk = m.mark()
            h2T = m.alloc("h2T", [128, NCH, SEQH], BF16)
            mk2 = m.mark()
            sq = m.alloc("sq", [128, NCH, TT], BF16)
            rstd = m.alloc("rstd", [128, TT], F32)
            for tt in range(NTT):
                k.rmsnorm_T(xT, "xT", gF32[:, 0:8], h2T, "h2T", slice(tt * TT, (tt + 1) * TT), tt, sq, "sq", rstd, "rstd")
            m.release(mk2)
            k.ffn_bufs()
            k.ffn(xT, "xT", h2T, "h2T", fg, fu, fd, FFN_DIM)
            m.release(mk)

        layer0(xp, 0)
        hc_zero_fill(k, zt, hc_d)
        mk = m.mark()
        win1 = m.alloc("win1", [128, NCH, SSM_IN], BF16)
        win1v = win1_d.rearrange("(c p) f -> p c f", p=128)
        for (a, b) in ((768, 1280), (1280, 1792), (1792, 2060)):
            k.dma("pool", win1[:, :, a:b], win1v[:, :, a:b], (), ["win1"], "win1")
        ssd_pass(k, xT, "xT", gC, win1, False, H, Hbf, halo, None, None)
        m.release(mk)
        k.ts(H[:, :], H[:, :], gC["flag"], None, ALU.mult, None, ["H", "gains"], ["H"])
        k.copy(Hbf[:, :], H[:, :], ["H"], ["Hbf"], eng="pool")
        for cc in range(10):
            k.ts(halo[:, cc, :], halo[:, cc, :], gC["flag"], None, ALU.mult, None, [("halo", cc), "gains"], [("halo", cc)])
        k.s.barrier()
        layer0(xo, NTT)
        mk = m.mark()
        mqT = m.alloc("mqT", [64, 4, SEQH], BF16)
        tokT = m.alloc("tokT1", [128, 6, SEQH], BF16)
        mk1 = m.mark()
        win1 = m.alloc("win1", [128, NCH, SSM_IN], BF16)
        for (a, b) in ((0, 512), (512, 1024), (1024, 1536), (1536, 2048), (2048, SSM_IN)):
            k.dma("pool", win1[:, :, a:b], win1v[:, :, a:b], (), ["win1"], "win1")
        ssd_pass(k, xT, "xT", gC, win1, True, H, Hbf, halo, tokT, mqT)
        m.release(mk1)
        mem_attn(k, mqT, mqT, KmT1, Vm1, "1")
        attn_outproj(k, xT, "xT", tokT, "tokT1", 6, mqT, wo1_d, "1")
        m.release(mk)
        m.release(mk_layers)
        moe_sparse(k, xT, "xT", xT_off, gF32[:, 8:16], wr, eg, eu, ed, y, hc_d, yc_d, cnt_d)
        k.s.emit()
    return nc


def kernel(**inputs):
    inp = {k: np.asarray(v) for k, v in inputs.items()}
    x = inp["x"].astype(np.float32, copy=False)
    ncore = 8
    if "fused" not in _cache:
        _cache["fused"] = build_fused()
    nc = _cache["fused"]
    cst = make_consts()
    parF = np.concatenate([pack_pp(inp["ln2_g"][0]), pack_pp(inp["ln2_g"][1])], axis=1)
    in_maps = []
    for i in range(ncore):
        b, hf = i // 2, i % 2
        in_maps.append({
            "xo": np.ascontiguousarray(x[b, hf * SEQH:(hf + 1) * SEQH]),
            "xp": np.ascontiguousarray(x[b, 0:SEQH]),
            "mem": np.ascontiguousarray(inp["mem"][b]),
            "cst": cst, "parA": pack_par_attn(inp, 0, float(hf)), "parC": pack_par_ssd(inp, float(hf)), "parF": parF,
            "win0": inp["da_w_in"][0], "win1": inp["ssm_w_in"][0], "wkv0": inp["mem_w_kv"][0], "wkv1": inp["mem_w_kv"][1],
            "wo0": inp["w_out"][0], "wo1": inp["w_out"][1],
            "fg": inp["ffn_w_gate"][0], "fu": inp["ffn_w_up"][0], "fd": inp["ffn_w_down"][0],
            "wr": inp["moe_w_router"][0], "eg": inp["moe_w_gate"][0], "eu": inp["moe_w_up"][0], "ed": inp["moe_w_down"][0],
        })
    res = run_bass_kernel_spmd(nc, in_maps, core_ids=list(range(ncore)))
    out = np.empty((4, 2 * SEQH, D), np.float32)
    for i in range(ncore):
        out[i // 2, (i % 2) * SEQH:(i % 2 + 1) * SEQH] = res.results[i]["y"]
    return out


def kernel_unfused(**inputs):
    inp = {k: np.asarray(v) for k, v in inputs.items()}
    x = inp["x"].astype(np.float32, copy=False)
    ncore = 8
    bs = [i // 2 for i in range(ncore)]
    hf = [i % 2 for i in range(ncore)]
    flags = [float(h) for h in hf]
    mems = [inp["mem"][b] for b in bs]
    xo = [x[bs[i], hf[i] * SEQH:(hf[i] + 1) * SEQH] for i in range(ncore)]
    xp = [x[bs[i], 0:SEQH] for i in range(ncore)]
    x1, _ = run_attn0(inp, xo, xp, flags, mems)
    x2 = run_ffn0(x1, inp["ln2_g"][0], inp["ffn_w_gate"][0], inp["ffn_w_up"][0], inp["ffn_w_down"][0])
    xp2 = [x2[2 * bs[i]] for i in range(ncore)]
    x3, _ = run_ssd(inp, x2, xp2, flags, mems)
    x4, _ = run_moe(x3, inp["ln2_g"][1], inp["moe_w_router"][0], inp["moe_w_gate"][0], inp["moe_w_up"][0],
                    inp["moe_w_down"][0])
    out = np.empty((4, 2 * SEQH, D), np.float32)
    for i in range(ncore):
        out[bs[i], hf[i] * SEQH:(hf[i] + 1) * SEQH] = x4[i]
    return out
```

```python
import math
from contextlib import ExitStack

import numpy as np
import concourse.bass as bass
import concourse.mybir as mybir
from concourse.bass_utils import run_bass_kernel_spmd

F32 = mybir.dt.float32
BF16 = mybir.dt.bfloat16
AF = mybir.ActivationFunctionType
ALU = mybir.AluOpType
AX = mybir.AxisListType

D = 1024
NCH = 8
SEQH = 2048
TT = 512
NTT = SEQH // TT
EPS = 1e-6
FFN_DIM = 2816
EXPERT_DIM = 3584
N_EXPERTS = 8
MEM_LEN = 256
TOK_W = 768
NEG = -30000.0


class _Op:
    __slots__ = ("eng", "fn", "deps", "eidx", "is_dma", "dkey", "dval", "signal", "tick", "region")


class Sched:
    ENGS = ("pe", "act", "dve", "pool", "sp")

    def __init__(self, nc):
        self.nc = nc
        self.eng_ops = {e: [] for e in self.ENGS}
        self.last_w = {}
        self.readers = {}
        self.dma_tot = {}
        self.last_dma = {}
        self.barrier_deps = []
        self.cur_region = None
        self.reg_keys = []
        self.cur_regs = {}

    def add(self, eng, fn, reads=(), writes=(), dma_key=None):
        op = _Op()
        op.eng = eng
        op.fn = fn
        op.is_dma = dma_key is not None
        op.dkey = dma_key
        op.signal = False
        op.tick = 0
        op.dval = 0
        op.eidx = len(self.eng_ops[eng])
        op.region = self.cur_region
        assert not (op.is_dma and op.region is not None and eng not in ("pool", "sp"))
        deps = {}

        def need(P, dval=None):
            if P is None or P is op:
                return
            if P.is_dma:
                deps[id(P)] = (P, self.dma_tot[P.dkey] if dval is None else dval)
                return
            if P.eng == eng:
                if eng == "pe":
                    return
                if (not op.is_dma) and (op.eidx - P.eidx) > 3:
                    cnt = 0
                    lst = self.eng_ops[eng]
                    for qi in range(len(lst) - 1, P.eidx, -1):
                        o = lst[qi]
                        if o.region is None or o.region == op.region:
                            cnt += 1
                            if cnt >= 3:
                                break
                    if cnt >= 3:
                        return
            deps[id(P)] = (P, 0)

        for (P, bval) in self.barrier_deps:
            need(P, bval)
        for t in reads:
            need(self.last_w.get(t))
        for t in writes:
            need(self.last_w.get(t))
            for r in self.readers.get(t, ()):
                need(r)
        for t in reads:
            self.readers.setdefault(t, []).append(op)
        for t in writes:
            self.last_w[t] = op
            self.readers[t] = []
        if op.is_dma:
            self.dma_tot[dma_key] = self.dma_tot.get(dma_key, 0) + 16
            op.dval = self.dma_tot[dma_key]
            self.last_dma[dma_key] = op
        op.deps = list(deps.values())
        self.eng_ops[eng].append(op)
        return op

    def barrier(self):
        deps = []
        for e in self.ENGS:
            for op in reversed(self.eng_ops[e]):
                if not op.is_dma:
                    deps.append((op, None))
                    break
        fz = getattr(self, "freeze_weights", False)
        deps.extend((P, self.dma_tot[P.dkey] if (fz and str(P.dkey)[:2] in ("wg", "wu", "wd")) else None)
                    for P in self.last_dma.values())
        self.barrier_deps = deps

    def emit(self):
        nc = self.nc
        for e in self.ENGS:
            for op in self.eng_ops[e]:
                for (P, _v) in op.deps:
                    if not P.is_dma:
                        P.signal = True
        for e in self.ENGS:
            c = 0
            for op in self.eng_ops[e]:
                if op.signal:
                    c += 1
                    op.tick = c
        with ExitStack() as st:
            esem = {e: st.enter_context(nc.semaphore("sem_" + e)) for e in self.ENGS}
            dsem = {k: st.enter_context(nc.semaphore("dsem_%d" % i)) for i, k in enumerate(self.dma_tot)}
            block = st.enter_context(nc.Block())

            def run(ename, eng):
                seen = {}
                regs = {}
                if ename in ("pe", "act", "dve", "pool", "sp"):
                    for rk in self.reg_keys:
                        regs[rk] = eng.alloc_register("r_%s_%s" % (ename, rk))
                self.cur_regs = regs

                def emit_op(op):
                    for (P, v) in op.deps:
                        if P.is_dma:
                            key = ("d", P.dkey)
                            sem = dsem[P.dkey]
                            val = v
                        else:
                            key = ("e", P.eng)
                            sem = esem[P.eng]
                            val = P.tick
                        if seen.get(key, 0) < val:
                            eng.wait_ge(sem, val)
                            seen[key] = val
                    inst = op.fn(eng)
                    if op.is_dma:
                        inst.then_inc(dsem[op.dkey], 16)
                    elif op.signal:
                        inst.then_inc(esem[ename], 1)

                ops = self.eng_ops[ename]
                i = 0
                tick_before = 0
                while i < len(ops):
                    reg = ops[i].region
                    j = i
                    while j < len(ops) and ops[j].region == reg:
                        j += 1
                    group = ops[i:j]
                    nsig = sum(1 for op in group if op.signal and not op.is_dma)
                    if reg is None:
                        for op in group:
                            emit_op(op)
                    else:
                        rk, thr = reg
                        with eng.If_lt(regs[rk], thr + 1):
                            if nsig:
                                if tick_before > 0:
                                    eng.wait_ge(esem[ename], tick_before)
                                eng.nop().then_inc(esem[ename], nsig)
                            else:
                                eng.nop()
                            dk = {}
                            for op in group:
                                if op.is_dma:
                                    first, n = dk.get(op.dkey, (op.dval - 16, 0))
                                    dk[op.dkey] = (first, n + 1)
                            for key, (first, n) in dk.items():
                                if first > 0:
                                    eng.wait_ge(dsem[key], first)
                                eng.nop().then_inc(dsem[key], 16 * n)
                        with eng.Else():
                            saved = dict(seen)
                            for op in group:
                                emit_op(op)
                            seen.clear()
                            seen.update(saved)
                    tick_before += nsig
                    i = j

            @block.tensor
            def _(eng):
                run("pe", eng)

            @block.scalar
            def _(eng):
                run("act", eng)

            @block.vector
            def _(eng):
                run("dve", eng)

            @block.gpsimd
            def _(eng):
                run("pool", eng)

            @block.sync
            def _(eng):
                run("sp", eng)


class Mem:
    def __init__(self, nc, sched):
        self.nc = nc
        self.s = sched
        self.off = 16512
        self.n = 0
        self.limit = 229376

    def alloc(self, name, shape, dtype):
        size = 1
        for d in shape[1:]:
            size *= d
        size *= 2 if dtype == BF16 else 4
        size = (size + 63) // 64 * 64
        self.n += 1
        t = self.nc.alloc_sbuf_tensor_at("%s_%d" % (name, self.n), list(shape), dtype, offset=self.off)
        self.off += size
        assert self.off <= self.limit, "SBUF overflow at %s: %d" % (name, self.off)
        return t

    def overlay(self, name, shape, dtype, off):
        self.n += 1
        return self.nc.alloc_sbuf_tensor_at("%s_%d" % (name, self.n), list(shape), dtype, offset=off)

    def mark(self):
        return self.off

    def release(self, mark):
        self.off = mark
        self.s.barrier()


class K:
    def __init__(self, nc):
        self.nc = nc
        self.s = Sched(nc)
        self.m = Mem(nc, self.s)
        self.st = ExitStack()
        self.psall = self.st.enter_context(nc.psum_tensor("psall", [128, 4096], F32))
        self.ps = [self.psall[:, i * 512:(i + 1) * 512] for i in range(8)]
        self.uid = 0

    def mm(self, out, pairs, reads, writes):
        n = len(pairs)

        def fn(eng, out=out, pairs=pairs, n=n):
            inst = None
            for i, (l, r) in enumerate(pairs):
                inst = eng.matmul(out, l, r, start=(i == 0), stop=(i == n - 1))
            return inst
        return self.s.add("pe", fn, reads, writes)

    def mm1(self, out, lhsT, rhs, start, stop, reads, writes):
        return self.s.add("pe", lambda eng, o=out, l=lhsT, r=rhs, a=start, b=stop: eng.matmul(o, l, r, start=a, stop=b),
                          reads, writes)

    def tr(self, out, in_, ident, reads, writes):
        return self.s.add("pe", lambda eng, o=out, i=in_, d=ident: eng.transpose(o, i, d), reads, writes)

    def act(self, out, in_, func, reads, writes, bias=None, scale=1.0, accum_out=None, eng="act"):
        def fn(e, out=out, in_=in_, func=func, bias=bias, scale=scale, accum_out=accum_out):
            kw = {}
            if bias is not None:
                kw["bias"] = bias
            if accum_out is not None:
                kw["accum_out"] = accum_out
            return e.activation(out=out, in_=in_, func=func, scale=scale, **kw)
        return self.s.add(eng, fn, reads, writes)

    def tt(self, out, in0, in1, op, reads, writes, eng="dve"):
        return self.s.add(eng, lambda e, o=out, a=in0, b=in1, p=op: e.tensor_tensor(o, a, b, p), reads, writes)

    def ts(self, out, in0, s1, s2, op0, op1, reads, writes, eng="dve"):
        def fn(e, o=out, a=in0, s1=s1, s2=s2, op0=op0, op1=op1):
            if op1 is None:
                return e.tensor_scalar(o, a, s1, None, op0)
            return e.tensor_scalar(o, a, s1, s2, op0, op1)
        return self.s.add(eng, fn, reads, writes)

    def stt(self, out, in0, scalar, in1, op0, op1, reads, writes, eng="dve"):
        return self.s.add(eng, lambda e, o=out, a=in0, sc=scalar, b=in1, p0=op0, p1=op1:
                          e.scalar_tensor_tensor(o, a, sc, b, p0, p1), reads, writes)

    def copy(self, out, in_, reads, writes, eng="dve"):
        if eng == "act":
            return self.s.add("act", lambda e, o=out, i=in_: e.copy(o, i), reads, writes)
        return self.s.add(eng, lambda e, o=out, i=in_: e.tensor_copy(o, i), reads, writes)

    def memset(self, ap, val, writes, eng="pool"):
        return self.s.add(eng, lambda e, a=ap, v=val: e.memset(a, v), (), writes)

    def dma(self, queue, out, in_, reads, writes, key):
        return self.s.add(queue, lambda e, o=out, i=in_: e.dma_start(out=o, in_=i), reads, writes, dma_key=key)

    def wdma(self, out, in_, tok, key):
        n = getattr(self, "_wn", 0)
        self._wn = n + 1
        reads = [("wdma", n - 2)] if n >= 2 else []
        return self.dma("pool", out, in_, reads, [tok, ("wdma", n)], key)

    @staticmethod
    def xtok(name, tile512, cs=range(NCH)):
        return [(name, c, tile512 * 4 + j) for c in cs for j in range(4)]

    def eps_ap(self, val):
        key = float(val)
        if key not in self.epsc:
            i = len(self.epsc)
            self.memset(self.epst[:, i:i + 1], key, ["epsc"], eng="dve")
            self.epsc[key] = self.epst[:, i:i + 1]
        return self.epsc[key]

    def setup_consts(self, cst):
        m = self.m
        self.c_f32 = m.alloc("cf32", [128, CONST_W], F32)
        self.dma("sp", self.c_f32[:], cst[:, :], (), ["cf32"], "cst")
        self.ident = self.c_f32[:, 0:128]
        self.ones_f = self.c_f32[:, 128:256]
        self.utri = self.c_f32[:, 256:384]
        self.maskneg = self.c_f32[:, 384:512]
        self.bd_f = self.c_f32[:, 512:640]
        self.sel = self.c_f32[0:8, 640:640 + 8 * 128]
        self.negutri = self.c_f32[:, 1664:1792]
        self.epst = m.alloc("epst", [128, 16], F32)
        self.epsc = {}
        self.c_bf = m.alloc("cbf", [128, 512], BF16)
        self.copy(self.c_bf[:, 0:128], self.ident, ["cf32"], ["cbf"])
        self.copy(self.c_bf[:, 128:256], self.ones_f, ["cf32"], ["cbf"])
        self.copy(self.c_bf[:, 256:384], self.bd_f, ["cf32"], ["cbf"])
        self.ident_b = self.c_bf[:, 0:128]
        self.ones_b = self.c_bf[:, 128:256]
        self.bd_b = self.c_bf[:, 256:384]
        self.copy(self.c_bf[:, 384:512], self.utri, ["cf32"], ["cbf"])
        self.utri_b = self.c_bf[:, 384:512]

    def load_xT(self, x_dram, xT, name, ntok=SEQH):
        m = self.m
        mk = m.mark()
        stg = [m.alloc("xstg", [128, D], F32) for _ in range(2)]
        for i in range(ntok // 128):
            sb = stg[i % 2]
            tk = "xstg%d" % (i % 2)
            self.dma("sp", sb[:], x_dram[i * 128:(i + 1) * 128, :], (), [tk], tk)
            for c in range(NCH):
                pb = self.ps[(i * NCH + c) % 2]
                pk = "ps%d" % ((i * NCH + c) % 2)
                self.tr(pb[:, 0:128], sb[:, c * 128:(c + 1) * 128], self.ident, [tk, "cf32"], [pk])
                eng = "act" if c % 2 == 0 else "dve"
                self.copy(xT[:, c, i * 128:(i + 1) * 128], pb[:, 0:128], [pk], [(name, c, i)], eng=eng)
        m.release(mk)

    def store_xT(self, xT, y_dram, name, ntok=SEQH, final=True):
        m = self.m
        mk = m.mark()
        stg = [m.alloc("ystg", [128, D], F32) for _ in range(2)]
        for i in range(ntok // 128):
            sb = stg[i % 2]
            tk = "ystg%d" % (i % 2)
            for c in range(NCH):
                pb = self.ps[(i * NCH + c) % 2]
                pk = "ps%d" % ((i * NCH + c) % 2)
                self.tr(pb[:, 0:128], xT[:, c, i * 128:(i + 1) * 128], self.ident, [(name, c, i), "cf32"], [pk])
                eng = "act" if c % 2 == 0 else "dve"
                self.copy(sb[:, c * 128:(c + 1) * 128], pb[:, 0:128], [pk], [tk], eng=eng)
            self.dma("sp", y_dram[i * 128:(i + 1) * 128, :], sb[:], [tk], [("yout", i)], "yout")
        if final:
            self.s.add("sp", lambda e: e.nop(), [("yout", i) for i in range(ntok // 128)], ())
        m.release(mk)

    def rmsnorm_T(self, xT, xname, g32, hT, hname, tsl, tile_id, sq, sqname, rstd, rname, ps_bank=6, htile=None):
        n = tsl.stop - tsl.start
        if htile is None:
            htile = tile_id
        pb = self.ps[ps_bank]
        pk = "ps%d" % ps_bank
        self.act(sq[:, :, 0:n], xT[:, :, tsl], AF.Square, self.xtok(xname, tile_id), [sqname])
        self.mm(pb[:, 0:n], [(self.ones_b, sq[:, c, 0:n]) for c in range(NCH)], [sqname, "cbf"], [pk])
        self.act(rstd[:, 0:n], pb[:, 0:n], AF.Ln, [pk, "epsc"], [rname], bias=self.eps_ap(D * EPS))
        self.act(rstd[:, 0:n], rstd[:, 0:n], AF.Exp, [rname], [rname], scale=-0.5)
        for c in range(NCH):
            self.stt(hT[:, c, 0:n] if hT.shape[2] == n else hT[:, c, tsl], xT[:, c, tsl], g32[:, c:c + 1], rstd[:, 0:n],
                     ALU.mult, ALU.mult, self.xtok(xname, tile_id, [c]) + [rname, "par"], [(hname, c, htile)],
                     eng="dve")

    def ffn_chunks(self, wg_d, wu_d, wd_d, F, gate_bc=None, gname=None, pre=None):
        FC = 512
        wgv = wg_d.rearrange("(c p) f -> p c f", p=128)
        wuv = wu_d.rearrange("(c p) f -> p c f", p=128)
        wdv = wd_d.rearrange("(s p) d -> p s d", p=128)
        out = []
        for fc in range((F + FC - 1) // FC):
            f0 = fc * FC
            fw = min(FC, F - f0)
            out.append(dict(wgv=wgv, wuv=wuv, wdv=wdv, f0=f0, fw=fw, gate_bc=gate_bc, gname=gname,
                            pre=pre if fc == 0 else None))
        return out

    def ffn_run(self, xT, xname, h2T, hname, chunks):
        nch = len(chunks)

        def load(ci):
            ch = chunks[ci]
            slot = ci % 2
            wg, wu, wd = self.wbuf[slot]
            kg, ku, kd = ("wg%d" % slot, "wu%d" % slot, "wd%d" % slot)
            f0, fw = ch["f0"], ch["fw"]
            nfs = fw // 128
            self.wdma(wg[:, :, 0:fw], ch["wgv"][:, :, f0:f0 + fw], kg, kg)
            self.wdma(wu[:, :, 0:fw], ch["wuv"][:, :, f0:f0 + fw], ku, ku)
            self.wdma(wd[:, 0:nfs, :], ch["wdv"][:, f0 // 128:f0 // 128 + nfs, :], kd, kd)

        units = [(ci, tt) for ci in range(nch) for tt in range(NTT)]

        def GU(ui):
            ci, tt = units[ui]
            ch = chunks[ci]
            if tt == 0 and ch["pre"] is not None:
                ch["pre"]()
            slot = ci % 2
            wg, wu, wd = self.wbuf[slot]
            kg, ku = "wg%d" % slot, "wu%d" % slot
            nfs = ch["fw"] // 128
            tsl = slice(tt * TT, (tt + 1) * TT)
            ab = ui % 2
            for fs in range(nfs):
                j = self.cnt % 2
                self.cnt += 1
                pg, pu = self.ps[j], self.ps[2 + j]
                kpg, kpu = "ps%d" % j, "ps%d" % (2 + j)
                hrd = [(hname, c, tt) for c in range(NCH)]
                self.mm(pg[:, :], [(wg[:, c, fs * 128:(fs + 1) * 128], h2T[:, c, tsl]) for c in range(NCH)], [kg] + hrd, [kpg])
                self.mm(pu[:, :], [(wu[:, c, fs * 128:(fs + 1) * 128], h2T[:, c, tsl]) for c in range(NCH)], [ku] + hrd, [kpu])
                sg = self.sgbuf[j]
                ksg = "sg%d" % j
                self.act(sg[:, :], pg[:, :], AF.Silu, [kpg], [ksg])
                if ch["gate_bc"] is not None:
                    self.tt(sg[:, :], sg[:, :], ch["gate_bc"][:, tsl], ALU.mult, [ksg, ch["gname"]], [ksg], eng="dve")
                self.tt(self.actbuf[ab][:, fs, :], pu[:, :], sg[:, :], ALU.mult, [kpu, ksg], [("actT", ab, fs)])

        def DN(ui):
            ci, tt = units[ui]
            ch = chunks[ci]
            slot = ci % 2
            wd = self.wbuf[slot][2]
            kd = "wd%d" % slot
            nfs = ch["fw"] // 128
            tsl = slice(tt * TT, (tt + 1) * TT)
            ab = ui % 2
            abuf = self.actbuf[ab]
            kab = [("actT", ab, fs) for fs in range(nfs)]
            for ds in range(NCH):
                j = self.cnt2 % 3
                self.cnt2 += 1
                pd = self.ps[4 + j]
                kpd = "ps%d" % (4 + j)
                self.mm(pd[:, :], [(wd[:, fs, ds * 128:(ds + 1) * 128], abuf[:, fs, :]) for fs in range(nfs)], [kd] + kab, [kpd])
                self.tt(xT[:, ds, tsl], pd[:, :], xT[:, ds, tsl], ALU.add, [kpd] + self.xtok(xname, tt, [ds]), self.xtok(xname, tt, [ds]))

        load(0)
        if nch > 1:
            load(1)
        GU(0)
        for ui in range(len(units)):
            if ui + 1 < len(units):
                GU(ui + 1)
            DN(ui)
            ci, tt = units[ui]
            if tt == NTT - 1 and ci + 2 < nch:
                load(ci + 2)

    def ffn(self, xT, xname, h2T, hname, wg_d, wu_d, wd_d, F, gate_bc=None, gname=None):
        self.ffn_run(xT, xname, h2T, hname, self.ffn_chunks(wg_d, wu_d, wd_d, F, gate_bc, gname))

    def ffn_bufs(self):
        m = self.m
        self.wbuf = [(m.alloc("wg", [128, NCH, 512], BF16), m.alloc("wu", [128, NCH, 512], BF16),
                      m.alloc("wd", [128, 4, D], BF16)) for _ in range(2)]
        self.sgbuf = [m.alloc("sg", [128, TT], F32) for _ in range(2)]
        self.actbuf = [m.alloc("actT", [128, 4, TT], BF16) for _ in range(2)]
        self.wslot = 0
        self.cnt = 0
        self.cnt2 = 0
        self.acnt = 0


CONST_W = 640 + 8 * 128 + 128


def make_consts():
    c = np.zeros((128, CONST_W), np.float32)
    c[:, 0:128] = np.eye(128, dtype=np.float32)
    c[:, 128:256] = 1.0
    r = np.arange(128)
    c[:, 256:384] = (r[:, None] <= r[None, :]).astype(np.float32)
    c[:, 384:512] = np.where(r[:, None] <= r[None, :], 0.0, NEG).astype(np.float32)
    bd = np.zeros((128, 128), np.float32)
    bd[:64, :64] = 1.0
    bd[64:, 64:] = 1.0
    c[:, 512:640] = bd
    for e in range(8):
        c[e, 640 + e * 128:640 + (e + 1) * 128] = 1.0
    c[:, 1664:1792] = -c[:, 256:384]
    return c


def pack_pp(v):
    return np.ascontiguousarray(np.asarray(v, np.float32).reshape(NCH, 128).T)


def build_ffn0():
    nc = bass.Bass("TRN2", target_bir_lowering=False)
    x = nc.dram_tensor("x", [SEQH, D], F32, kind="ExternalInput").ap()
    cst = nc.dram_tensor("cst", [128, CONST_W], F32, kind="ExternalInput").ap()
    par = nc.dram_tensor("par", [128, 8], F32, kind="ExternalInput").ap()
    wg = nc.dram_tensor("wg", [D, FFN_DIM], F32, kind="ExternalInput").ap()
    wu = nc.dram_tensor("wu", [D, FFN_DIM], F32, kind="ExternalInput").ap()
    wd = nc.dram_tensor("wd", [FFN_DIM, D], F32, kind="ExternalInput").ap()
    y = nc.dram_tensor("y", [SEQH, D], F32, kind="ExternalOutput").ap()
    k = K(nc)
    with k.st:
        m = k.m
        k.setup_consts(cst)
        xT = m.alloc("xT", [128, NCH, SEQH], F32)
        g = m.alloc("g", [128, 8], F32)
        g32 = m.alloc("g32", [128, 8], F32)
        k.dma("sp", g[:], par[:, :], (), ["graw"], "par")
        k.ts(g32[:], g[:], 32.0, None, ALU.mult, None, ["graw"], ["par"])
        k.load_xT(x, xT, "xT")
        h2T = m.alloc("h2T", [128, NCH, SEQH], BF16)
        mk = m.mark()
        sq = m.alloc("sq", [128, NCH, TT], BF16)
        rstd = m.alloc("rstd", [128, TT], F32)
        for tt in range(NTT):
            k.rmsnorm_T(xT, "xT", g32, h2T, "h2T", slice(tt * TT, (tt + 1) * TT), tt, sq, "sq", rstd, "rstd")
        m.release(mk)
        k.ffn_bufs()
        k.ffn(xT, "xT", h2T, "h2T", wg, wu, wd, FFN_DIM)
        k.store_xT(xT, y, "xT")
        k.s.emit()
    return nc


_cache = {}


def run_ffn0(x_shards, ln2_g0, wg, wu, wd):
    if "ffn0" not in _cache:
        _cache["ffn0"] = build_ffn0()
    nc = _cache["ffn0"]
    cst = make_consts()
    par = pack_pp(ln2_g0)
    in_maps = [{"x": np.ascontiguousarray(xs), "cst": cst, "par": par, "wg": wg, "wu": wu, "wd": wd} for xs in x_shards]
    res = run_bass_kernel_spmd(nc, in_maps, core_ids=list(range(8)))
    return [r["y"] for r in res.results]


def moe_block(k, xT, xname, h2T, hname, wr_d, wg_d, wu_d, wd_d):
    m = k.m
    NS = SEQH // 128
    wr = m.alloc("wr", [128, NCH, 8], BF16)
    k.dma("pool", wr[:], wr_d.rearrange("(c p) e -> p c e", p=128), (), ["wr"], "wr")
    lg = m.alloc("lg", [128, NS, 8], F32)
    pb = k.ps[7]
    for i in range(NS):
        k.mm(pb[:, i * 8:(i + 1) * 8], [(h2T[:, c, i * 128:(i + 1) * 128], wr[:, c, :]) for c in range(NCH)],
             ["wr"] + [(hname, c, i // 4) for c in range(NCH)], ["ps7"])
    k.copy(lg[:].rearrange("p a b -> p (a b)"), pb[:, 0:NS * 8], ["ps7"], ["lg"], eng="act")
    mk = m.mark()
    m1 = m.alloc("m1", [128, NS], F32)
    m2 = m.alloc("m2", [128, NS], F32)
    eq1 = m.alloc("eq1", [128, NS, 8], F32)
    eq2 = m.alloc("eq2", [128, NS, 8], F32)
    lg2 = m.alloc("lg2", [128, NS, 8], F32)
    w1 = m.alloc("w1", [128, NS], F32)
    w2 = m.alloc("w2", [128, NS], F32)
    gates = m.alloc("gates", [128, NS, 8], F32)

    def bc(t):
        return t[:, :].unsqueeze(2).broadcast_to([128, NS, 8])

    s = k.s
    s.add("dve", lambda e: e.tensor_reduce(m1[:, :], lg[:, :, :], AX.X, ALU.max), ["lg"], ["m1"])
    k.tt(eq1[:], lg[:], bc(m1), ALU.is_equal, ["lg", "m1"], ["eq1"])
    k.stt(lg2[:], eq1[:], -1e30, lg[:], ALU.mult, ALU.add, ["eq1", "lg"], ["lg2"])
    s.add("dve", lambda e: e.tensor_reduce(m2[:, :], lg2[:, :, :], AX.X, ALU.max), ["lg2"], ["m2"])
    k.tt(eq2[:], lg2[:], bc(m2), ALU.is_equal, ["lg2", "m2"], ["eq2"])
    k.tt(w1[:], m1[:], m2[:], ALU.subtract, ["m1", "m2"], ["w1"])
    k.act(w1[:], w1[:], AF.Sigmoid, ["w1"], ["w1"])
    k.ts(w2[:], w1[:], -1.0, 1.0, ALU.mult, ALU.add, ["w1"], ["w2"])
    k.tt(eq1[:], eq1[:], bc(w1), ALU.mult, ["eq1", "w1"], ["eq1"])
    k.tt(eq2[:], eq2[:], bc(w2), ALU.mult, ["eq2", "w2"], ["eq2"])
    k.tt(gates[:], eq1[:], eq2[:], ALU.add, ["eq1", "eq2"], ["gates"])
    gT = m.alloc("gT", [8, SEQH], F32)
    for t4 in range(NTT):
        pbk = k.ps[6]
        for j in range(4):
            i = t4 * 4 + j
            k.tr(pbk[0:8, j * 128:(j + 1) * 128], gates[:, i, :], k.ident, ["gates", "cf32"], ["ps6"])
        k.copy(gT[:, t4 * TT:(t4 + 1) * TT], pbk[0:8, :], ["ps6"], [("gT", t4)], eng="act")
    gbc = [m.alloc("gbc", [128, SEQH], F32) for _ in range(2)]
    k.ffn_bufs()

    def make_pre(e):
        def pre():
            gb = gbc[e % 2]
            gk = "gbc%d" % (e % 2)
            for t4 in range(NTT):
                pbk = k.ps[7]
                pkk = "ps7"
                k.mm(pbk[:, :], [(k.sel[:, e * 128:(e + 1) * 128], gT[:, t4 * TT:(t4 + 1) * TT])], [("gT", t4), "cf32"], [pkk])
                k.copy(gb[:, t4 * TT:(t4 + 1) * TT], pbk[:, :], [pkk], [gk], eng="act")
        return pre

    chunks = []
    for e in range(N_EXPERTS):
        chunks += k.ffn_chunks(wg_d[e], wu_d[e], wd_d[e], EXPERT_DIM, gate_bc=gbc[e % 2], gname="gbc%d" % (e % 2),
                               pre=make_pre(e))
    k.ffn_run(xT, xname, h2T, hname, chunks)


CAP = SEQH
ST = 512
NSLT = CAP // ST
I32 = mybir.dt.int32


def hc_zero_fill(k, zt, hc_d):
    k.memset(zt[:, :], 0.0, ["zt"], eng="pool")
    for j in range(N_EXPERTS * CAP // 128):
        k.dma("sp", hc_d[j * 128:(j + 1) * 128, :], zt[:, :], ["zt"], ["hcz"], "hcz")


def moe_sparse(k, xT, xname, xT_off, g32, wr_d, wg_d, wu_d, wd_d, y_dram, hc_d, yc_d, cnt_d):
    m = k.m
    s = k.s
    NS = SEQH // 128
    NFC = EXPERT_DIM // 512
    k.ffn_bufs()
    chunks = []
    for e in range(N_EXPERTS):
        wgv = wg_d[e].rearrange("(c p) f -> p c f", p=128)
        wuv = wu_d[e].rearrange("(c p) f -> p c f", p=128)
        wdv = wd_d[e].rearrange("(s p) d -> p s d", p=128)
        for fc in range(NFC):
            chunks.append(dict(e=e, fc=fc, f0=fc * 512, wgv=wgv, wuv=wuv, wdv=wdv))
    nch = len(chunks)

    def load(ci):
        ch = chunks[ci]
        slot = ci % 2
        wg, wu, wd = k.wbuf[slot]
        kg, ku, kd = ("wg%d" % slot, "wu%d" % slot, "wd%d" % slot)
        f0 = ch["f0"]
        k.wdma(wg[:, :, :], ch["wgv"][:, :, f0:f0 + 512], kg, kg)
        k.wdma(wu[:, :, :], ch["wuv"][:, :, f0:f0 + 512], ku, ku)
        k.wdma(wd[:, :, :], ch["wdv"][:, f0 // 128:f0 // 128 + 4, :], kd, kd)

    load(0)
    load(1)
    d1i = m.alloc("d1i", [128, NS], I32)
    d2i = m.alloc("d2i", [128, NS], I32)
    w1 = m.alloc("w1", [128, NS], F32)
    w2 = m.alloc("w2", [128, NS], F32)
    cnti = m.alloc("cnti", [128, 8], I32)
    stg_off = [m.off, m.off + 8192]
    stg = [m.alloc("stg", [128, 4, D], BF16) for _ in range(2)]
    h2T_off = m.off
    h2T = m.alloc("h2T", [128, NCH, SEQH], BF16)
    hname = "h2T"
    mkA = m.mark()
    sq = m.overlay("sq", [128, NCH, TT], BF16, stg_off[0])
    rstd = m.overlay("rstd", [128, TT], F32, stg_off[1])
    sst = [m.overlay("sst", [128, D], BF16, stg_off[1] + 2048 * (1 + j)) for j in range(2)]
    for tt in range(NTT):
        k.rmsnorm_T(xT, xname, g32, h2T, hname, slice(tt * TT, (tt + 1) * TT), tt, sq, "sq", rstd, "rstd")
    wr = m.alloc("wr", [128, NCH, 8], BF16)
    wrf = m.alloc("wrf", [128, NCH, 8], F32)
    k.dma("sp", wrf[:], wr_d.rearrange("(c p) e -> p c e", p=128), (), ["wrf"], "wrf")
    k.copy(wr[:], wrf[:], ["wrf"], ["wr"])
    lg = m.alloc("lg", [128, NS, 8], F32)
    pb = k.ps[7]
    for i in range(NS):
        k.mm(pb[:, i * 8:(i + 1) * 8], [(h2T[:, c, i * 128:(i + 1) * 128], wr[:, c, :]) for c in range(NCH)],
             ["wr"] + [(hname, c, i // 4) for c in range(NCH)], ["ps7"])
    k.copy(lg[:].rearrange("p a b -> p (a b)"), pb[:, 0:NS * 8], ["ps7"], ["lg"], eng="act")
    m1 = m.alloc("m1", [128, NS], F32)
    m2 = m.alloc("m2", [128, NS], F32)
    eq1 = m.alloc("eq1", [128, NS, 8], F32)
    eq2 = m.alloc("eq2", [128, NS, 8], F32)
    lg2 = m.alloc("lg2", [128, NS, 8], F32)
    mask = m.alloc("mask", [128, NS, 8], F32)
    maskb = m.alloc("maskb", [128, NS, 8], BF16)
    it = m.alloc("it", [128, 2, NS, 8], F32)
    off = m.alloc("off", [128, NS, 8], F32)
    ebase = m.alloc("ebase", [128, NS, 8], F32)
    rank = m.alloc("rank", [128, NS, 8], F32)
    tmp = m.alloc("rtmp", [128, NS, 8], F32)
    d1f = m.alloc("d1f", [128, NS], F32)
    d2f = m.alloc("d2f", [128, NS], F32)
    cntf = m.alloc("cntf", [128, 8], F32)

    def bc(t):
        return t[:, :].unsqueeze(2).broadcast_to([128, NS, 8])

    s.add("dve", lambda e: e.tensor_reduce(m1[:, :], lg[:, :, :], AX.X, ALU.max), ["lg"], ["m1"])
    k.tt(eq1[:], lg[:], bc(m1), ALU.is_equal, ["lg", "m1"], ["eq1"])
    k.stt(lg2[:], eq1[:], -1e30, lg[:], ALU.mult, ALU.add, ["eq1", "lg"], ["lg2"])
    s.add("dve", lambda e: e.tensor_reduce(m2[:, :], lg2[:, :, :], AX.X, ALU.max), ["lg2"], ["m2"])
    k.tt(eq2[:], lg2[:], bc(m2), ALU.is_equal, ["lg2", "m2"], ["eq2"])
    k.tt(w1[:], m1[:], m2[:], ALU.subtract, ["m1", "m2"], ["w1"])
    k.act(w1[:], w1[:], AF.Sigmoid, ["w1"], ["w1"])
    k.ts(w2[:], w1[:], -1.0, 1.0, ALU.mult, ALU.add, ["w1"], ["w2"])
    k.tt(mask[:], eq1[:], eq2[:], ALU.add, ["eq1", "eq2"], ["mask"])
    k.copy(maskb[:], mask[:], ["mask"], ["maskb"])
    for e in range(N_EXPERTS):
        k.memset(ebase[:, :, e:e + 1], float(e * CAP), ["ebase"], eng="dve")
    mb2 = maskb[:].rearrange("p a b -> p (a b)")
    k.mm(k.ps[6][:, 0:128], [(k.utri_b, mb2)], ["maskb", "cbf"], ["ps6"])
    k.mm(k.ps[6][:, 128:256], [(k.ones_b, mb2)], ["maskb", "cbf"], ["ps6"])
    k.copy(it[:].rearrange("p t a b -> p (t a b)"), k.ps[6][:, 0:256], ["ps6"], ["it"], eng="act")
    k.memset(off[:, 0, :], 0.0, [("off", 0)], eng="dve")
    for i in range(1, NS):
        k.tt(off[:, i, :], off[:, i - 1, :], it[:, 1, i - 1, :], ALU.add, [("off", i - 1), "it"], [("off", i)])
    k.tt(cntf[:, :], off[:, NS - 1, :], it[:, 1, NS - 1, :], ALU.add, [("off", NS - 1), "it"], ["cntf"])
    k.copy(cnti[:, :], cntf[:, :], ["cntf"], ["cnti"])
    k.dma("sp", cnt_d[0:1, :], cnti[0:1, :], ["cnti"], ["cntd"], "cntd")
    offr = [("off", i) for i in range(NS)]
    k.tt(rank[:], it[:, 0, :, :], mask[:], ALU.subtract, ["it", "mask"], ["rank"])
    k.tt(rank[:], rank[:], off[:], ALU.add, ["rank"] + offr, ["rank"])
    k.tt(rank[:], rank[:], ebase[:], ALU.add, ["rank", "ebase"], ["rank"])
    k.tt(tmp[:], rank[:], eq1[:], ALU.mult, ["rank", "eq1"], ["rtmp"])
    s.add("dve", lambda e: e.tensor_reduce(d1f[:, :], tmp[:, :, :], AX.X, ALU.add), ["rtmp"], ["d1f"])
    k.copy(d1i[:, :], d1f[:, :], ["d1f"], ["d1i"])
    k.tt(tmp[:], rank[:], eq2[:], ALU.mult, ["rank", "eq2", "d1f"], ["rtmp"])
    s.add("dve", lambda e: e.tensor_reduce(d2f[:, :], tmp[:, :, :], AX.X, ALU.add), ["rtmp"], ["d2f"])
    k.copy(d2i[:, :], d2f[:, :], ["d2f"], ["d2i"])
    s.reg_keys = list(range(N_EXPERTS))
    for en in ("pe", "act", "dve", "pool", "sp"):
        for e in range(N_EXPERTS):
            s.add(en, lambda eng, e=e: eng.reg_load(s.cur_regs[e], cnt_d[0:1, e:e + 1]), ["cntd"], ())
    for i in range(NS):
        j = i % 2
        pbf = k.ps[j].bitcast(BF16)
        pk = "ps%d" % j
        for c in range(NCH):
            k.tr(pbf[:, c * 128:(c + 1) * 128], h2T[:, c, i * 128:(i + 1) * 128], k.ident_b,
                 [(hname, c, i // 4), "cbf"], [pk])
        tk = "sst%d" % j
        k.copy(sst[j][:, :], pbf[:, 0:1024], [pk], [tk], eng="act" if j == 0 else "dve")
        for a, di in enumerate((d1i, d2i)):
            s.add("pool", lambda eng, di=di, i=i, j=j: eng.indirect_dma_start(
                out=hc_d[:, :], out_offset=bass.IndirectOffsetOnAxis(ap=di[:, i:i + 1], axis=0),
                in_=sst[j][:, :], in_offset=None),
                [tk, "hcz", "d1i", "d2i"], [("hcs", i, a)], dma_key="hcs")
    hcs_tokens = [("hcs", i, a) for i in range(NS) for a in range(2)]
    k.store_xT(xT, y_dram, xname, final=False)
    s.freeze_weights = True
    m.release(mkA)
    yacc = m.overlay("yacc", [128, CAP // 128, D], F32, xT_off)
    hTe = m.overlay("hTe", [128, NCH, CAP], BF16, h2T_off)

    def prep(e, tts, region=None):
        for tt in tts:
            sb = stg[tt % 2]
            tk = "stg%d" % (tt % 2)
            r0 = e * CAP + tt * ST
            s.cur_region = region
            k.dma("sp", sb[:, :, :], hc_d[r0:r0 + ST, :].rearrange("(b p) d -> p b d", p=128), hcs_tokens, [tk], tk)
            for blk in range(4):
                pbf = k.ps[7].bitcast(BF16)
                for c in range(NCH):
                    k.tr(pbf[:, c * 128:(c + 1) * 128], sb[:, blk, c * 128:(c + 1) * 128], k.ident_b, [tk, "cbf"], ["ps7"])
                col = (tt * 4 + blk) * 128
                k.copy(hTe[:, :, col:col + 128], pbf[:, 0:1024].rearrange("p (c q) -> p c q", c=NCH), ["ps7"],
                       [("hTe", tt * 4 + blk)], eng="act")
            s.cur_region = None

    MAINW = 640
    main_tiles = [(0, 512, 0), (512, 128, 1)]
    rare_tiles = [(640, 384, 1), (1024, 512, 0), (1536, 512, 1)]

    def GU(ci, tile, region):
        ch = chunks[ci]
        t0, tw, ab = tile
        s.cur_region = region
        slot = ci % 2
        wg, wu, wd = k.wbuf[slot]
        kg, ku = "wg%d" % slot, "wu%d" % slot
        tsl = slice(t0, t0 + tw)
        hrd = [("hTe", b_) for b_ in range(t0 // 128, (t0 + tw) // 128)]
        for fs in range(4):
            j = k.cnt % 2
            k.cnt += 1
            pg, pu = k.ps[j], k.ps[2 + j]
            kpg, kpu = "ps%d" % j, "ps%d" % (2 + j)
            k.mm(pg[:, 0:tw], [(wg[:, c, fs * 128:(fs + 1) * 128], hTe[:, c, tsl]) for c in range(NCH)], [kg] + hrd, [kpg])
            k.mm(pu[:, 0:tw], [(wu[:, c, fs * 128:(fs + 1) * 128], hTe[:, c, tsl]) for c in range(NCH)], [ku] + hrd, [kpu])
            sg = k.sgbuf[j]
            ksg = "sg%d" % j
            k.act(sg[:, 0:tw], pg[:, 0:tw], AF.Silu, [kpg], [ksg])
            k.tt(k.actbuf[ab][:, fs, 0:tw], pu[:, 0:tw], sg[:, 0:tw], ALU.mult, [kpu, ksg], [("actT", ab, fs)])
        s.cur_region = None

    def DN(ci, tile, region):
        ch = chunks[ci]
        t0, tw, ab = tile
        s.cur_region = region
        wd = k.wbuf[ci % 2][2]
        kd = "wd%d" % (ci % 2)
        abuf = k.actbuf[ab]
        kab = [("actT", ab, fs) for fs in range(4)]
        for blk in range(tw // 128):
            gb = t0 // 128 + blk
            for dh in range(2):
                j = k.cnt2 % 3
                k.cnt2 += 1
                pd = k.ps[4 + j]
                kpd = "ps%d" % (4 + j)
                k.mm(pd[:, :], [(abuf[:, fs, blk * 128:(blk + 1) * 128], wd[:, fs, dh * 512:(dh + 1) * 512]) for fs in range(4)],
                     [kd] + kab, [kpd])
                dst = yacc[:, gb, dh * 512:(dh + 1) * 512]
                tok = ("yacc", gb, dh)
                if ch["fc"] == 0:
                    k.copy(dst, pd[:, :], [kpd], [tok])
                else:
                    k.tt(dst, pd[:, :], dst, ALU.add, [kpd, tok], [tok])
        s.cur_region = None

    def ystore(e, gb0, gb1, region=None):
        r0 = e * CAP + gb0 * 128
        s.cur_region = region
        k.dma("sp", yc_d[r0:r0 + (gb1 - gb0) * 128, :].rearrange("(b p) d -> p b d", p=128), yacc[:, gb0:gb1, :],
              [("yacc", gb, dh) for gb in range(gb0, gb1) for dh in range(2)], [("ycd", e, gb0)], "ycd")
        s.cur_region = None

    ycd_tokens = []
    prep(0, (0, 1))
    for ci in range(nch):
        e = chunks[ci]["e"]
        lastc = chunks[ci]["fc"] == NFC - 1
        GU(ci, main_tiles[0], None)
        GU(ci, main_tiles[1], None)
        if lastc and e + 1 < N_EXPERTS:
            prep(e + 1, (0, 1))
        DN(ci, main_tiles[0], None)
        DN(ci, main_tiles[1], None)
        if lastc:
            ystore(e, 0, 4)
            ystore(e, 4, 5)
            ycd_tokens += [("ycd", e, 0), ("ycd", e, 4)]
        if ci + 2 < nch:
            load(ci + 2)
    for e in range(N_EXPERTS):
        for (reg, ptiles, rtiles, stores) in (((e, MAINW), (1,), rare_tiles[0:1], ((5, 8),)),
                                              ((e, 2 * ST), (2, 3), rare_tiles[1:3], ((8, 12), (12, 16)))):
            prep(e, ptiles, region=reg)
            for fc in range(NFC):
                ci = e * NFC + fc
                s.cur_region = reg
                load(ci)
                s.cur_region = None
                for tile in rtiles:
                    GU(ci, tile, reg)
                    DN(ci, tile, reg)
            for (g0, g1) in stores:
                ystore(e, g0, g1, region=reg)
                ycd_tokens.append(("ycd", e, g0))
    mkB = m.mark()
    xs = [m.alloc("xs", [128, D], F32) for _ in range(2)]
    a1 = [m.alloc("a1", [128, D], F32) for _ in range(2)]
    a2 = [m.alloc("a2", [128, D], F32) for _ in range(2)]
    for i in range(NS):
        j = i % 2
        k.dma("sp", xs[j][:, :], y_dram[i * 128:(i + 1) * 128, :], [("yout", i)], ["xs%d" % j], "xs%d" % j)
        for (ab_, di, nm) in ((a1, d1i, "a1"), (a2, d2i, "a2")):
            s.add("pool", lambda eng, ab_=ab_, di=di, i=i, j=j: eng.indirect_dma_start(
                out=ab_[j][:, :], out_offset=None, in_=yc_d[:, :],
                in_offset=bass.IndirectOffsetOnAxis(ap=di[:, i:i + 1], axis=0)),
                ycd_tokens + ["d1i", "d2i"], ["%s%d" % (nm, j)], dma_key="%s%d" % (nm, j))
        k.stt(xs[j][:, :], a1[j][:, :], w1[:, i:i + 1], xs[j][:, :], ALU.mult, ALU.add, ["a1%d" % j, "xs%d" % j, "w1"], ["xs%d" % j])
        k.stt(xs[j][:, :], a2[j][:, :], w2[:, i:i + 1], xs[j][:, :], ALU.mult, ALU.add, ["a2%d" % j, "xs%d" % j, "w2"], ["xs%d" % j])
        k.dma("sp", y_dram[i * 128:(i + 1) * 128, :], xs[j][:, :], ["xs%d" % j], [("yfin", i)], "yfin")
    s.add("sp", lambda e: e.nop(), [("yfin", i) for i in range(NS)], ())
    m.release(mkB)


def build_moe():
    nc = bass.Bass("TRN2", target_bir_lowering=False)
    x = nc.dram_tensor("x", [SEQH, D], F32, kind="ExternalInput").ap()
    cst = nc.dram_tensor("cst", [128, CONST_W], F32, kind="ExternalInput").ap()
    par = nc.dram_tensor("par", [128, 8], F32, kind="ExternalInput").ap()
    wr = nc.dram_tensor("wr", [D, N_EXPERTS], F32, kind="ExternalInput").ap()
    wg = nc.dram_tensor("wg", [N_EXPERTS, D, EXPERT_DIM], F32, kind="ExternalInput").ap()
    wu = nc.dram_tensor("wu", [N_EXPERTS, D, EXPERT_DIM], F32, kind="ExternalInput").ap()
    wd = nc.dram_tensor("wd", [N_EXPERTS, EXPERT_DIM, D], F32, kind="ExternalInput").ap()
    y = nc.dram_tensor("y", [SEQH, D], F32, kind="ExternalOutput").ap()
    hc_d = nc.dram_tensor("hcd", [N_EXPERTS * CAP, D], BF16).ap()
    yc_d = nc.dram_tensor("ycd", [N_EXPERTS * CAP, D], F32).ap()
    cnt_d = nc.dram_tensor("cntd", [1, 8], I32).ap()
    k = K(nc)
    with k.st:
        m = k.m
        k.setup_consts(cst)
        xT_off = m.off
        xT = m.alloc("xT", [128, NCH, SEQH], F32)
        g = m.alloc("g", [128, 8], F32)
        g32 = m.alloc("g32", [128, 8], F32)
        zt = m.alloc("zt", [128, D], BF16)
        k.dma("sp", g[:], par[:, :], (), ["graw"], "par")
        k.ts(g32[:], g[:], 32.0, None, ALU.mult, None, ["graw"], ["par"])
        k.load_xT(x, xT, "xT")
        hc_zero_fill(k, zt, hc_d)
        moe_sparse(k, xT, "xT", xT_off, g32, wr, wg, wu, wd, y, hc_d, yc_d, cnt_d)
        k.s.emit()
    return nc


def run_moe(x_shards, ln2_g1, wr, wg, wu, wd, trace=False):
    if "moe" not in _cache:
        _cache["moe"] = build_moe()
    nc = _cache["moe"]
    cst = make_consts()
    par = pack_pp(ln2_g1)
    in_maps = [{"x": np.ascontiguousarray(xs), "cst": cst, "par": par, "wr": wr, "wg": wg, "wu": wu, "wd": wd}
               for xs in x_shards]
    res = run_bass_kernel_spmd(nc, in_maps, core_ids=list(range(8)), trace=trace)
    return [r["y"] for r in res.results], res


PA_W = 24 + 256


def pack_par_attn(inp, layer, flag):
    p = np.zeros((128, PA_W), np.float32)
    p[:, 0:8] = pack_pp(inp["ln1_g"][layer])
    p[:, 8:16] = pack_pp(inp["mem_norm_g"])
    p[:, 16] = np.tile(np.asarray(inp["da_qn_g"][0], np.float32), 2)
    p[:, 17] = np.tile(np.asarray(inp["da_kn_g"][0], np.float32), 2)
    p[:, 18] = np.tile(np.asarray(inp["mem_qn_g"][layer], np.float32), 2)
    p[:, 19] = np.tile(np.asarray(inp["mem_kn_g"][layer], np.float32), 2)
    p[:, 20] = np.asarray(inp["da_sub_g"][0], np.float32)
    p[:, 21] = flag
    p[:, 22] = NEG if flag == 0 else 0.0
    lv = np.concatenate([np.asarray(inp[n][0], np.float32) for n in ("da_lq1", "da_lk1", "da_lq2", "da_lk2")])
    p[:, 24:24 + 256] = lv[None, :]
    return p


def headnorm(k, raw, P, n, gain, ones_bf, out, reads, writes, tmp):
    sq, ksq, rs, krs, pb, kpb = tmp["sq"], tmp["ksq"], tmp["rs"], tmp["krs"], tmp["pb"], tmp["kpb"]
    k.act(sq[0:P, 0:n], raw, AF.Square, reads, [ksq])
    k.mm(pb[0:P, 0:n], [(ones_bf, sq[0:P, 0:n])], [ksq, "cbf"], [kpb])
    k.act(rs[0:P, 0:n], pb[0:P, 0:n], AF.Ln, [kpb, "epsc"], [krs], bias=k.eps_ap(64 * EPS)[0:P, :])
    k.act(rs[0:P, 0:n], rs[0:P, 0:n], AF.Exp, [krs], [krs], scale=-0.5)
    k.stt(out, raw, gain, rs[0:P, 0:n], ALU.mult, ALU.mult, list(reads) + [krs, "gains"], writes)


def mem_kv_setup(k, mem_d, wkv_d, gmem32, gmk8, KmT, Vm, lname):
    m = k.m
    mk = m.mark()
    memT = m.alloc("memT", [128, NCH, MEM_LEN], F32)
    k.load_xT(mem_d, memT, "memT" + lname, ntok=MEM_LEN)
    mnT = m.alloc("mnT", [128, NCH, MEM_LEN], BF16)
    sq = m.alloc("msq", [128, NCH, MEM_LEN], BF16)
    rstd = m.alloc("mrstd", [128, MEM_LEN], F32)
    pb = k.ps[6]
    rd = [("memT" + lname, c, i) for c in range(NCH) for i in range(2)]
    k.act(sq[:, :, :], memT[:, :, :], AF.Square, rd, ["msq"])
    k.mm(pb[:, 0:MEM_LEN], [(k.ones_b, sq[:, c, :]) for c in range(NCH)], ["msq", "cbf"], ["ps6"])
    k.act(rstd[:, :], pb[:, 0:MEM_LEN], AF.Ln, ["ps6", "epsc"], ["mrstd"], bias=k.eps_ap(D * EPS))
    k.act(rstd[:, :], rstd[:, :], AF.Exp, ["mrstd"], ["mrstd"], scale=-0.5)
    for c in range(NCH):
        k.stt(mnT[:, c, :], memT[:, c, :], gmem32[:, c:c + 1], rstd[:, :], ALU.mult, ALU.mult,
              rd + ["mrstd", "gains"], [("mnT", c)])
    wkv = m.alloc("wkv", [128, NCH, 512], BF16)
    k.dma("pool", wkv[:], wkv_d.rearrange("(c p) f -> p c f", p=128), (), ["wkv"], "wkv")
    tmp = dict(sq=m.alloc("hsq", [128, 512], BF16), ksq="hsq", rs=m.alloc("hrs", [128, 512], F32), krs="hrs",
               pb=k.ps[7], kpb="ps7")
    mn_rd = [("mnT", c) for c in range(NCH)]
    for hm in range(4):
        pr = k.ps[hm % 2]
        kpr = "ps%d" % (hm % 2)
        k.mm(pr[0:64, 0:MEM_LEN], [(wkv[:, c, hm * 64:(hm + 1) * 64], mnT[:, c, :]) for c in range(NCH)],
             ["wkv"] + mn_rd, [kpr])
        headnorm(k, pr[0:64, 0:MEM_LEN], 64, MEM_LEN, gmk8[0:64, :], k.ones_b[0:64, 0:64], KmT[0:64, hm, :],
                 [kpr], [("KmT" + lname, hm)], tmp)
    for mt in range(2):
        pr = k.ps[2 + mt]
        kpr = "ps%d" % (2 + mt)
        k.mm(pr[:, 0:256], [(mnT[:, c, mt * 128:(mt + 1) * 128], wkv[:, c, 256:512]) for c in range(NCH)],
             ["wkv"] + mn_rd, [kpr])
        k.copy(Vm[:, mt, :], pr[:, 0:256], [kpr], [("Vm" + lname, mt)], eng="act")
    m.release(mk)


def attn_inproj(k, x_tiles, xT_own, g32, win, gq8, gk8, gmq8, QT, mqT, kscr, vscr, ctx0):
    m = k.m
    mk = m.mark()
    xtmp = m.alloc("xtmp", [128, NCH, TT], F32) if any(o is None for (_x, o) in x_tiles) else None
    hT = m.alloc("hT", [128, NCH, TT], BF16)
    sq = m.alloc("sq", [128, NCH, TT], BF16)
    rstd = m.alloc("rstd", [128, TT], F32)
    tmps = [dict(sq=m.alloc("hsq", [128, 512], BF16), ksq="hsq%d" % j, rs=m.alloc("hrs", [128, 512], F32), krs="hrs%d" % j,
                 pb=k.ps[7 - j], kpb="ps%d" % (7 - j)) for j in range(2)]
    hn = [0]

    def tmp_next():
        hn[0] += 1
        return tmps[hn[0] % 2]
    kt_sb = [m.alloc("ktsb", [128, TT], BF16) for _ in range(2)]
    vt_sb = [m.alloc("vtsb", [128, TOK_W], BF16) for _ in range(2)]
    stg = [m.alloc("xstg", [128, D], F32) for _ in range(2)]
    cnt = 0
    vcnt = 0
    for ti, (xd, own) in enumerate(x_tiles):
        g = ctx0 + ti
        if own is None:
            dst, dname, dtile, dsl = xtmp, "xtmp", 0, slice(0, TT)
        else:
            dst, dname, dtile, dsl = xT_own, "xT", own, slice(own * TT, (own + 1) * TT)
        for i in range(4):
            sb = stg[i % 2]
            tk = "xstg%d" % (i % 2)
            k.dma("sp", sb[:], xd[i * 128:(i + 1) * 128, :], (), [tk], tk)
            for c in range(NCH):
                pb = k.ps[(i * NCH + c) % 2]
                pk = "ps%d" % ((i * NCH + c) % 2)
                k.tr(pb[:, 0:128], sb[:, c * 128:(c + 1) * 128], k.ident, [tk, "cf32"], [pk])
                k.copy(dst[:, c, dsl.start + i * 128:dsl.start + (i + 1) * 128], pb[:, 0:128], [pk],
                       [(dname, c, dtile * 4 + i)], eng="act" if c % 2 == 0 else "dve")
        k.rmsnorm_T(dst, dname, g32, hT, "hT", dsl, dtile, sq, "sq", rstd, "rstd", htile=0)
        h_rd = [("hT", c, 0) for c in range(NCH)]
        for h in range(6):
            pr = k.ps[2 + cnt % 2]
            kpr = "ps%d" % (2 + cnt % 2)
            k.mm(pr[:, :], [(win[:, c, 768 + h * 128:768 + (h + 1) * 128], hT[:, c, :]) for c in range(NCH)],
                 ["win"] + h_rd, [kpr])
            kb = kt_sb[cnt % 2]
            kkb = "ktsb%d" % (cnt % 2)
            cnt += 1
            headnorm(k, pr[:, :], 128, TT, gk8, k.bd_b, kb[:, :], [kpr], [kkb], tmp_next())
            k.dma("sp", kscr[h, :, g * TT:(g + 1) * TT], kb[:, :], [kkb], [("kscr", h, g)], "kscr")
        for i in range(4):
            vb = vt_sb[vcnt % 2]
            kvb = "vtsb%d" % (vcnt % 2)
            vcnt += 1
            for (c0, cw, bank) in ((0, 512, 4), (512, 256, 5)):
                pr = k.ps[bank]
                kpr = "ps%d" % bank
                k.mm(pr[:, 0:cw], [(hT[:, c, i * 128:(i + 1) * 128], win[:, c, 1536 + c0:1536 + c0 + cw])
                                   for c in range(NCH)], ["win"] + h_rd, [kpr])
                k.copy(vb[:, c0:c0 + cw], pr[:, 0:cw], [kpr], [kvb], eng="act" if bank == 4 else "dve")
            k.dma("sp", vscr.rearrange("h p t e -> p h t e")[:, :, g * 4 + i, :],
                  vb[:, :].rearrange("p (h e) -> p h e", e=128), [kvb], [("vscr", g * 4 + i)], "vscr")
        if own is None:
            continue
        for h in range(6):
            pr = k.ps[2 + cnt % 2]
            kpr = "ps%d" % (2 + cnt % 2)
            cnt += 1
            k.mm(pr[:, :], [(win[:, c, h * 128:(h + 1) * 128], hT[:, c, :]) for c in range(NCH)], ["win"] + h_rd, [kpr])
            headnorm(k, pr[:, :], 128, TT, gq8, k.bd_b, QT[:, h, dsl], [kpr], [("QT", h, own)], tmp_next())
        for hm in range(4):
            pr = k.ps[2 + cnt % 2]
            kpr = "ps%d" % (2 + cnt % 2)
            cnt += 1
            k.mm(pr[0:64, :], [(win[:, c, 2304 + hm * 64:2304 + (hm + 1) * 64], hT[:, c, :]) for c in range(NCH)],
                 ["win"] + h_rd, [kpr])
            headnorm(k, pr[0:64, :], 64, TT, gmq8[0:64, :], k.ones_b[0:64, 0:64], mqT[0:64, hm, dsl], [kpr],
                     [("mqT", hm, own)], tmp_next())
    m.release(mk)


def attn_core(k, QT, tokT, kscr, vscr, neglam, subgs, flagb, nprev):
    m = k.m
    mk = m.mark()
    nctx = nprev + NTT
    nkt = nctx * 4
    kbuf = [m.alloc("kbuf", [128, nctx * TT], BF16) for _ in range(2)]
    vbuf = [m.alloc("vbuf", [128, nkt, 128], BF16) for _ in range(2)]
    pb = [m.alloc("pp", [128, 2, TT], BF16) for _ in range(2)]
    acc = m.alloc("acc", [128, 2, TT], F32)
    rs1 = m.alloc("rs1", [128, TT], F32)
    rs2 = m.alloc("rs2", [128, TT], F32)
    t1 = m.alloc("t1", [128, TT], F32)
    t2 = m.alloc("t2", [128, TT], F32)
    sqb = m.alloc("asq", [128, TT], BF16)
    rsn = m.alloc("rsn", [128, TT], F32)
    steps = []
    for h in range(6):
        for qt in range(NTT):
            kts = [(g, 0, True) for g in range(nprev * 4)] + [(nprev * 4 + j, 0, False) for j in range(qt * 4)] + \
                  [(nprev * 4 + qt * 4 + j, 128 * j, False) for j in range(4)]
            for idx, (g, n0, isprev) in enumerate(kts):
                steps.append((h, qt, idx, len(kts), g, n0, isprev))
    loaded = set()

    def load_head(h):
        if h in loaded or h >= 6:
            return
        loaded.add(h)
        kb, vb = kbuf[h % 2], vbuf[h % 2]
        kkb, kvb = "kbuf%d" % (h % 2), "vbuf%d" % (h % 2)
        k.dma("sp", kb[:, :], kscr[h, :, 0:nctx * TT], [("kscr", h, g) for g in range(nctx)], [kkb], kkb)
        k.dma("sp", vb[:, :, :], vscr[h, :, 0:nkt, :], [("vscr", g) for g in range(nkt)], [kvb], kvb)

    def S(i):
        h, qt, idx, nk, g, n0, isprev = steps[i]
        load_head(h)
        b = i % 2
        kb = kbuf[h % 2]
        kkb = "kbuf%d" % (h % 2)
        k.mm1(k.ps[2 * b][:, n0:TT], kb[0:64, g * 128:(g + 1) * 128], QT[0:64, h, qt * TT + n0:(qt + 1) * TT], True, True,
              [kkb, ("QT", h, qt)], ["ps%d" % (2 * b)])
        k.mm1(k.ps[2 * b + 1][:, n0:TT], kb[64:128, g * 128:(g + 1) * 128], QT[64:128, h, qt * TT + n0:(qt + 1) * TT], True, True,
              [kkb, ("QT", h, qt)], ["ps%d" % (2 * b + 1)])

    load_head(0)
    S(0)
    for i in range(len(steps)):
        h, qt, idx, nk, g, n0, isprev = steps[i]
        if i + 1 < len(steps):
            S(i + 1)
        if idx == 0 and qt == 0:
            load_head(h + 1)
        b = i % 2
        vb = vbuf[h % 2]
        kvb = "vbuf%d" % (h % 2)
        P = pb[b]
        kp = "pp%d" % b
        sview = k.psall[:, 2 * b * 512:(2 * b + 2) * 512].rearrange("p (a c) -> p a c", c=512)
        diag = n0 > 0 or (g >= nprev * 4 + qt * 4)
        bias = flagb if isprev else None
        rdb = ["gains"] if isprev else []
        k.act(P[:, :, n0:TT], sview[:, :, n0:TT], AF.Exp, ["ps%d" % (2 * b), "ps%d" % (2 * b + 1)] + rdb, [kp],
              bias=bias, scale=0.125)
        if diag:
            k.memset(P[64:128, :, n0:n0 + 64], 0.0, [kp], eng="pool")
        if idx == 0:
            k.copy(acc[:, 0, :], P[:, 0, :], [kp], ["acc0"], eng="dve")
            k.copy(acc[:, 1, :], P[:, 1, :], [kp], ["acc1"], eng="pool")
        else:
            k.tt(acc[:, 0, n0:TT], P[:, 0, n0:TT], acc[:, 0, n0:TT], ALU.add, [kp, "acc0"], ["acc0"], eng="dve")
            k.tt(acc[:, 1, n0:TT], P[:, 1, n0:TT], acc[:, 1, n0:TT], ALU.add, [kp, "acc1"], ["acc1"], eng="pool")
        k.mm1(k.ps[4][:, n0:TT], vb[:, g, :], P[:, 0, n0:TT], idx == 0, idx == nk - 1, [kvb, kp], ["ps4"])
        k.mm1(k.ps[5][:, n0:TT], vb[:, g, :], P[:, 1, n0:TT], idx == 0, idx == nk - 1, [kvb, kp], ["ps5"])
        if idx != nk - 1:
            continue
        qsl = slice(qt * TT, (qt + 1) * TT)
        k.mm(k.ps[6][:, :], [(k.ones_f, acc[:, 0, :])], ["acc0", "cf32"], ["ps6"])
        k.mm(k.ps[7][:, :], [(k.ones_f, acc[:, 1, :])], ["acc1", "cf32"], ["ps7"])
        k.s.add("dve", lambda e, o=rs1[:, :], i_=k.ps[6][:, :]: e.reciprocal(o, i_), ["ps6"], ["rs1"])
        k.s.add("dve", lambda e, o=rs2[:, :], i_=k.ps[7][:, :]: e.reciprocal(o, i_), ["ps7"], ["rs2"])
        k.tt(t1[:, :], k.ps[4][:, :], rs1[:, :], ALU.mult, ["ps4", "rs1"], ["t1"])
        k.tt(t2[:, :], k.ps[5][:, :], rs2[:, :], ALU.mult, ["ps5", "rs2"], ["t2"])
        k.stt(t1[:, :], t2[:, :], neglam, t1[:, :], ALU.mult, ALU.add, ["t1", "t2", "gains"], ["t1"])
        k.act(sqb[:, :], t1[:, :], AF.Square, ["t1"], ["asq"])
        k.mm(k.ps[6][:, :], [(k.ones_b, sqb[:, :])], ["asq", "cbf"], ["ps6"])
        k.act(rsn[:, :], k.ps[6][:, :], AF.Ln, ["ps6", "epsc"], ["rsn"], bias=k.eps_ap(128 * EPS))
        k.act(rsn[:, :], rsn[:, :], AF.Exp, ["rsn"], ["rsn"], scale=-0.5)
        k.stt(tokT[:, h, qsl], t1[:, :], subgs, rsn[:, :], ALU.mult, ALU.mult, ["t1", "rsn", "gains"],
              [("tokT", h, qt), ("QT", h, qt)])
    m.release(mk)


def mem_attn(k, mqT, memT, KmT, Vm, lname):
    m = k.m
    mk = m.mark()
    p1 = [m.alloc("pm", [128, TT], BF16) for _ in range(2)]
    rs1 = m.alloc("rsm", [128, TT], F32)
    it = 0
    for qt in range(NTT):
        qsl = slice(qt * TT, (qt + 1) * TT)
        for hm in range(4):
            for mt in range(2):
                b = it % 2
                it += 1
                s1 = k.ps[b]
                ks1 = "ps%d" % b
                P1 = p1[b]
                kp1 = "pm_%d" % b
                k.mm1(s1[:, :], KmT[0:64, hm, mt * 128:(mt + 1) * 128], mqT[0:64, hm, qsl], True, True,
                      [("KmT" + lname, hm), ("mqT", hm, qt)], [ks1])
                k.act(P1[:, :], s1[:, :], AF.Exp, [ks1], [kp1], scale=0.125)
                k.mm1(k.ps[4][0:64, :], Vm[:, mt, hm * 64:(hm + 1) * 64], P1[:, :], mt == 0, mt == 1,
                      [("Vm" + lname, mt), kp1], ["ps4"])
                k.mm1(k.ps[5][0:64, :], k.ones_b[:, 0:64], P1[:, :], mt == 0, mt == 1, ["cbf", kp1], ["ps5"])
            k.s.add("dve", lambda e, o=rs1[0:64, :], i=k.ps[5][0:64, :]: e.reciprocal(o, i), ["ps5"], ["rsm"])
            k.tt(memT[0:64, hm, qsl], k.ps[4][0:64, :], rs1[0:64, :], ALU.mult, ["ps4", "rsm"],
                 [("memT", hm, qt), ("mqT", hm, qt)])
    m.release(mk)


def attn_outproj(k, xT, xname, tokT, tokname, nk, memT, wo_d, lname):
    m = k.m
    mk = m.mark()
    wo = m.alloc("wo", [128, 6, D], BF16)
    wom = m.alloc("wom", [64, 4, D], BF16)
    k.dma("pool", wo[:], wo_d[0:768, :].rearrange("(c p) d -> p c d", p=128), (), ["wo"], "wo")
    k.dma("pool", wom[:], wo_d[768:1024, :].rearrange("(h p) d -> p h d", p=64), (), ["wom"], "wom")
    cnt = 0
    for qt in range(NTT):
        qsl = slice(qt * TT, (qt + 1) * TT)
        rd = [(tokname, kc, qt) for kc in range(nk)] + [("memT", hm, qt) for hm in range(4)]
        for ds in range(NCH):
            pb = k.ps[cnt % 2]
            kpb = "ps%d" % (cnt % 2)
            cnt += 1
            pairs = [(wo[:, kc, ds * 128:(ds + 1) * 128], tokT[:, kc, qsl]) for kc in range(nk)] + \
                    [(wom[0:64, hm, ds * 128:(ds + 1) * 128], memT[0:64, hm, qsl]) for hm in range(4)]
            k.mm(pb[:, :], pairs, ["wo", "wom"] + rd, [kpb])
            k.tt(xT[:, ds, qsl], pb[:, :], xT[:, ds, qsl], ALU.add, [kpb] + k.xtok(xname, qt, [ds]), k.xtok(xname, qt, [ds]))
    m.release(mk)


def attn_gains(k, par_d):
    m = k.m
    par = m.alloc("par", [128, PA_W], F32)
    k.dma("sp", par[:], par_d[:, :], (), ["parraw"], "par")
    gn = m.alloc("gains", [128, 32], F32)
    k.ts(gn[:, 0:16], par[:, 0:16], 32.0, None, ALU.mult, None, ["parraw"], ["par"])
    k.ts(gn[:, 16:20], par[:, 16:20], 8.0, None, ALU.mult, None, ["parraw"], ["gains"])
    lam_init = 0.8 - 0.6 * math.exp(-0.3 * 0)
    k.ts(gn[:, 20:21], par[:, 20:21], float(math.sqrt(128.0) * (1.0 - lam_init)), None, ALU.mult, None, ["parraw"], ["g20"])
    k.copy(gn[:, 21:23], par[:, 21:23], ["parraw"], ["g21"])
    pr = m.alloc("lprod", [128, 2, 64], F32)
    k.tt(pr[:, 0, :], par[:, 24:88], par[:, 88:152], ALU.mult, ["parraw"], ["lprod0"])
    k.tt(pr[:, 1, :], par[:, 152:216], par[:, 216:280], ALU.mult, ["parraw"], ["lprod1"])
    k.s.add("dve", lambda e: e.tensor_reduce(gn[:, 24:26], pr[:, :, :], AX.X, ALU.add), ["lprod0", "lprod1"], ["g24"])
    k.act(gn[:, 24:26], gn[:, 24:26], AF.Exp, ["g24"], ["g24"])
    k.tt(gn[:, 26:27], gn[:, 25:26], gn[:, 24:25], ALU.subtract, ["g24"], ["g26"])
    k.ts(gn[:, 27:28], gn[:, 26:27], float(-lam_init), None, ALU.add, None, ["g26"], ["g27"])
    k.copy(gn[:, 28:29], gn[:, 27:28], ["g27", "g20", "g21", "par", "gains"], ["gains"])
    return dict(g32=gn[:, 0:8], gmem32=gn[:, 8:16], gq8=gn[:, 16:17], gk8=gn[:, 17:18], gmq8=gn[:, 18:19],
                gmk8=gn[:, 19:20], subgs=gn[:, 20:21], flag=gn[:, 21:22], flagb=gn[:, 22:23], neglam=gn[:, 27:28])


def build_attn0():
    nc = bass.Bass("TRN2", target_bir_lowering=False)
    xo = nc.dram_tensor("xo", [SEQH, D], F32, kind="ExternalInput").ap()
    xp = nc.dram_tensor("xp", [SEQH, D], F32, kind="ExternalInput").ap()
    mem = nc.dram_tensor("mem", [MEM_LEN, D], F32, kind="ExternalInput").ap()
    cst = nc.dram_tensor("cst", [128, CONST_W], F32, kind="ExternalInput").ap()
    par = nc.dram_tensor("par", [128, PA_W], F32, kind="ExternalInput").ap()
    win_d = nc.dram_tensor("win", [D, 2560], F32, kind="ExternalInput").ap()
    wkv_d = nc.dram_tensor("wkv", [D, 512], F32, kind="ExternalInput").ap()
    wo_d = nc.dram_tensor("wo", [D, D], F32, kind="ExternalInput").ap()
    y = nc.dram_tensor("y", [SEQH, D], F32, kind="ExternalOutput").ap()
    kscr = nc.dram_tensor("kscr", [6, 128, 2 * SEQH], BF16).ap()
    vscr = nc.dram_tensor("vscr", [6, 128, 32, 128], BF16).ap()
    k = K(nc)
    with k.st:
        m = k.m
        k.setup_consts(cst)
        gd = attn_gains(k, par)
        xT = m.alloc("xT", [128, NCH, SEQH], F32)
        KmT = m.alloc("KmT", [64, 4, MEM_LEN], BF16)
        Vm = m.alloc("Vm", [128, 2, 256], BF16)
        mem_kv_setup(k, mem, wkv_d, gd["gmem32"], gd["gmk8"], KmT, Vm, "0")
        QT = m.alloc("QT", [128, 6, SEQH], BF16)
        mqT = m.alloc("mqT", [64, 4, SEQH], BF16)
        mk1 = m.mark()
        win = m.alloc("win", [128, NCH, 2560], BF16)
        winv = win_d.rearrange("(c p) f -> p c f", p=128)
        for j in range(5):
            k.dma("pool", win[:, :, j * 512:(j + 1) * 512], winv[:, :, j * 512:(j + 1) * 512], (), ["win"], "win")
        tiles = [(xp[i * TT:(i + 1) * TT, :], None) for i in range(NTT)] + [(xo[i * TT:(i + 1) * TT, :], i) for i in range(NTT)]
        attn_inproj(k, tiles, xT, gd["g32"], win, gd["gq8"], gd["gk8"], gd["gmq8"], QT, mqT, kscr, vscr, 0)
        m.release(mk1)
        tokT = m.alloc("tokT", [128, 6, SEQH], BF16)
        memT = m.alloc("memTo", [64, 4, SEQH], BF16)
        attn_core(k, QT, tokT, kscr, vscr, gd["neglam"], gd["subgs"], gd["flagb"], NTT)
        mem_attn(k, mqT, memT, KmT, Vm, "0")
        attn_outproj(k, xT, "xT", tokT, "tokT", 6, memT, wo_d, "0")
        k.store_xT(xT, y, "xT")
        k.s.emit()
    return nc


def run_attn0(inp, xo_shards, xp_shards, flags, mems, trace=False):
    if "attn0" not in _cache:
        _cache["attn0"] = build_attn0()
    nc = _cache["attn0"]
    cst = make_consts()
    in_maps = []
    for i in range(8):
        in_maps.append({"xo": np.ascontiguousarray(xo_shards[i]), "xp": np.ascontiguousarray(xp_shards[i]),
                        "mem": np.ascontiguousarray(mems[i]), "cst": cst, "par": pack_par_attn(inp, 0, flags[i]),
                        "win": np.asarray(inp["da_w_in"][0]), "wkv": np.asarray(inp["mem_w_kv"][0]),
                        "wo": np.asarray(inp["w_out"][0])})
    res = run_bass_kernel_spmd(nc, in_maps, core_ids=list(range(8)), trace=trace)
    return [r["y"] for r in res.results], res


PC_W = 880
TC = 256
NTC = SEQH // TC
SSM_IN = 2316


def pack_par_ssd(inp, flag):
    p = np.zeros((128, PC_W), np.float32)
    p[:, 0:8] = pack_pp(inp["ln1_g"][1])
    p[:, 8:16] = pack_pp(inp["mem_norm_g"])
    p[:, 16] = np.tile(np.asarray(inp["mem_qn_g"][1], np.float32), 2)
    p[:, 17] = np.tile(np.asarray(inp["mem_kn_g"][1], np.float32), 2)
    p[:, 18] = flag
    cw = np.asarray(inp["ssm_conv_w"][0], np.float32)
    p[:, 20:60] = cw.reshape(4, 10, 128).transpose(2, 1, 0).reshape(128, 40)
    p[:, 60:70] = np.asarray(inp["ssm_conv_b"][0], np.float32).reshape(10, 128).T
    p[:, 70:82] = np.asarray(inp["ssm_dt_bias"][0], np.float32)[None, :]
    p[:, 82:94] = np.asarray(inp["ssm_a_log"][0], np.float32)[None, :]
    p[:, 94:106] = np.asarray(inp["ssm_d"][0], np.float32)[None, :]
    p[:, 106:874] = np.asarray(inp["ssm_norm_g"][0], np.float32)[None, :]
    return p


def ssd_gains(k, par_d):
    m = k.m
    par = m.alloc("parc", [128, PC_W], F32)
    k.dma("sp", par[:], par_d[:, :], (), ["parraw"], "par")
    gn = m.alloc("gainc", [128, 48], F32)
    k.ts(gn[:, 0:16], par[:, 0:16], 32.0, None, ALU.mult, None, ["parraw"], ["par"])
    k.ts(gn[:, 16:18], par[:, 16:18], 8.0, None, ALU.mult, None, ["parraw"], ["gains"])
    k.act(gn[:, 20:32], par[:, 82:94], AF.Exp, ["parraw"], ["g20"])
    k.ts(gn[:, 20:32], gn[:, 20:32], -1.0, None, ALU.mult, None, ["g20"], ["g20"])
    k.copy(gn[:, 32:33], par[:, 18:19], ["parraw", "g20", "par", "gains"], ["gains"])
    return dict(g32=gn[:, 0:8], gmem32=gn[:, 8:16], gmq8=gn[:, 16:17], gmk8=gn[:, 17:18], a_bc=gn[:, 20:32],
                flag=gn[:, 32:33], cw=par[:, 20:60], cb=par[:, 60:70], dtb=par[:, 70:82], dsk=par[:, 94:106],
                ng=par[:, 106:874])


def ssd_pass(k, xT, xname, gd, win, own, H, Hbf, halo, tokT, mqT):
    m = k.m
    mk = m.mark()
    h_off = m.mark()
    dabc = m.alloc("dabc", [128, 12, 128], F32)
    hT = m.overlay("hT1", [128, NCH, TC], BF16, h_off)
    sq_off = m.mark()
    expE = m.alloc("expE", [128, 12, 128], F32)
    sq = m.overlay("sq1", [128, NCH, TC], BF16, sq_off)
    ytmp = m.overlay("ytmp", [128, TOK_W], F32, sq_off)
    rstd = m.alloc("rstd1", [128, TC], F32)
    raw = [m.alloc("raw", [128, TC + 3], F32) for _ in range(2)]
    cacc = [m.alloc("cacc", [128, TC], F32) for _ in range(2)]
    xbcT = m.alloc("xbcT", [128, 10, TC], BF16)
    dtt = m.alloc("dtt", [128, 2, 12], F32)
    dAt = m.alloc("dAt", [128, 2, 12], F32)
    xB = m.alloc("xB", [128, 1024], BF16)
    cs = m.alloc("cs", [128, 24], F32)
    ed = m.alloc("ed", [128, 24], F32)
    wend = m.alloc("wend", [128, 12], F32)
    xend = m.alloc("xend", [128, TOK_W], BF16)
    tmp = dict(sq=m.alloc("hsq", [128, 384], BF16), ksq="hsq", rs=m.alloc("hrs", [128, 384], F32), krs="hrs",
               pb=k.ps[7], kpb="ps7")
    if own:
        zs = m.alloc("zs", [128, 2, TOK_W], BF16)
        cb = m.alloc("cb", [128, 2, 128], F32)
        Wt = m.alloc("Wt", [128, 12, 128], BF16)
        xdt = m.alloc("xdt", [128, TOK_W], BF16)
        xD = m.alloc("xD", [128, TOK_W], BF16)
        yn = m.alloc("yn", [128, TOK_W], BF16)
        ss = m.alloc("ss", [128, 4], F32)
        junk = tmp["rs"]
    cw, cbias = gd["cw"], gd["cb"]
    ps = k.ps
    rc = 0
    for ti in range(NTC):
        tsl = slice(ti * TC, (ti + 1) * TC)
        xrd = [(xname, c, ti * 2 + j) for c in range(NCH) for j in range(2)]
        k.act(sq[:, :, :], xT[:, :, tsl], AF.Square, xrd, ["sq1", "ytmp"] + [("expE", q) for q in range(3)])
        k.mm(ps[6][:, 0:TC], [(k.ones_b, sq[:, c, :]) for c in range(NCH)], ["sq1", "cbf"], ["ps6"])
        k.act(rstd[:, :], ps[6][:, 0:TC], AF.Ln, ["ps6", "epsc"], ["rstd1"], bias=k.eps_ap(D * EPS))
        k.act(rstd[:, :], rstd[:, :], AF.Exp, ["rstd1"], ["rstd1"], scale=-0.5)
        for c in range(NCH):
            k.stt(hT[:, c, :], xT[:, c, tsl], gd["g32"][:, c:c + 1], rstd[:, :], ALU.mult, ALU.mult,
                  [(xname, c, ti * 2), (xname, c, ti * 2 + 1), "rstd1", "par"], [("hT1", c), "dabc"])
        h_rd = [("hT1", c) for c in range(NCH)]
        for cc in range(10):
            b = rc % 2
            rc += 1
            pr, kpr = ps[b], "ps%d" % b
            rw, krw = raw[b], "raw%d" % b
            ca, kca = cacc[b], "cacc%d" % b
            k.mm(pr[:, 0:TC], [(win[:, c, 768 + cc * 128:768 + (cc + 1) * 128], hT[:, c, :]) for c in range(NCH)],
                 ["win1"] + h_rd, [kpr])
            k.copy(rw[:, 3:TC + 3], pr[:, 0:TC], [kpr], [krw], eng="act")
            k.copy(rw[:, 0:3], halo[:, cc, :], [("halo", cc)], [krw], eng="pool")
            k.copy(halo[:, cc, :], rw[:, TC:TC + 3], [krw], [("halo", cc)], eng="pool")
            k.ts(ca[:, :], rw[:, 0:TC], cw[:, cc * 4:cc * 4 + 1], cbias[:, cc:cc + 1], ALU.mult, ALU.add,
                 [krw, "parraw"], [kca])
            for j in range(1, 4):
                k.stt(ca[:, :], rw[:, j:j + TC], cw[:, cc * 4 + j:cc * 4 + j + 1], ca[:, :], ALU.mult, ALU.add,
                      [krw, kca, "parraw"], [kca])
            k.act(xbcT[:, cc, :], ca[:, :], AF.Silu, [kca], [("xbcT", cc)])
        for j in range(2):
            k.mm(ps[2][:, j * 12:(j + 1) * 12], [(hT[:, c, j * 128:(j + 1) * 128], win[:, c, 2048:2060]) for c in range(NCH)],
                 ["win1"] + h_rd, ["ps2"])
        k.tt(dtt[:, :, :], ps[2][:, 0:24].rearrange("p (a b) -> p a b", b=12),
             gd["dtb"].unsqueeze(1).broadcast_to([128, 2, 12]), ALU.add, ["ps2", "parraw"], ["dtt"])
        k.act(dtt[:, :, :], dtt[:, :, :], AF.Exp, ["dtt"], ["dtt"])
        k.act(dtt[:, :, :], dtt[:, :, :], AF.Ln, ["dtt", "epsc"], ["dtt"], bias=k.eps_ap(1.0))
        k.tt(dAt[:, :, :], dtt[:, :, :], gd["a_bc"].unsqueeze(1).broadcast_to([128, 2, 12]), ALU.mult, ["dtt", "gains"], ["dAt"])
        if own:
            for j in range(2):
                for (c0, cwid, bank) in ((0, 512, 3), (512, 256, 4)):
                    k.mm(ps[bank][:, 0:cwid], [(hT[:, c, j * 128:(j + 1) * 128], win[:, c, c0:c0 + cwid]) for c in range(NCH)],
                         ["win1"] + h_rd, ["ps%d" % bank])
                    k.act(zs[:, j, c0:c0 + cwid], ps[bank][:, 0:cwid], AF.Silu, ["ps%d" % bank], [("zs", j)])
            for hm in range(4):
                k.mm(ps[5][0:64, 0:TC], [(win[:, c, 2060 + hm * 64:2060 + (hm + 1) * 64], hT[:, c, :]) for c in range(NCH)],
                     ["win1"] + h_rd, ["ps5"])
                headnorm(k, ps[5][0:64, 0:TC], 64, TC, gd["gmq8"][0:64, :], k.ones_b[0:64, 0:64], mqT[0:64, hm, tsl],
                         ["ps5"], [("mqT", hm, ti // 2)], tmp)
        for w in range(2):
            wsl = slice(w * 128, (w + 1) * 128)
            gw = ti * 2 + w
            pbf = ps[0].bitcast(BF16)
            for cc in range(8):
                k.tr(pbf[:, cc * 128:(cc + 1) * 128], xbcT[:, cc, wsl], k.ident_b, [("xbcT", cc), "cbf"], ["ps0"])
            k.copy(xB[:, :], pbf[:, 0:1024], ["ps0"], ["xB"], eng="act")
            k.mm(ps[1][:, 0:12], [(k.utri, dAt[:, w, :])], ["dAt", "cf32"], ["ps1"])
            k.mm(ps[1][:, 12:24], [(k.ones_f, dAt[:, w, :])], ["dAt", "cf32"], ["ps1"])
            k.copy(cs[:, :], ps[1][:, 0:24], ["ps1"], ["cs"], eng="dve")
            k.act(ed[:, :], ps[1][:, 0:24], AF.Exp, ["ps1"], ["ed"])
            k.tt(wend[:, :], cs[:, 12:24], cs[:, 0:12], ALU.subtract, ["cs"], ["wend"])
            k.act(wend[:, :], wend[:, :], AF.Exp, ["wend"], ["wend"])
            k.tt(wend[:, :], wend[:, :], dtt[:, w, :], ALU.mult, ["wend", "dtt"], ["wend"])
            x3 = xB[:, 0:TOK_W].rearrange("p (h d) -> p h d", d=64)
            k.tt(xend[:, :].rearrange("p (h d) -> p h d", d=64), x3, wend[:, :].unsqueeze(2).broadcast_to([128, 12, 64]),
                 ALU.mult, ["xB", "wend"], ["xend"])
            if own:
                k.tt(xdt[:, :].rearrange("p (h d) -> p h d", d=64), x3, dtt[:, w, :].unsqueeze(2).broadcast_to([128, 12, 64]),
                     ALU.mult, ["xB", "dtt"], ["xdt"], eng="dve")
                k.tt(xD[:, :].rearrange("p (h d) -> p h d", d=64), x3, gd["dsk"].unsqueeze(2).broadcast_to([128, 12, 64]),
                     ALU.mult, ["xB", "parraw"], ["xD"], eng="dve")
                k.copy(dabc[:, :, :], dAt[:, w, :].unsqueeze(2).broadcast_to([128, 12, 128]), ["dAt"], ["dabc"] + [("hT1", c) for c in range(NCH)], eng="pool")
                for h in range(12):
                    bank = 2 + h // 4
                    o = ps[bank][:, (h % 4) * 128:(h % 4 + 1) * 128]
                    kb = "ps%d" % bank
                    k.mm1(o, dabc[:, h, :], k.utri, True, False, ["dabc", "cf32"], [kb])
                    k.mm1(o, k.negutri, dabc[:, h, :], False, False, ["dabc", "cf32"], [kb])
                    k.mm1(o, k.ident, k.maskneg, False, True, ["cf32"], [kb])
                for q in range(3):
                    k.act(expE[:, q * 4:(q + 1) * 4, :].rearrange("p a b -> p (a b)"), ps[2 + q][:, :], AF.Exp,
                          ["ps%d" % (2 + q), "sq1", "ytmp"], [("expE", q)])
                for g in range(2):
                    k.mm1(ps[1][:, 256 + g * 128:256 + (g + 1) * 128], xbcT[:, 6 + g, wsl], xbcT[:, 8 + g, wsl], True, True,
                          [("xbcT", 6 + g), ("xbcT", 8 + g)], ["ps1"])
                k.copy(cb[:, :, :].rearrange("p a b -> p (a b)"), ps[1][:, 256:512], ["ps1"], ["cb"], eng="dve")
                for g in range(2):
                    k.tt(Wt[:, 6 * g:6 * g + 6, :], expE[:, 6 * g:6 * g + 6, :], cb[:, g, :].unsqueeze(1).broadcast_to([128, 6, 128]),
                         ALU.mult, [("expE", q) for q in range(3)] + ["cb"], [("Wt", g)])
                k.mm1(ps[2][:, 0:512], k.ident_b, xD[:, 0:512], True, False, ["xD", "cbf"], ["ps2"])
                for h in range(8):
                    k.mm1(ps[2][:, h * 64:(h + 1) * 64], Wt[:, h, :], xdt[:, h * 64:(h + 1) * 64], False, h == 7,
                          [("Wt", h // 6), "xdt"], ["ps2"])
                k.mm1(ps[3][:, 0:256], k.ident_b, xD[:, 512:768], True, False, ["xD", "cbf"], ["ps3"])
                for h in range(8, 12):
                    k.mm1(ps[3][:, (h - 8) * 64:(h - 7) * 64], Wt[:, h, :], xdt[:, h * 64:(h + 1) * 64], False, h == 11,
                          [("Wt", h // 6), "xdt"], ["ps3"])
                for g in range(2):
                    k.mm1(ps[5 + g][:, 0:384], xbcT[:, 8 + g, wsl], Hbf[:, g * 384:(g + 1) * 384], True, True,
                          [("xbcT", 8 + g), "Hbf"], ["ps%d" % (5 + g)])
                for g in range(2):
                    k.tt(ytmp[:, g * 384:(g + 1) * 384].rearrange("p (h d) -> p h d", d=64),
                         ps[5 + g][:, 0:384].rearrange("p (h d) -> p h d", d=64),
                         ed[:, 6 * g:6 * g + 6].unsqueeze(2).broadcast_to([128, 6, 64]), ALU.mult, ["ps%d" % (5 + g), "ed"], ["ytmp", "sq1"] + [("expE", q) for q in range(3)])
                k.tt(ytmp[:, 0:512], ps[2][:, 0:512], ytmp[:, 0:512], ALU.add, ["ps2", "ytmp"], ["ytmp"])
                k.tt(ytmp[:, 512:768], ps[3][:, 0:256], ytmp[:, 512:768], ALU.add, ["ps3", "ytmp"], ["ytmp"])
                k.tt(ytmp[:, :], ytmp[:, :], zs[:, w, :], ALU.mult, ["ytmp", ("zs", w)], ["ytmp"])
                for g in range(2):
                    k.act(junk[:, :], ytmp[:, g * 384:(g + 1) * 384], AF.Square, ["ytmp"], ["hrs", ("ss", g)],
                          accum_out=ss[:, g:g + 1])
                k.act(ss[:, 2:4], ss[:, 0:2], AF.Ln, [("ss", 0), ("ss", 1), "epsc"], ["ss2"], bias=k.eps_ap(EPS), scale=1.0 / 384.0)
                k.act(ss[:, 2:4], ss[:, 2:4], AF.Exp, ["ss2"], ["ss2"], scale=-0.5)
                for g in range(2):
                    k.stt(yn[:, g * 384:(g + 1) * 384], ytmp[:, g * 384:(g + 1) * 384], ss[:, 2 + g:3 + g],
                          gd["ng"][:, g * 384:(g + 1) * 384], ALU.mult, ALU.mult, ["ytmp", "ss2", "parraw"], [("yn", g)])
                pbf4 = ps[4].bitcast(BF16)
                for kc in range(6):
                    k.tr(pbf4[:, kc * 128:(kc + 1) * 128], yn[:, kc * 128:(kc + 1) * 128], k.ident_b,
                         [("yn", kc // 3), "cbf"], ["ps4"])
                k.copy(tokT[:, :, gw * 128:(gw + 1) * 128], pbf4[:, 0:768].rearrange("p (a b) -> p a b", b=128), ["ps4"],
                       [("tokT1", kc, gw // 4) for kc in range(6)], eng="act")
            for g in range(2):
                k.mm1(ps[7 - g][:, 0:384], xB[:, 768 + g * 128:768 + (g + 1) * 128], xend[:, g * 384:(g + 1) * 384], True, True,
                      ["xB", "xend"], ["ps%d" % (7 - g)])
            for g in range(2):
                Hg = H[:, g * 384:(g + 1) * 384].rearrange("p (h d) -> p h d", d=64)
                k.tt(Hg, Hg, ed[:, 12 + 6 * g:12 + 6 * g + 6].unsqueeze(2).broadcast_to([128, 6, 64]), ALU.mult,
                     ["H", "ed", "Hbf"], ["H"])
                k.tt(H[:, g * 384:(g + 1) * 384], ps[7 - g][:, 0:384], H[:, g * 384:(g + 1) * 384], ALU.add,
                     ["ps%d" % (7 - g), "H"], ["H"])
            k.copy(Hbf[:, :], H[:, :], ["H"], ["Hbf"], eng="act")
    m.release(mk)


def build_ssd():
    nc = bass.Bass("TRN2", target_bir_lowering=False)
    xo = nc.dram_tensor("xo", [SEQH, D], F32, kind="ExternalInput").ap()
    xp = nc.dram_tensor("xp", [SEQH, D], F32, kind="ExternalInput").ap()
    mem = nc.dram_tensor("mem", [MEM_LEN, D], F32, kind="ExternalInput").ap()
    cst = nc.dram_tensor("cst", [128, CONST_W], F32, kind="ExternalInput").ap()
    par = nc.dram_tensor("par", [128, PC_W], F32, kind="ExternalInput").ap()
    win_d = nc.dram_tensor("win", [D, SSM_IN], F32, kind="ExternalInput").ap()
    wkv_d = nc.dram_tensor("wkv", [D, 512], F32, kind="ExternalInput").ap()
    wo_d = nc.dram_tensor("wo", [D, D], F32, kind="ExternalInput").ap()
    y = nc.dram_tensor("y", [SEQH, D], F32, kind="ExternalOutput").ap()
    k = K(nc)
    with k.st:
        m = k.m
        k.setup_consts(cst)
        gd = ssd_gains(k, par)
        xT = m.alloc("xT", [128, NCH, SEQH], F32)
        KmT = m.alloc("KmT", [64, 4, MEM_LEN], BF16)
        Vm = m.alloc("Vm", [128, 2, 256], BF16)
        mem_kv_setup(k, mem, wkv_d, gd["gmem32"], gd["gmk8"], KmT, Vm, "1")
        H = m.alloc("H", [128, TOK_W], F32)
        Hbf = m.alloc("Hbf", [128, TOK_W], BF16)
        halo = m.alloc("halo", [128, 10, 3], F32)
        k.memset(H[:, :], 0.0, ["H"], eng="dve")
        k.memset(Hbf[:, :], 0.0, ["Hbf"], eng="pool")
        k.memset(halo[:, :, :], 0.0, [("halo", cc) for cc in range(10)], eng="pool")
        mqT = m.alloc("mqT", [64, 4, SEQH], BF16)
        tokT = m.alloc("tokT1", [128, 6, SEQH], BF16)
        mk1 = m.mark()
        win = m.alloc("win1", [128, NCH, SSM_IN], BF16)
        winv = win_d.rearrange("(c p) f -> p c f", p=128)
        for (a, b) in ((0, 512), (512, 1024), (1024, 1536), (1536, 2048), (2048, SSM_IN)):
            k.dma("pool", win[:, :, a:b], winv[:, :, a:b], (), ["win1"], "win1")
        k.load_xT(xp, xT, "xT")
        ssd_pass(k, xT, "xT", gd, win, False, H, Hbf, halo, None, None)
        k.ts(H[:, :], H[:, :], gd["flag"], None, ALU.mult, None, ["H", "gains"], ["H"])
        k.copy(Hbf[:, :], H[:, :], ["H"], ["Hbf"], eng="pool")
        for cc in range(10):
            k.ts(halo[:, cc, :], halo[:, cc, :], gd["flag"], None, ALU.mult, None, [("halo", cc), "gains"], [("halo", cc)])
        k.s.barrier()
        k.load_xT(xo, xT, "xT")
        ssd_pass(k, xT, "xT", gd, win, True, H, Hbf, halo, tokT, mqT)
        m.release(mk1)
        memT = m.alloc("memTo", [64, 4, SEQH], BF16)
        mem_attn(k, mqT, memT, KmT, Vm, "1")
        attn_outproj(k, xT, "xT", tokT, "tokT1", 6, memT, wo_d, "1")
        k.store_xT(xT, y, "xT")
        k.s.emit()
    return nc


def run_ssd(inp, xo_shards, xp_shards, flags, mems, trace=False):
    if "ssd" not in _cache:
        _cache["ssd"] = build_ssd()
    nc = _cache["ssd"]
    cst = make_consts()
    in_maps = []
    for i in range(8):
        in_maps.append({"xo": np.ascontiguousarray(xo_shards[i]), "xp": np.ascontiguousarray(xp_shards[i]),
                        "mem": np.ascontiguousarray(mems[i]), "cst": cst, "par": pack_par_ssd(inp, flags[i]),
                        "win": np.asarray(inp["ssm_w_in"][0]), "wkv": np.asarray(inp["mem_w_kv"][1]),
                        "wo": np.asarray(inp["w_out"][1])})
    res = run_bass_kernel_spmd(nc, in_maps, core_ids=list(range(8)), trace=trace)
    return [r["y"] for r in res.results], res


def build_fused():
    nc = bass.Bass("TRN2", target_bir_lowering=False)
    dt = lambda name, shape: nc.dram_tensor(name, shape, F32, kind="ExternalInput").ap()
    xo = dt("xo", [SEQH, D])
    xp = dt("xp", [SEQH, D])
    mem = dt("mem", [MEM_LEN, D])
    cst = dt("cst", [128, CONST_W])
    parA = dt("parA", [128, PA_W])
    parC = dt("parC", [128, PC_W])
    parF = dt("parF", [128, 16])
    win0_d = dt("win0", [D, 2560])
    win1_d = dt("win1", [D, SSM_IN])
    wkv0_d = dt("wkv0", [D, 512])
    wkv1_d = dt("wkv1", [D, 512])
    wo0_d = dt("wo0", [D, D])
    wo1_d = dt("wo1", [D, D])
    fg = dt("fg", [D, FFN_DIM])
    fu = dt("fu", [D, FFN_DIM])
    fd = dt("fd", [FFN_DIM, D])
    wr = dt("wr", [D, N_EXPERTS])
    eg = dt("eg", [N_EXPERTS, D, EXPERT_DIM])
    eu = dt("eu", [N_EXPERTS, D, EXPERT_DIM])
    ed = dt("ed", [N_EXPERTS, EXPERT_DIM, D])
    y = nc.dram_tensor("y", [SEQH, D], F32, kind="ExternalOutput").ap()
    kscr = nc.dram_tensor("kscr", [6, 128, 2 * SEQH], BF16).ap()
    vscr = nc.dram_tensor("vscr", [6, 128, 32, 128], BF16).ap()
    hc_d = nc.dram_tensor("hcd", [N_EXPERTS * CAP, D], BF16).ap()
    yc_d = nc.dram_tensor("ycd", [N_EXPERTS * CAP, D], F32).ap()
    cnt_d = nc.dram_tensor("cntd", [1, 8], I32).ap()
    k = K(nc)
    with k.st:
        m = k.m
        k.setup_consts(cst)
        xT_off = m.off
        xT = m.alloc("xT", [128, NCH, SEQH], F32)
        gF = m.alloc("gF", [128, 16], F32)
        gF32 = m.alloc("gF32", [128, 16], F32)
        zt = m.alloc("zt", [128, D], BF16)
        k.dma("sp", gF[:], parF[:, :], (), ["gFraw"], "parF")
        k.ts(gF32[:], gF[:], 32.0, None, ALU.mult, None, ["gFraw"], ["par"])
        mk_layers = m.mark()
        gA = attn_gains(k, parA)
        gC = ssd_gains(k, parC)
        KmT0 = m.alloc("KmT0", [64, 4, MEM_LEN], BF16)
        Vm0 = m.alloc("Vm0", [128, 2, 256], BF16)
        KmT1 = m.alloc("KmT1", [64, 4, MEM_LEN], BF16)
        Vm1 = m.alloc("Vm1", [128, 2, 256], BF16)
        mem_kv_setup(k, mem, wkv0_d, gA["gmem32"], gA["gmk8"], KmT0, Vm0, "0")
        mem_kv_setup(k, mem, wkv1_d, gC["gmem32"], gC["gmk8"], KmT1, Vm1, "1")
        H = m.alloc("H", [128, TOK_W], F32)
        Hbf = m.alloc("Hbf", [128, TOK_W], BF16)
        halo = m.alloc("halo", [128, 10, 3], F32)
        k.memset(H[:, :], 0.0, ["H"], eng="dve")
        k.memset(Hbf[:, :], 0.0, ["Hbf"], eng="pool")
        k.memset(halo[:, :, :], 0.0, [("halo", cc) for cc in range(10)], eng="pool")

        def layer0(xd, nprev):
            mk = m.mark()
            QT = m.alloc("QT", [128, 6, SEQH], BF16)
            mqT = m.alloc("mqT", [64, 4, SEQH], BF16)
            mk1 = m.mark()
            win = m.alloc("win", [128, NCH, 2560], BF16)
            winv = win0_d.rearrange("(c p) f -> p c f", p=128)
            for j in range(5):
                k.dma("pool", win[:, :, j * 512:(j + 1) * 512], winv[:, :, j * 512:(j + 1) * 512], (), ["win"], "win")
            tiles = [(xd[i * TT:(i + 1) * TT, :], i) for i in range(NTT)]
            attn_inproj(k, tiles, xT, gA["g32"], win, gA["gq8"], gA["gk8"], gA["gmq8"], QT, mqT, kscr, vscr, nprev)
            m.release(mk1)
            mem_attn(k, mqT, mqT, KmT0, Vm0, "0")
            attn_core(k, QT, QT, kscr, vscr, gA["neglam"], gA["subgs"], gA["flagb"], nprev)
            attn_outproj(k, xT, "xT", QT, "tokT", 6, mqT, wo0_d, "0")
            m.release(mk)
            mk = m.mark()
            h2T = m.alloc("h2T", [128, NCH, SEQH], BF16)
            mk2 = m.mark()
            sq = m.alloc("sq", [128, NCH, TT], BF16)
            rstd = m.alloc("rstd", [128, TT], F32)
            for tt in range(NTT):
                k.rmsnorm_T(xT, "xT", gF32[:, 0:8], h2T, "h2T", slice(tt * TT, (tt + 1) * TT), tt, sq, "sq", rstd, "rstd")
            m.release(mk2)
            k.ffn_bufs()
            k.ffn(xT, "xT", h2T, "h2T", fg, fu, fd, FFN_DIM)
            m.release(mk)

        layer0(xp, 0)
        hc_zero_fill(k, zt, hc_d)
        mk = m.mark()
        win1 = m.alloc("win1", [128, NCH, SSM_IN], BF16)
        win1v = win1_d.rearrange("(c p) f -> p c f", p=128)
        for (a, b) in ((768, 1280), (1280, 1792), (1792, 2060)):
            k.dma("pool", win1[:, :, a:b], win1v[:, :, a:b], (), ["win1"], "win1")
        ssd_pass(k, xT, "xT", gC, win1, False, H, Hbf, halo, None, None)
        m.release(mk)
        k.ts(H[:, :], H[:, :], gC["flag"], None, ALU.mult, None, ["H", "gains"], ["H"])
        k.copy(Hbf[:, :], H[:, :], ["H"], ["Hbf"], eng="pool")
        for cc in range(10):
            k.ts(halo[:, cc, :], halo[:, cc, :], gC["flag"], None, ALU.mult, None, [("halo", cc), "gains"], [("halo", cc)])
        k.s.barrier()
        layer0(xo, NTT)
        mk = m.mark()
        mqT = m.alloc("mqT", [64, 4, SEQH], BF16)
        tokT = m.alloc("tokT1", [128, 6, SEQH], BF16)
        mk1 = m.mark()
        win1 = m.alloc("win1", [128, NCH, SSM_IN], BF16)
        for (a, b) in ((0, 512), (512, 1024), (1024, 1536), (1536, 2048), (2048, SSM_IN)):
            k.dma("pool", win1[:, :, a:b], win1v[:, :, a:b], (), ["win1"], "win1")
        ssd_pass(k, xT, "xT", gC, win1, True, H, Hbf, halo, tokT, mqT)
        m.release(mk1)
        mem_attn(k, mqT, mqT, KmT1, Vm1, "1")
        attn_outproj(k, xT, "xT", tokT, "tokT1", 6, mqT, wo1_d, "1")
        m.release(mk)
        m.release(mk_layers)
        moe_sparse(k, xT, "xT", xT_off, gF32[:, 8:16], wr, eg, eu, ed, y, hc_d, yc_d, cnt_d)
        k.s.emit()
    return nc


def kernel(**inputs):
    inp = {k: np.asarray(v) for k, v in inputs.items()}
    x = inp["x"].astype(np.float32, copy=False)
    ncore = 8
    if "fused" not in _cache:
        _cache["fused"] = build_fused()
    nc = _cache["fused"]
    cst = make_consts()
    parF = np.concatenate([pack_pp(inp["ln2_g"][0]), pack_pp(inp["ln2_g"][1])], axis=1)
    in_maps = []
    for i in range(ncore):
        b, hf = i // 2, i % 2
        in_maps.append({
            "xo": np.ascontiguousarray(x[b, hf * SEQH:(hf + 1) * SEQH]),
            "xp": np.ascontiguousarray(x[b, 0:SEQH]),
            "mem": np.ascontiguousarray(inp["mem"][b]),
            "cst": cst, "parA": pack_par_attn(inp, 0, float(hf)), "parC": pack_par_ssd(inp, float(hf)), "parF": parF,
            "win0": inp["da_w_in"][0], "win1": inp["ssm_w_in"][0], "wkv0": inp["mem_w_kv"][0], "wkv1": inp["mem_w_kv"][1],
            "wo0": inp["w_out"][0], "wo1": inp["w_out"][1],
            "fg": inp["ffn_w_gate"][0], "fu": inp["ffn_w_up"][0], "fd": inp["ffn_w_down"][0],
            "wr": inp["moe_w_router"][0], "eg": inp["moe_w_gate"][0], "eu": inp["moe_w_up"][0], "ed": inp["moe_w_down"][0],
        })
    res = run_bass_kernel_spmd(nc, in_maps, core_ids=list(range(ncore)))
    out = np.empty((4, 2 * SEQH, D), np.float32)
    for i in range(ncore):
        out[i // 2, (i % 2) * SEQH:(i % 2 + 1) * SEQH] = res.results[i]["y"]
    return out


def kernel_unfused(**inputs):
    inp = {k: np.asarray(v) for k, v in inputs.items()}
    x = inp["x"].astype(np.float32, copy=False)
    ncore = 8
    bs = [i // 2 for i in range(ncore)]
    hf = [i % 2 for i in range(ncore)]
    flags = [float(h) for h in hf]
    mems = [inp["mem"][b] for b in bs]
    xo = [x[bs[i], hf[i] * SEQH:(hf[i] + 1) * SEQH] for i in range(ncore)]
    xp = [x[bs[i], 0:SEQH] for i in range(ncore)]
    x1, _ = run_attn0(inp, xo, xp, flags, mems)
    x2 = run_ffn0(x1, inp["ln2_g"][0], inp["ffn_w_gate"][0], inp["ffn_w_up"][0], inp["ffn_w_down"][0])
    xp2 = [x2[2 * bs[i]] for i in range(ncore)]
    x3, _ = run_ssd(inp, x2, xp2, flags, mems)
    x4, _ = run_moe(x3, inp["ln2_g"][1], inp["moe_w_router"][0], inp["moe_w_gate"][0], inp["moe_w_up"][0],
                    inp["moe_w_down"][0])
    out = np.empty((4, 2 * SEQH, D), np.float32)
    for i in range(ncore):
        out[bs[i], hf[i] * SEQH:(hf[i] + 1) * SEQH] = x4[i]
    return out
```

```python
import math
from contextlib import ExitStack

import numpy as np
import concourse.bass as bass
import concourse.mybir as mybir
from concourse.bass_utils import run_bass_kernel_spmd

F32 = mybir.dt.float32
BF16 = mybir.dt.bfloat16
AF = mybir.ActivationFunctionType
ALU = mybir.AluOpType
AX = mybir.AxisListType

D = 1024
NCH = 8
SEQH = 2048
TT = 512
NTT = SEQH // TT
EPS = 1e-6
FFN_DIM = 2816
EXPERT_DIM = 3584
N_EXPERTS = 8
MEM_LEN = 256
TOK_W = 768
NEG = -30000.0


class _Op:
    __slots__ = ("eng", "fn", "deps", "eidx", "is_dma", "dkey", "dval", "signal", "tick", "region")


class Sched:
    ENGS = ("pe", "act", "dve", "pool", "sp")

    def __init__(self, nc):
        self.nc = nc
        self.eng_ops = {e: [] for e in self.ENGS}
        self.last_w = {}
        self.readers = {}
        self.dma_tot = {}
        self.last_dma = {}
        self.barrier_deps = []
        self.cur_region = None
        self.reg_keys = []
        self.cur_regs = {}

    def add(self, eng, fn, reads=(), writes=(), dma_key=None):
        op = _Op()
        op.eng = eng
        op.fn = fn
        op.is_dma = dma_key is not None
        op.dkey = dma_key
        op.signal = False
        op.tick = 0
        op.dval = 0
        op.eidx = len(self.eng_ops[eng])
        op.region = self.cur_region
        assert not (op.is_dma and op.region is not None and eng not in ("pool", "sp"))
        deps = {}

        def need(P, dval=None):
            if P is None or P is op:
                return
            if P.is_dma:
                deps[id(P)] = (P, self.dma_tot[P.dkey] if dval is None else dval)
                return
            if P.eng == eng:
                if eng == "pe":
                    return
                if (not op.is_dma) and (op.eidx - P.eidx) > 3:
                    cnt = 0
                    lst = self.eng_ops[eng]
                    for qi in range(len(lst) - 1, P.eidx, -1):
                        o = lst[qi]
                        if o.region is None or o.region == op.region:
                            cnt += 1
                            if cnt >= 3:
                                break
                    if cnt >= 3:
                        return
            deps[id(P)] = (P, 0)

        for (P, bval) in self.barrier_deps:
            need(P, bval)
        for t in reads:
            need(self.last_w.get(t))
        for t in writes:
            need(self.last_w.get(t))
            for r in self.readers.get(t, ()):
                need(r)
        for t in reads:
            self.readers.setdefault(t, []).append(op)
        for t in writes:
            self.last_w[t] = op
            self.readers[t] = []
        if op.is_dma:
            self.dma_tot[dma_key] = self.dma_tot.get(dma_key, 0) + 16
            op.dval = self.dma_tot[dma_key]
            self.last_dma[dma_key] = op
        op.deps = list(deps.values())
        self.eng_ops[eng].append(op)
        return op

    def barrier(self):
        deps = []
        for e in self.ENGS:
            for op in reversed(self.eng_ops[e]):
                if not op.is_dma:
                    deps.append((op, None))
                    break
        fz = getattr(self, "freeze_weights", False)
        deps.extend((P, self.dma_tot[P.dkey] if (fz and str(P.dkey)[:2] in ("wg", "wu", "wd")) else None)
                    for P in self.last_dma.values())
        self.barrier_deps = deps

    def emit(self):
        nc = self.nc
        for e in self.ENGS:
            for op in self.eng_ops[e]:
                for (P, _v) in op.deps:
                    if not P.is_dma:
                        P.signal = True
        for e in self.ENGS:
            c = 0
            for op in self.eng_ops[e]:
                if op.signal:
                    c += 1
                    op.tick = c
        with ExitStack() as st:
            esem = {e: st.enter_context(nc.semaphore("sem_" + e)) for e in self.ENGS}
            dsem = {k: st.enter_context(nc.semaphore("dsem_%d" % i)) for i, k in enumerate(self.dma_tot)}
            block = st.enter_context(nc.Block())

            def run(ename, eng):
                seen = {}
                regs = {}
                if ename in ("pe", "act", "dve", "pool", "sp"):
                    for rk in self.reg_keys:
                        regs[rk] = eng.alloc_register("r_%s_%s" % (ename, rk))
                self.cur_regs = regs

                def emit_op(op):
                    for (P, v) in op.deps:
                        if P.is_dma:
                            key = ("d", P.dkey)
                            sem = dsem[P.dkey]
                            val = v
                        else:
                            key = ("e", P.eng)
                            sem = esem[P.eng]
                            val = P.tick
                        if seen.get(key, 0) < val:
                            eng.wait_ge(sem, val)
                            seen[key] = val
                    inst = op.fn(eng)
                    if op.is_dma:
                        inst.then_inc(dsem[op.dkey], 16)
                    elif op.signal:
                        inst.then_inc(esem[ename], 1)

                ops = self.eng_ops[ename]
                i = 0
                tick_before = 0
                while i < len(ops):
                    reg = ops[i].region
                    j = i
                    while j < len(ops) and ops[j].region == reg:
                        j += 1
                    group = ops[i:j]
                    nsig = sum(1 for op in group if op.signal and not op.is_dma)
                    if reg is None:
                        for op in group:
                            emit_op(op)
                    else:
                        rk, thr = reg
                        with eng.If_lt(regs[rk], thr + 1):
                            if nsig:
                                if tick_before > 0:
                                    eng.wait_ge(esem[ename], tick_before)
                                eng.nop().then_inc(esem[ename], nsig)
                            else:
                                eng.nop()
                            dk = {}
                            for op in group:
                                if op.is_dma:
                                    first, n = dk.get(op.dkey, (op.dval - 16, 0))
                                    dk[op.dkey] = (first, n + 1)
                            for key, (first, n) in dk.items():
                                if first > 0:
                                    eng.wait_ge(dsem[key], first)
                                eng.nop().then_inc(dsem[key], 16 * n)
                        with eng.Else():
                            saved = dict(seen)
                            for op in group:
                                emit_op(op)
                            seen.clear()
                            seen.update(saved)
                    tick_before += nsig
                    i = j

            @block.tensor
            def _(eng):
                run("pe", eng)

            @block.scalar
            def _(eng):
                run("act", eng)

            @block.vector
            def _(eng):
                run("dve", eng)

            @block.gpsimd
            def _(eng):
                run("pool", eng)

            @block.sync
            def _(eng):
                run("sp", eng)


class Mem:
    def __init__(self, nc, sched):
        self.nc = nc
        self.s = sched
        self.off = 16512
        self.n = 0
        self.limit = 229376

    def alloc(self, name, shape, dtype):
        size = 1
        for d in shape[1:]:
            size *= d
        size *= 2 if dtype == BF16 else 4
        size = (size + 63) // 64 * 64
        self.n += 1
        t = self.nc.alloc_sbuf_tensor_at("%s_%d" % (name, self.n), list(shape), dtype, offset=self.off)
        self.off += size
        assert self.off <= self.limit, "SBUF overflow at %s: %d" % (name, self.off)
        return t

    def overlay(self, name, shape, dtype, off):
        self.n += 1
        return self.nc.alloc_sbuf_tensor_at("%s_%d" % (name, self.n), list(shape), dtype, offset=off)

    def mark(self):
        return self.off

    def release(self, mark):
        self.off = mark
        self.s.barrier()


class K:
    def __init__(self, nc):
        self.nc = nc
        self.s = Sched(nc)
        self.m = Mem(nc, self.s)
        self.st = ExitStack()
        self.psall = self.st.enter_context(nc.psum_tensor("psall", [128, 4096], F32))
        self.ps = [self.psall[:, i * 512:(i + 1) * 512] for i in range(8)]
        self.uid = 0

    def mm(self, out, pairs, reads, writes):
        n = len(pairs)

        def fn(eng, out=out, pairs=pairs, n=n):
            inst = None
            for i, (l, r) in enumerate(pairs):
                inst = eng.matmul(out, l, r, start=(i == 0), stop=(i == n - 1))
            return inst
        return self.s.add("pe", fn, reads, writes)

    def mm1(self, out, lhsT, rhs, start, stop, reads, writes):
        return self.s.add("pe", lambda eng, o=out, l=lhsT, r=rhs, a=start, b=stop: eng.matmul(o, l, r, start=a, stop=b),
                          reads, writes)

    def tr(self, out, in_, ident, reads, writes):
        return self.s.add("pe", lambda eng, o=out, i=in_, d=ident: eng.transpose(o, i, d), reads, writes)

    def act(self, out, in_, func, reads, writes, bias=None, scale=1.0, accum_out=None, eng="act"):
        def fn(e, out=out, in_=in_, func=func, bias=bias, scale=scale, accum_out=accum_out):
            kw = {}
            if bias is not None:
                kw["bias"] = bias
            if accum_out is not None:
                kw["accum_out"] = accum_out
            return e.activation(out=out, in_=in_, func=func, scale=scale, **kw)
        return self.s.add(eng, fn, reads, writes)

    def tt(self, out, in0, in1, op, reads, writes, eng="dve"):
        return self.s.add(eng, lambda e, o=out, a=in0, b=in1, p=op: e.tensor_tensor(o, a, b, p), reads, writes)

    def ts(self, out, in0, s1, s2, op0, op1, reads, writes, eng="dve"):
        def fn(e, o=out, a=in0, s1=s1, s2=s2, op0=op0, op1=op1):
            if op1 is None:
                return e.tensor_scalar(o, a, s1, None, op0)
            return e.tensor_scalar(o, a, s1, s2, op0, op1)
        return self.s.add(eng, fn, reads, writes)

    def stt(self, out, in0, scalar, in1, op0, op1, reads, writes, eng="dve"):
        return self.s.add(eng, lambda e, o=out, a=in0, sc=scalar, b=in1, p0=op0, p1=op1:
                          e.scalar_tensor_tensor(o, a, sc, b, p0, p1), reads, writes)

    def copy(self, out, in_, reads, writes, eng="dve"):
        if eng == "act":
            return self.s.add("act", lambda e, o=out, i=in_: e.copy(o, i), reads, writes)
        return self.s.add(eng, lambda e, o=out, i=in_: e.tensor_copy(o, i), reads, writes)

    def memset(self, ap, val, writes, eng="pool"):
        return self.s.add(eng, lambda e, a=ap, v=val: e.memset(a, v), (), writes)

    def dma(self, queue, out, in_, reads, writes, key):
        return self.s.add(queue, lambda e, o=out, i=in_: e.dma_start(out=o, in_=i), reads, writes, dma_key=key)

    def wdma(self, out, in_, tok, key):
        n = getattr(self, "_wn", 0)
        self._wn = n + 1
        reads = [("wdma", n - 2)] if n >= 2 else []
        return self.dma("pool", out, in_, reads, [tok, ("wdma", n)], key)

    @staticmethod
    def xtok(name, tile512, cs=range(NCH)):
        return [(name, c, tile512 * 4 + j) for c in cs for j in range(4)]

    def eps_ap(self, val):
        key = float(val)
        if key not in self.epsc:
            i = len(self.epsc)
            self.memset(self.epst[:, i:i + 1], key, ["epsc"], eng="dve")
            self.epsc[key] = self.epst[:, i:i + 1]
        return self.epsc[key]

    def setup_consts(self, cst):
        m = self.m
        self.c_f32 = m.alloc("cf32", [128, CONST_W], F32)
        self.dma("sp", self.c_f32[:], cst[:, :], (), ["cf32"], "cst")
        self.ident = self.c_f32[:, 0:128]
        self.ones_f = self.c_f32[:, 128:256]
        self.utri = self.c_f32[:, 256:384]
        self.maskneg = self.c_f32[:, 384:512]
        self.bd_f = self.c_f32[:, 512:640]
        self.sel = self.c_f32[0:8, 640:640 + 8 * 128]
        self.negutri = self.c_f32[:, 1664:1792]
        self.epst = m.alloc("epst", [128, 16], F32)
        self.epsc = {}
        self.c_bf = m.alloc("cbf", [128, 512], BF16)
        self.copy(self.c_bf[:, 0:128], self.ident, ["cf32"], ["cbf"])
        self.copy(self.c_bf[:, 128:256], self.ones_f, ["cf32"], ["cbf"])
        self.copy(self.c_bf[:, 256:384], self.bd_f, ["cf32"], ["cbf"])
        self.ident_b = self.c_bf[:, 0:128]
        self.ones_b = self.c_bf[:, 128:256]
        self.bd_b = self.c_bf[:, 256:384]
        self.copy(self.c_bf[:, 384:512], self.utri, ["cf32"], ["cbf"])
        self.utri_b = self.c_bf[:, 384:512]

    def load_xT(self, x_dram, xT, name, ntok=SEQH):
        m = self.m
        mk = m.mark()
        stg = [m.alloc("xstg", [128, D], F32) for _ in range(2)]
        for i in range(ntok // 128):
            sb = stg[i % 2]
            tk = "xstg%d" % (i % 2)
            self.dma("sp", sb[:], x_dram[i * 128:(i + 1) * 128, :], (), [tk], tk)
            for c in range(NCH):
                pb = self.ps[(i * NCH + c) % 2]
                pk = "ps%d" % ((i * NCH + c) % 2)
                self.tr(pb[:, 0:128], sb[:, c * 128:(c + 1) * 128], self.ident, [tk, "cf32"], [pk])
                eng = "act" if c % 2 == 0 else "dve"
                self.copy(xT[:, c, i * 128:(i + 1) * 128], pb[:, 0:128], [pk], [(name, c, i)], eng=eng)
        m.release(mk)

    def store_xT(self, xT, y_dram, name, ntok=SEQH, final=True):
        m = self.m
        mk = m.mark()
        stg = [m.alloc("ystg", [128, D], F32) for _ in range(2)]
        for i in range(ntok // 128):
            sb = stg[i % 2]
            tk = "ystg%d" % (i % 2)
            for c in range(NCH):
                pb = self.ps[(i * NCH + c) % 2]
                pk = "ps%d" % ((i * NCH + c) % 2)
                self.tr(pb[:, 0:128], xT[:, c, i * 128:(i + 1) * 128], self.ident, [(name, c, i), "cf32"], [pk])
                eng = "act" if c % 2 == 0 else "dve"
                self.copy(sb[:, c * 128:(c + 1) * 128], pb[:, 0:128], [pk], [tk], eng=eng)
            self.dma("sp", y_dram[i * 128:(i + 1) * 128, :], sb[:], [tk], [("yout", i)], "yout")
        if final:
            self.s.add("sp", lambda e: e.nop(), [("yout", i) for i in range(ntok // 128)], ())
        m.release(mk)

    def rmsnorm_T(self, xT, xname, g32, hT, hname, tsl, tile_id, sq, sqname, rstd, rname, ps_bank=6, htile=None):
        n = tsl.stop - tsl.start
        if htile is None:
            htile = tile_id
        pb = self.ps[ps_bank]
        pk = "ps%d" % ps_bank
        self.act(sq[:, :, 0:n], xT[:, :, tsl], AF.Square, self.xtok(xname, tile_id), [sqname])
        self.mm(pb[:, 0:n], [(self.ones_b, sq[:, c, 0:n]) for c in range(NCH)], [sqname, "cbf"], [pk])
        self.act(rstd[:, 0:n], pb[:, 0:n], AF.Ln, [pk, "epsc"], [rname], bias=self.eps_ap(D * EPS))
        self.act(rstd[:, 0:n], rstd[:, 0:n], AF.Exp, [rname], [rname], scale=-0.5)
        for c in range(NCH):
            self.stt(hT[:, c, 0:n] if hT.shape[2] == n else hT[:, c, tsl], xT[:, c, tsl], g32[:, c:c + 1], rstd[:, 0:n],
                     ALU.mult, ALU.mult, self.xtok(xname, tile_id, [c]) + [rname, "par"], [(hname, c, htile)],
                     eng="dve")

    def ffn_chunks(self, wg_d, wu_d, wd_d, F, gate_bc=None, gname=None, pre=None):
        FC = 512
        wgv = wg_d.rearrange("(c p) f -> p c f", p=128)
        wuv = wu_d.rearrange("(c p) f -> p c f", p=128)
        wdv = wd_d.rearrange("(s p) d -> p s d", p=128)
        out = []
        for fc in range((F + FC - 1) // FC):
            f0 = fc * FC
            fw = min(FC, F - f0)
            out.append(dict(wgv=wgv, wuv=wuv, wdv=wdv, f0=f0, fw=fw, gate_bc=gate_bc, gname=gname,
                            pre=pre if fc == 0 else None))
        return out

    def ffn_run(self, xT, xname, h2T, hname, chunks):
        nch = len(chunks)

        def load(ci):
            ch = chunks[ci]
            slot = ci % 2
            wg, wu, wd = self.wbuf[slot]
            kg, ku, kd = ("wg%d" % slot, "wu%d" % slot, "wd%d" % slot)
            f0, fw = ch["f0"], ch["fw"]
            nfs = fw // 128
            self.wdma(wg[:, :, 0:fw], ch["wgv"][:, :, f0:f0 + fw], kg, kg)
            self.wdma(wu[:, :, 0:fw], ch["wuv"][:, :, f0:f0 + fw], ku, ku)
            self.wdma(wd[:, 0:nfs, :], ch["wdv"][:, f0 // 128:f0 // 128 + nfs, :], kd, kd)

        units = [(ci, tt) for ci in range(nch) for tt in range(NTT)]

        def GU(ui):
            ci, tt = units[ui]
            ch = chunks[ci]
            if tt == 0 and ch["pre"] is not None:
                ch["pre"]()
            slot = ci % 2
            wg, wu, wd = self.wbuf[slot]
            kg, ku = "wg%d" % slot, "wu%d" % slot
            nfs = ch["fw"] // 128
            tsl = slice(tt * TT, (tt + 1) * TT)
            ab = ui % 2
            for fs in range(nfs):
                j = self.cnt % 2
                self.cnt += 1
                pg, pu = self.ps[j], self.ps[2 + j]
                kpg, kpu = "ps%d" % j, "ps%d" % (2 + j)
                hrd = [(hname, c, tt) for c in range(NCH)]
                self.mm(pg[:, :], [(wg[:, c, fs * 128:(fs + 1) * 128], h2T[:, c, tsl]) for c in range(NCH)], [kg] + hrd, [kpg])
                self.mm(pu[:, :], [(wu[:, c, fs * 128:(fs + 1) * 128], h2T[:, c, tsl]) for c in range(NCH)], [ku] + hrd, [kpu])
                sg = self.sgbuf[j]
                ksg = "sg%d" % j
                self.act(sg[:, :], pg[:, :], AF.Silu, [kpg], [ksg])
                if ch["gate_bc"] is not None:
                    self.tt(sg[:, :], sg[:, :], ch["gate_bc"][:, tsl], ALU.mult, [ksg, ch["gname"]], [ksg], eng="dve")
                self.tt(self.actbuf[ab][:, fs, :], pu[:, :], sg[:, :], ALU.mult, [kpu, ksg], [("actT", ab, fs)])

        def DN(ui):
            ci, tt = units[ui]
            ch = chunks[ci]
            slot = ci % 2
            wd = self.wbuf[slot][2]
            kd = "wd%d" % slot
            nfs = ch["fw"] // 128
            tsl = slice(tt * TT, (tt + 1) * TT)
            ab = ui % 2
            abuf = self.actbuf[ab]
            kab = [("actT", ab, fs) for fs in range(nfs)]
            for ds in range(NCH):
                j = self.cnt2 % 3
                self.cnt2 += 1
                pd = self.ps[4 + j]
                kpd = "ps%d" % (4 + j)
                self.mm(pd[:, :], [(wd[:, fs, ds * 128:(ds + 1) * 128], abuf[:, fs, :]) for fs in range(nfs)], [kd] + kab, [kpd])
                self.tt(xT[:, ds, tsl], pd[:, :], xT[:, ds, tsl], ALU.add, [kpd] + self.xtok(xname, tt, [ds]), self.xtok(xname, tt, [ds]))

        load(0)
        if nch > 1:
            load(1)
        GU(0)
        for ui in range(len(units)):
            if ui + 1 < len(units):
                GU(ui + 1)
            DN(ui)
            ci, tt = units[ui]
            if tt == NTT - 1 and ci + 2 < nch:
                load(ci + 2)

    def ffn(self, xT, xname, h2T, hname, wg_d, wu_d, wd_d, F, gate_bc=None, gname=None):
        self.ffn_run(xT, xname, h2T, hname, self.ffn_chunks(wg_d, wu_d, wd_d, F, gate_bc, gname))

    def ffn_bufs(self):
        m = self.m
        self.wbuf = [(m.alloc("wg", [128, NCH, 512], BF16), m.alloc("wu", [128, NCH, 512], BF16),
                      m.alloc("wd", [128, 4, D], BF16)) for _ in range(2)]
        self.sgbuf = [m.alloc("sg", [128, TT], F32) for _ in range(2)]
        self.actbuf = [m.alloc("actT", [128, 4, TT], BF16) for _ in range(2)]
        self.wslot = 0
        self.cnt = 0
        self.cnt2 = 0
        self.acnt = 0


CONST_W = 640 + 8 * 128 + 128


def make_consts():
    c = np.zeros((128, CONST_W), np.float32)
    c[:, 0:128] = np.eye(128, dtype=np.float32)
    c[:, 128:256] = 1.0
    r = np.arange(128)
    c[:, 256:384] = (r[:, None] <= r[None, :]).astype(np.float32)
    c[:, 384:512] = np.where(r[:, None] <= r[None, :], 0.0, NEG).astype(np.float32)
    bd = np.zeros((128, 128), np.float32)
    bd[:64, :64] = 1.0
    bd[64:, 64:] = 1.0
    c[:, 512:640] = bd
    for e in range(8):
        c[e, 640 + e * 128:640 + (e + 1) * 128] = 1.0
    c[:, 1664:1792] = -c[:, 256:384]
    return c


def pack_pp(v):
    return np.ascontiguousarray(np.asarray(v, np.float32).reshape(NCH, 128).T)


def build_ffn0():
    nc = bass.Bass("TRN2", target_bir_lowering=False)
    x = nc.dram_tensor("x", [SEQH, D], F32, kind="ExternalInput").ap()
    cst = nc.dram_tensor("cst", [128, CONST_W], F32, kind="ExternalInput").ap()
    par = nc.dram_tensor("par", [128, 8], F32, kind="ExternalInput").ap()
    wg = nc.dram_tensor("wg", [D, FFN_DIM], F32, kind="ExternalInput").ap()
    wu = nc.dram_tensor("wu", [D, FFN_DIM], F32, kind="ExternalInput").ap()
    wd = nc.dram_tensor("wd", [FFN_DIM, D], F32, kind="ExternalInput").ap()
    y = nc.dram_tensor("y", [SEQH, D], F32, kind="ExternalOutput").ap()
    k = K(nc)
    with k.st:
        m = k.m
        k.setup_consts(cst)
        xT = m.alloc("xT", [128, NCH, SEQH], F32)
        g = m.alloc("g", [128, 8], F32)
        g32 = m.alloc("g32", [128, 8], F32)
        k.dma("sp", g[:], par[:, :], (), ["graw"], "par")
        k.ts(g32[:], g[:], 32.0, None, ALU.mult, None, ["graw"], ["par"])
        k.load_xT(x, xT, "xT")
        h2T = m.alloc("h2T", [128, NCH, SEQH], BF16)
        mk = m.mark()
        sq = m.alloc("sq", [128, NCH, TT], BF16)
        rstd = m.alloc("rstd", [128, TT], F32)
        for tt in range(NTT):
            k.rmsnorm_T(xT, "xT", g32, h2T, "h2T", slice(tt * TT, (tt + 1) * TT), tt, sq, "sq", rstd, "rstd")
        m.release(mk)
        k.ffn_bufs()
        k.ffn(xT, "xT", h2T, "h2T", wg, wu, wd, FFN_DIM)
        k.store_xT(xT, y, "xT")
        k.s.emit()
    return nc


_cache = {}


def run_ffn0(x_shards, ln2_g0, wg, wu, wd):
    if "ffn0" not in _cache:
        _cache["ffn0"] = build_ffn0()
    nc = _cache["ffn0"]
    cst = make_consts()
    par = pack_pp(ln2_g0)
    in_maps = [{"x": np.ascontiguousarray(xs), "cst": cst, "par": par, "wg": wg, "wu": wu, "wd": wd} for xs in x_shards]
    res = run_bass_kernel_spmd(nc, in_maps, core_ids=list(range(8)))
    return [r["y"] for r in res.results]


def moe_block(k, xT, xname, h2T, hname, wr_d, wg_d, wu_d, wd_d):
    m = k.m
    NS = SEQH // 128
    wr = m.alloc("wr", [128, NCH, 8], BF16)
    k.dma("pool", wr[:], wr_d.rearrange("(c p) e -> p c e", p=128), (), ["wr"], "wr")
    lg = m.alloc("lg", [128, NS, 8], F32)
    pb = k.ps[7]
    for i in range(NS):
        k.mm(pb[:, i * 8:(i + 1) * 8], [(h2T[:, c, i * 128:(i + 1) * 128], wr[:, c, :]) for c in range(NCH)],
             ["wr"] + [(hname, c, i // 4) for c in range(NCH)], ["ps7"])
    k.copy(lg[:].rearrange("p a b -> p (a b)"), pb[:, 0:NS * 8], ["ps7"], ["lg"], eng="act")
    mk = m.mark()
    m1 = m.alloc("m1", [128, NS], F32)
    m2 = m.alloc("m2", [128, NS], F32)
    eq1 = m.alloc("eq1", [128, NS, 8], F32)
    eq2 = m.alloc("eq2", [128, NS, 8], F32)
    lg2 = m.alloc("lg2", [128, NS, 8], F32)
    w1 = m.alloc("w1", [128, NS], F32)
    w2 = m.alloc("w2", [128, NS], F32)
    gates = m.alloc("gates", [128, NS, 8], F32)

    def bc(t):
        return t[:, :].unsqueeze(2).broadcast_to([128, NS, 8])

    s = k.s
    s.add("dve", lambda e: e.tensor_reduce(m1[:, :], lg[:, :, :], AX.X, ALU.max), ["lg"], ["m1"])
    k.tt(eq1[:], lg[:], bc(m1), ALU.is_equal, ["lg", "m1"], ["eq1"])
    k.stt(lg2[:], eq1[:], -1e30, lg[:], ALU.mult, ALU.add, ["eq1", "lg"], ["lg2"])
    s.add("dve", lambda e: e.tensor_reduce(m2[:, :], lg2[:, :, :], AX.X, ALU.max), ["lg2"], ["m2"])
    k.tt(eq2[:], lg2[:], bc(m2), ALU.is_equal, ["lg2", "m2"], ["eq2"])
    k.tt(w1[:], m1[:], m2[:], ALU.subtract, ["m1", "m2"], ["w1"])
    k.act(w1[:], w1[:], AF.Sigmoid, ["w1"], ["w1"])
    k.ts(w2[:], w1[:], -1.0, 1.0, ALU.mult, ALU.add, ["w1"], ["w2"])
    k.tt(eq1[:], eq1[:], bc(w1), ALU.mult, ["eq1", "w1"], ["eq1"])
    k.tt(eq2[:], eq2[:], bc(w2), ALU.mult, ["eq2", "w2"], ["eq2"])
    k.tt(gates[:], eq1[:], eq2[:], ALU.add, ["eq1", "eq2"], ["gates"])
    gT = m.alloc("gT", [8, SEQH], F32)
    for t4 in range(NTT):
        pbk = k.ps[6]
        for j in range(4):
            i = t4 * 4 + j
            k.tr(pbk[0:8, j * 128:(j + 1) * 128], gates[:, i, :], k.ident, ["gates", "cf32"], ["ps6"])
        k.copy(gT[:, t4 * TT:(t4 + 1) * TT], pbk[0:8, :], ["ps6"], [("gT", t4)], eng="act")
    gbc = [m.alloc("gbc", [128, SEQH], F32) for _ in range(2)]
    k.ffn_bufs()

    def make_pre(e):
        def pre():
            gb = gbc[e % 2]
            gk = "gbc%d" % (e % 2)
            for t4 in range(NTT):
                pbk = k.ps[7]
                pkk = "ps7"
                k.mm(pbk[:, :], [(k.sel[:, e * 128:(e + 1) * 128], gT[:, t4 * TT:(t4 + 1) * TT])], [("gT", t4), "cf32"], [pkk])
                k.copy(gb[:, t4 * TT:(t4 + 1) * TT], pbk[:, :], [pkk], [gk], eng="act")
        return pre

    chunks = []
    for e in range(N_EXPERTS):
        chunks += k.ffn_chunks(wg_d[e], wu_d[e], wd_d[e], EXPERT_DIM, gate_bc=gbc[e % 2], gname="gbc%d" % (e % 2),
                               pre=make_pre(e))
    k.ffn_run(xT, xname, h2T, hname, chunks)


CAP = SEQH
ST = 512
NSLT = CAP // ST
I32 = mybir.dt.int32


def hc_zero_fill(k, zt, hc_d):
    k.memset(zt[:, :], 0.0, ["zt"], eng="pool")
    for j in range(N_EXPERTS * CAP // 128):
        k.dma("sp", hc_d[j * 128:(j + 1) * 128, :], zt[:, :], ["zt"], ["hcz"], "hcz")


def moe_sparse(k, xT, xname, xT_off, g32, wr_d, wg_d, wu_d, wd_d, y_dram, hc_d, yc_d, cnt_d):
    m = k.m
    s = k.s
    NS = SEQH // 128
    NFC = EXPERT_DIM // 512
    k.ffn_bufs()
    chunks = []
    for e in range(N_EXPERTS):
        wgv = wg_d[e].rearrange("(c p) f -> p c f", p=128)
        wuv = wu_d[e].rearrange("(c p) f -> p c f", p=128)
        wdv = wd_d[e].rearrange("(s p) d -> p s d", p=128)
        for fc in range(NFC):
            chunks.append(dict(e=e, fc=fc, f0=fc * 512, wgv=wgv, wuv=wuv, wdv=wdv))
    nch = len(chunks)

    def load(ci):
        ch = chunks[ci]
        slot = ci % 2
        wg, wu, wd = k.wbuf[slot]
        kg, ku, kd = ("wg%d" % slot, "wu%d" % slot, "wd%d" % slot)
        f0 = ch["f0"]
        k.wdma(wg[:, :, :], ch["wgv"][:, :, f0:f0 + 512], kg, kg)
        k.wdma(wu[:, :, :], ch["wuv"][:, :, f0:f0 + 512], ku, ku)
        k.wdma(wd[:, :, :], ch["wdv"][:, f0 // 128:f0 // 128 + 4, :], kd, kd)

    load(0)
    load(1)
    d1i = m.alloc("d1i", [128, NS], I32)
    d2i = m.alloc("d2i", [128, NS], I32)
    w1 = m.alloc("w1", [128, NS], F32)
    w2 = m.alloc("w2", [128, NS], F32)
    cnti = m.alloc("cnti", [128, 8], I32)
    stg_off = [m.off, m.off + 8192]
    stg = [m.alloc("stg", [128, 4, D], BF16) for _ in range(2)]
    h2T_off = m.off
    h2T = m.alloc("h2T", [128, NCH, SEQH], BF16)
    hname = "h2T"
    mkA = m.mark()
    sq = m.overlay("sq", [128, NCH, TT], BF16, stg_off[0])
    rstd = m.overlay("rstd", [128, TT], F32, stg_off[1])
    sst = [m.overlay("sst", [128, D], BF16, stg_off[1] + 2048 * (1 + j)) for j in range(2)]
    sst += [m.overlay("sst", [128, D], BF16, stg_off[0] + 2048 * j) for j in range(2)]
    for tt in range(NTT):
        k.rmsnorm_T(xT, xname, g32, h2T, hname, slice(tt * TT, (tt + 1) * TT), tt, sq, "sq", rstd, "rstd")
    wr = m.alloc("wr", [128, NCH, 8], BF16)
    wrf = m.alloc("wrf", [128, NCH, 8], F32)
    k.dma("sp", wrf[:], wr_d.rearrange("(c p) e -> p c e", p=128), (), ["wrf"], "wrf")
    k.copy(wr[:], wrf[:], ["wrf"], ["wr"])
    lg = m.alloc("lg", [128, NS, 8], F32)
    pb = k.ps[7]
    for i in range(NS):
        k.mm(pb[:, i * 8:(i + 1) * 8], [(h2T[:, c, i * 128:(i + 1) * 128], wr[:, c, :]) for c in range(NCH)],
             ["wr"] + [(hname, c, i // 4) for c in range(NCH)], ["ps7"])
    k.copy(lg[:].rearrange("p a b -> p (a b)"), pb[:, 0:NS * 8], ["ps7"], ["lg"], eng="act")
    m1 = m.alloc("m1", [128, NS], F32)
    m2 = m.alloc("m2", [128, NS], F32)
    eq1 = m.alloc("eq1", [128, NS, 8], F32)
    eq2 = m.alloc("eq2", [128, NS, 8], F32)
    lg2 = m.alloc("lg2", [128, NS, 8], F32)
    mask = m.alloc("mask", [128, NS, 8], F32)
    maskb = m.alloc("maskb", [128, NS, 8], BF16)
    it = m.alloc("it", [128, 2, NS, 8], F32)
    off = m.alloc("off", [128, NS, 8], F32)
    ebase = m.alloc("ebase", [128, NS, 8], F32)
    rank = m.alloc("rank", [128, NS, 8], F32)
    tmp = m.alloc("rtmp", [128, NS, 8], F32)
    d1f = m.alloc("d1f", [128, NS], F32)
    d2f = m.alloc("d2f", [128, NS], F32)
    cntf = m.alloc("cntf", [128, 8], F32)

    def bc(t):
        return t[:, :].unsqueeze(2).broadcast_to([128, NS, 8])

    s.add("dve", lambda e: e.tensor_reduce(m1[:, :], lg[:, :, :], AX.X, ALU.max), ["lg"], ["m1"])
    k.tt(eq1[:], lg[:], bc(m1), ALU.is_equal, ["lg", "m1"], ["eq1"])
    k.stt(lg2[:], eq1[:], -1e30, lg[:], ALU.mult, ALU.add, ["eq1", "lg"], ["lg2"])
    s.add("dve", lambda e: e.tensor_reduce(m2[:, :], lg2[:, :, :], AX.X, ALU.max), ["lg2"], ["m2"])
    k.tt(eq2[:], lg2[:], bc(m2), ALU.is_equal, ["lg2", "m2"], ["eq2"])
    k.tt(w1[:], m1[:], m2[:], ALU.subtract, ["m1", "m2"], ["w1"])
    k.act(w1[:], w1[:], AF.Sigmoid, ["w1"], ["w1"])
    k.ts(w2[:], w1[:], -1.0, 1.0, ALU.mult, ALU.add, ["w1"], ["w2"])
    k.tt(mask[:], eq1[:], eq2[:], ALU.add, ["eq1", "eq2"], ["mask"])
    k.copy(maskb[:], mask[:], ["mask"], ["maskb"])
    for e in range(N_EXPERTS):
        k.memset(ebase[:, :, e:e + 1], float(e * CAP), ["ebase"], eng="dve")
    mb2 = maskb[:].rearrange("p a b -> p (a b)")
    k.mm(k.ps[6][:, 0:128], [(k.utri_b, mb2)], ["maskb", "cbf"], ["ps6"])
    k.mm(k.ps[6][:, 128:256], [(k.ones_b, mb2)], ["maskb", "cbf"], ["ps6"])
    k.copy(it[:].rearrange("p t a b -> p (t a b)"), k.ps[6][:, 0:256], ["ps6"], ["it"], eng="act")
    k.memset(off[:, 0, :], 0.0, [("off", 0)], eng="dve")
    for i in range(1, NS):
        k.tt(off[:, i, :], off[:, i - 1, :], it[:, 1, i - 1, :], ALU.add, [("off", i - 1), "it"], [("off", i)])
    k.tt(cntf[:, :], off[:, NS - 1, :], it[:, 1, NS - 1, :], ALU.add, [("off", NS - 1), "it"], ["cntf"])
    k.copy(cnti[:, :], cntf[:, :], ["cntf"], ["cnti"])
    k.dma("sp", cnt_d[0:1, :], cnti[0:1, :], ["cnti"], ["cntd"], "cntd")
    offr = [("off", i) for i in range(NS)]
    k.tt(rank[:], it[:, 0, :, :], mask[:], ALU.subtract, ["it", "mask"], ["rank"])
    k.tt(rank[:], rank[:], off[:], ALU.add, ["rank"] + offr, ["rank"])
    k.tt(rank[:], rank[:], ebase[:], ALU.add, ["rank", "ebase"], ["rank"])
    k.tt(tmp[:], rank[:], eq1[:], ALU.mult, ["rank", "eq1"], ["rtmp"])
    s.add("dve", lambda e: e.tensor_reduce(d1f[:, :], tmp[:, :, :], AX.X, ALU.add), ["rtmp"], ["d1f"])
    k.copy(d1i[:, :], d1f[:, :], ["d1f"], ["d1i"])
    k.tt(tmp[:], rank[:], eq2[:], ALU.mult, ["rank", "eq2", "d1f"], ["rtmp"])
    s.add("dve", lambda e: e.tensor_reduce(d2f[:, :], tmp[:, :, :], AX.X, ALU.add), ["rtmp"], ["d2f"])
    k.copy(d2i[:, :], d2f[:, :], ["d2f"], ["d2i"])
    s.reg_keys = list(range(N_EXPERTS))
    for en in ("pe", "act", "dve", "pool", "sp"):
        for e in range(N_EXPERTS):
            s.add(en, lambda eng, e=e: eng.reg_load(s.cur_regs[e], cnt_d[0:1, e:e + 1]), ["cntd"], ())
    ystg = [m.alloc("ystg", [128, D], F32) for _ in range(2)]
    for i in range(NS):
        j = i % 4
        pbf = k.ps[i % 2].bitcast(BF16)
        pk = "ps%d" % (i % 2)
        for c in range(NCH):
            k.tr(pbf[:, c * 128:(c + 1) * 128], h2T[:, c, i * 128:(i + 1) * 128], k.ident_b,
                 [(hname, c, i // 4), "cbf"], [pk])
        tk = "sst%d" % j
        k.copy(sst[j][:, :], pbf[:, 0:1024], [pk], [tk], eng="act" if i % 2 == 0 else "dve")
        for a, di in enumerate((d1i, d2i)):
            s.add("pool", lambda eng, di=di, i=i, j=j: eng.indirect_dma_start(
                out=hc_d[:, :], out_offset=bass.IndirectOffsetOnAxis(ap=di[:, i:i + 1], axis=0),
                in_=sst[j][:, :], in_offset=None),
                [tk, "hcz", "d1i", "d2i"], [("hcs", i, a)], dma_key="hcs")
        sb = ystg[i % 2]
        tky = "ystg%d" % (i % 2)
        for c in range(NCH):
            bank = 2 + (i * NCH + c) % 2
            pb2 = k.ps[bank]
            pk2 = "ps%d" % bank
            k.tr(pb2[:, 0:128], xT[:, c, i * 128:(i + 1) * 128], k.ident, [(xname, c, i), "cf32"], [pk2])
            k.copy(sb[:, c * 128:(c + 1) * 128], pb2[:, 0:128], [pk2], [tky], eng="act" if c % 2 == 0 else "dve")
        k.dma("sp", y_dram[i * 128:(i + 1) * 128, :], sb[:], [tky], [("yout", i)], "yout")
    hcs_tokens = [("hcs", i, a) for i in range(NS) for a in range(2)]
    s.freeze_weights = True
    m.release(mkA)
    yacc = m.overlay("yacc", [128, CAP // 128, D], F32, xT_off)
    hTe = m.overlay("hTe", [128, NCH, CAP], BF16, h2T_off)

    def prep(e, tts, region=None):
        for tt in tts:
            sb = stg[tt % 2]
            tk = "stg%d" % (tt % 2)
            r0 = e * CAP + tt * ST
            s.cur_region = region
            k.dma("sp", sb[:, :, :], hc_d[r0:r0 + ST, :].rearrange("(b p) d -> p b d", p=128), hcs_tokens, [tk], tk)
            for blk in range(4):
                pbf = k.ps[7].bitcast(BF16)
                for c in range(NCH):
                    k.tr(pbf[:, c * 128:(c + 1) * 128], sb[:, blk, c * 128:(c + 1) * 128], k.ident_b, [tk, "cbf"], ["ps7"])
                col = (tt * 4 + blk) * 128
                k.copy(hTe[:, :, col:col + 128], pbf[:, 0:1024].rearrange("p (c q) -> p c q", c=NCH), ["ps7"],
                       [("hTe", tt * 4 + blk)], eng="act")
            s.cur_region = None

    MAINW = 640
    main_tiles = [(0, 512, 0), (512, 128, 1)]
    rare_tiles = [(640, 384, 1), (1024, 512, 0), (1536, 512, 1)]

    def GU(ci, tile, region):
        ch = chunks[ci]
        t0, tw, ab = tile
        s.cur_region = region
        slot = ci % 2
        wg, wu, wd = k.wbuf[slot]
        kg, ku = "wg%d" % slot, "wu%d" % slot
        tsl = slice(t0, t0 + tw)
        hrd = [("hTe", b_) for b_ in range(t0 // 128, (t0 + tw) // 128)]
        for fs in range(4):
            j = k.cnt % 2
            k.cnt += 1
            pg, pu = k.ps[j], k.ps[2 + j]
            kpg, kpu = "ps%d" % j, "ps%d" % (2 + j)
            k.mm(pg[:, 0:tw], [(wg[:, c, fs * 128:(fs + 1) * 128], hTe[:, c, tsl]) for c in range(NCH)], [kg] + hrd, [kpg])
            k.mm(pu[:, 0:tw], [(wu[:, c, fs * 128:(fs + 1) * 128], hTe[:, c, tsl]) for c in range(NCH)], [ku] + hrd, [kpu])
            sg = k.sgbuf[j]
            ksg = "sg%d" % j
            k.act(sg[:, 0:tw], pg[:, 0:tw], AF.Silu, [kpg], [ksg])
            k.tt(k.actbuf[ab][:, fs, 0:tw], pu[:, 0:tw], sg[:, 0:tw], ALU.mult, [kpu, ksg], [("actT", ab, fs)])
        s.cur_region = None

    def DN(ci, tile, region):
        ch = chunks[ci]
        t0, tw, ab = tile
        s.cur_region = region
        wd = k.wbuf[ci % 2][2]
        kd = "wd%d" % (ci % 2)
        abuf = k.actbuf[ab]
        kab = [("actT", ab, fs) for fs in range(4)]
        for blk in range(tw // 128):
            gb = t0 // 128 + blk
            for dh in range(2):
                j = k.cnt2 % 3
                k.cnt2 += 1
                pd = k.ps[4 + j]
                kpd = "ps%d" % (4 + j)
                k.mm(pd[:, :], [(abuf[:, fs, blk * 128:(blk + 1) * 128], wd[:, fs, dh * 512:(dh + 1) * 512]) for fs in range(4)],
                     [kd] + kab, [kpd])
                dst = yacc[:, gb, dh * 512:(dh + 1) * 512]
                tok = ("yacc", gb, dh)
                if ch["fc"] == 0:
                    k.copy(dst, pd[:, :], [kpd], [tok])
                else:
                    k.tt(dst, pd[:, :], dst, ALU.add, [kpd, tok], [tok])
        s.cur_region = None

    def ystore(e, gb0, gb1, region=None):
        r0 = e * CAP + gb0 * 128
        s.cur_region = region
        k.dma("sp", yc_d[r0:r0 + (gb1 - gb0) * 128, :].rearrange("(b p) d -> p b d", p=128), yacc[:, gb0:gb1, :],
              [("yacc", gb, dh) for gb in range(gb0, gb1) for dh in range(2)], [("ycd", e, gb0)], "ycd")
        s.cur_region = None

    ycd_tokens = []
    prep(0, (0, 1))
    for ci in range(nch):
        e = chunks[ci]["e"]
        lastc = chunks[ci]["fc"] == NFC - 1
        GU(ci, main_tiles[0], None)
        GU(ci, main_tiles[1], None)
        if lastc and e + 1 < N_EXPERTS:
            prep(e + 1, (0, 1))
        DN(ci, main_tiles[0], None)
        DN(ci, main_tiles[1], None)
        if lastc:
            ystore(e, 0, 4)
            ystore(e, 4, 5)
            ycd_tokens += [("ycd", e, 0), ("ycd", e, 4)]
        if ci + 2 < nch:
            load(ci + 2)
    for e in range(N_EXPERTS):
        for (reg, ptiles, rtiles, stores) in (((e, MAINW), (1,), rare_tiles[0:1], ((5, 8),)),
                                              ((e, 2 * ST), (2, 3), rare_tiles[1:3], ((8, 12), (12, 16)))):
            prep(e, ptiles, region=reg)
            for fc in range(NFC):
                ci = e * NFC + fc
                s.cur_region = reg
                load(ci)
                s.cur_region = None
                for tile in rtiles:
                    GU(ci, tile, reg)
                    DN(ci, tile, reg)
            for (g0, g1) in stores:
                ystore(e, g0, g1, region=reg)
                ycd_tokens.append(("ycd", e, g0))
    mkB = m.mark()
    xs = [m.alloc("xs", [128, D], F32) for _ in range(2)]
    a1 = [m.alloc("a1", [128, D], F32) for _ in range(2)]
    a2 = [m.alloc("a2", [128, D], F32) for _ in range(2)]
    for i in range(NS):
        j = i % 2
        k.dma("sp", xs[j][:, :], y_dram[i * 128:(i + 1) * 128, :], [("yout", i)], ["xs%d" % j], "xs%d" % j)
        for (ab_, di, nm) in ((a1, d1i, "a1"), (a2, d2i, "a2")):
            s.add("pool", lambda eng, ab_=ab_, di=di, i=i, j=j: eng.indirect_dma_start(
                out=ab_[j][:, :], out_offset=None, in_=yc_d[:, :],
                in_offset=bass.IndirectOffsetOnAxis(ap=di[:, i:i + 1], axis=0)),
                ycd_tokens + ["d1i", "d2i"], ["%s%d" % (nm, j)], dma_key="%s%d" % (nm, j))
        k.stt(xs[j][:, :], a1[j][:, :], w1[:, i:i + 1], xs[j][:, :], ALU.mult, ALU.add, ["a1%d" % j, "xs%d" % j, "w1"], ["xs%d" % j])
        k.stt(xs[j][:, :], a2[j][:, :], w2[:, i:i + 1], xs[j][:, :], ALU.mult, ALU.add, ["a2%d" % j, "xs%d" % j, "w2"], ["xs%d" % j])
        k.dma("sp", y_dram[i * 128:(i + 1) * 128, :], xs[j][:, :], ["xs%d" % j], [("yfin", i)], "yfin")
    s.add("sp", lambda e: e.nop(), [("yfin", i) for i in range(NS)], ())
    m.release(mkB)


def build_moe():
    nc = bass.Bass("TRN2", target_bir_lowering=False)
    x = nc.dram_tensor("x", [SEQH, D], F32, kind="ExternalInput").ap()
    cst = nc.dram_tensor("cst", [128, CONST_W], F32, kind="ExternalInput").ap()
    par = nc.dram_tensor("par", [128, 8], F32, kind="ExternalInput").ap()
    wr = nc.dram_tensor("wr", [D, N_EXPERTS], F32, kind="ExternalInput").ap()
    wg = nc.dram_tensor("wg", [N_EXPERTS, D, EXPERT_DIM], F32, kind="ExternalInput").ap()
    wu = nc.dram_tensor("wu", [N_EXPERTS, D, EXPERT_DIM], F32, kind="ExternalInput").ap()
    wd = nc.dram_tensor("wd", [N_EXPERTS, EXPERT_DIM, D], F32, kind="ExternalInput").ap()
    y = nc.dram_tensor("y", [SEQH, D], F32, kind="ExternalOutput").ap()
    hc_d = nc.dram_tensor("hcd", [N_EXPERTS * CAP, D], BF16).ap()
    yc_d = nc.dram_tensor("ycd", [N_EXPERTS * CAP, D], F32).ap()
    cnt_d = nc.dram_tensor("cntd", [1, 8], I32).ap()
    k = K(nc)
    with k.st:
        m = k.m
        k.setup_consts(cst)
        xT_off = m.off
        xT = m.alloc("xT", [128, NCH, SEQH], F32)
        g = m.alloc("g", [128, 8], F32)
        g32 = m.alloc("g32", [128, 8], F32)
        zt = m.alloc("zt", [128, D], BF16)
        k.dma("sp", g[:], par[:, :], (), ["graw"], "par")
        k.ts(g32[:], g[:], 32.0, None, ALU.mult, None, ["graw"], ["par"])
        k.load_xT(x, xT, "xT")
        hc_zero_fill(k, zt, hc_d)
        moe_sparse(k, xT, "xT", xT_off, g32, wr, wg, wu, wd, y, hc_d, yc_d, cnt_d)
        k.s.emit()
    return nc


def run_moe(x_shards, ln2_g1, wr, wg, wu, wd, trace=False):
    if "moe" not in _cache:
        _cache["moe"] = build_moe()
    nc = _cache["moe"]
    cst = make_consts()
    par = pack_pp(ln2_g1)
    in_maps = [{"x": np.ascontiguousarray(xs), "cst": cst, "par": par, "wr": wr, "wg": wg, "wu": wu, "wd": wd}
               for xs in x_shards]
    res = run_bass_kernel_spmd(nc, in_maps, core_ids=list(range(8)), trace=trace)
    return [r["y"] for r in res.results], res


PA_W = 24 + 256


def pack_par_attn(inp, layer, flag):
    p = np.zeros((128, PA_W), np.float32)
    p[:, 0:8] = pack_pp(inp["ln1_g"][layer])
    p[:, 8:16] = pack_pp(inp["mem_norm_g"])
    p[:, 16] = np.tile(np.asarray(inp["da_qn_g"][0], np.float32), 2)
    p[:, 17] = np.tile(np.asarray(inp["da_kn_g"][0], np.float32), 2)
    p[:, 18] = np.tile(np.asarray(inp["mem_qn_g"][layer], np.float32), 2)
    p[:, 19] = np.tile(np.asarray(inp["mem_kn_g"][layer], np.float32), 2)
    p[:, 20] = np.asarray(inp["da_sub_g"][0], np.float32)
    p[:, 21] = flag
    p[:, 22] = NEG if flag == 0 else 0.0
    lv = np.concatenate([np.asarray(inp[n][0], np.float32) for n in ("da_lq1", "da_lk1", "da_lq2", "da_lk2")])
    p[:, 24:24 + 256] = lv[None, :]
    return p


def headnorm(k, raw, P, n, gain, ones_bf, out, reads, writes, tmp):
    sq, ksq, rs, krs, pb, kpb = tmp["sq"], tmp["ksq"], tmp["rs"], tmp["krs"], tmp["pb"], tmp["kpb"]
    k.act(sq[0:P, 0:n], raw, AF.Square, reads, [ksq])
    k.mm(pb[0:P, 0:n], [(ones_bf, sq[0:P, 0:n])], [ksq, "cbf"], [kpb])
    k.act(rs[0:P, 0:n], pb[0:P, 0:n], AF.Ln, [kpb, "epsc"], [krs], bias=k.eps_ap(64 * EPS)[0:P, :])
    k.act(rs[0:P, 0:n], rs[0:P, 0:n], AF.Exp, [krs], [krs], scale=-0.5)
    k.stt(out, raw, gain, rs[0:P, 0:n], ALU.mult, ALU.mult, list(reads) + [krs, "gains"], writes)


def mem_kv_setup(k, mem_d, wkv_d, gmem32, gmk8, KmT, Vm, lname):
    m = k.m
    mk = m.mark()
    memT = m.alloc("memT", [128, NCH, MEM_LEN], F32)
    k.load_xT(mem_d, memT, "memT" + lname, ntok=MEM_LEN)
    mnT = m.alloc("mnT", [128, NCH, MEM_LEN], BF16)
    sq = m.alloc("msq", [128, NCH, MEM_LEN], BF16)
    rstd = m.alloc("mrstd", [128, MEM_LEN], F32)
    pb = k.ps[6]
    rd = [("memT" + lname, c, i) for c in range(NCH) for i in range(2)]
    k.act(sq[:, :, :], memT[:, :, :], AF.Square, rd, ["msq"])
    k.mm(pb[:, 0:MEM_LEN], [(k.ones_b, sq[:, c, :]) for c in range(NCH)], ["msq", "cbf"], ["ps6"])
    k.act(rstd[:, :], pb[:, 0:MEM_LEN], AF.Ln, ["ps6", "epsc"], ["mrstd"], bias=k.eps_ap(D * EPS))
    k.act(rstd[:, :], rstd[:, :], AF.Exp, ["mrstd"], ["mrstd"], scale=-0.5)
    for c in range(NCH):
        k.stt(mnT[:, c, :], memT[:, c, :], gmem32[:, c:c + 1], rstd[:, :], ALU.mult, ALU.mult,
              rd + ["mrstd", "gains"], [("mnT", c)])
    wkv = m.alloc("wkv", [128, NCH, 512], BF16)
    k.dma("pool", wkv[:], wkv_d.rearrange("(c p) f -> p c f", p=128), (), ["wkv"], "wkv")
    tmp = dict(sq=m.alloc("hsq", [128, 512], BF16), ksq="hsq", rs=m.alloc("hrs", [128, 512], F32), krs="hrs",
               pb=k.ps[7], kpb="ps7")
    mn_rd = [("mnT", c) for c in range(NCH)]
    for hm in range(4):
        pr = k.ps[hm % 2]
        kpr = "ps%d" % (hm % 2)
        k.mm(pr[0:64, 0:MEM_LEN], [(wkv[:, c, hm * 64:(hm + 1) * 64], mnT[:, c, :]) for c in range(NCH)],
             ["wkv"] + mn_rd, [kpr])
        headnorm(k, pr[0:64, 0:MEM_LEN], 64, MEM_LEN, gmk8[0:64, :], k.ones_b[0:64, 0:64], KmT[0:64, hm, :],
                 [kpr], [("KmT" + lname, hm)], tmp)
    for mt in range(2):
        pr = k.ps[2 + mt]
        kpr = "ps%d" % (2 + mt)
        k.mm(pr[:, 0:256], [(mnT[:, c, mt * 128:(mt + 1) * 128], wkv[:, c, 256:512]) for c in range(NCH)],
             ["wkv"] + mn_rd, [kpr])
        k.copy(Vm[:, mt, :], pr[:, 0:256], [kpr], [("Vm" + lname, mt)], eng="act")
    m.release(mk)


def attn_inproj(k, x_tiles, xT_own, g32, win, gq8, gk8, gmq8, QT, mqT, kscr, vscr, ctx0):
    m = k.m
    mk = m.mark()
    xtmp = m.alloc("xtmp", [128, NCH, TT], F32) if any(o is None for (_x, o) in x_tiles) else None
    hT = m.alloc("hT", [128, NCH, TT], BF16)
    sq = m.alloc("sq", [128, NCH, TT], BF16)
    rstd = m.alloc("rstd", [128, TT], F32)
    tmps = [dict(sq=m.alloc("hsq", [128, 512], BF16), ksq="hsq%d" % j, rs=m.alloc("hrs", [128, 512], F32), krs="hrs%d" % j,
                 pb=k.ps[7 - j], kpb="ps%d" % (7 - j)) for j in range(2)]
    hn = [0]

    def tmp_next():
        hn[0] += 1
        return tmps[hn[0] % 2]
    kt_sb = [m.alloc("ktsb", [128, TT], BF16) for _ in range(2)]
    vt_sb = [m.alloc("vtsb", [128, TOK_W], BF16) for _ in range(2)]
    stg = [m.alloc("xstg", [128, D], F32) for _ in range(2)]
    cnt = 0
    vcnt = 0
    for ti, (xd, own) in enumerate(x_tiles):
        g = ctx0 + ti
        if own is None:
            dst, dname, dtile, dsl = xtmp, "xtmp", 0, slice(0, TT)
        else:
            dst, dname, dtile, dsl = xT_own, "xT", own, slice(own * TT, (own + 1) * TT)
        for i in range(4):
            sb = stg[i % 2]
            tk = "xstg%d" % (i % 2)
            k.dma("sp", sb[:], xd[i * 128:(i + 1) * 128, :], (), [tk], tk)
            for c in range(NCH):
                pb = k.ps[(i * NCH + c) % 2]
                pk = "ps%d" % ((i * NCH + c) % 2)
                k.tr(pb[:, 0:128], sb[:, c * 128:(c + 1) * 128], k.ident, [tk, "cf32"], [pk])
                k.copy(dst[:, c, dsl.start + i * 128:dsl.start + (i + 1) * 128], pb[:, 0:128], [pk],
                       [(dname, c, dtile * 4 + i)], eng="act" if c % 2 == 0 else "dve")
        k.rmsnorm_T(dst, dname, g32, hT, "hT", dsl, dtile, sq, "sq", rstd, "rstd", htile=0)
        h_rd = [("hT", c, 0) for c in range(NCH)]
        for h in range(6):
            pr = k.ps[2 + cnt % 2]
            kpr = "ps%d" % (2 + cnt % 2)
            k.mm(pr[:, :], [(win[:, c, 768 + h * 128:768 + (h + 1) * 128], hT[:, c, :]) for c in range(NCH)],
                 ["win"] + h_rd, [kpr])
            kb = kt_sb[cnt % 2]
            kkb = "ktsb%d" % (cnt % 2)
            cnt += 1
            headnorm(k, pr[:, :], 128, TT, gk8, k.bd_b, kb[:, :], [kpr], [kkb], tmp_next())
            k.dma("sp", kscr[h, :, g * TT:(g + 1) * TT], kb[:, :], [kkb], [("kscr", h, g)], "kscr")
        for i in range(4):
            vb = vt_sb[vcnt % 2]
            kvb = "vtsb%d" % (vcnt % 2)
            vcnt += 1
            for (c0, cw, bank) in ((0, 512, 4), (512, 256, 5)):
                pr = k.ps[bank]
                kpr = "ps%d" % bank
                k.mm(pr[:, 0:cw], [(hT[:, c, i * 128:(i + 1) * 128], win[:, c, 1536 + c0:1536 + c0 + cw])
                                   for c in range(NCH)], ["win"] + h_rd, [kpr])
                k.copy(vb[:, c0:c0 + cw], pr[:, 0:cw], [kpr], [kvb], eng="act" if bank == 4 else "dve")
            k.dma("sp", vscr.rearrange("h p t e -> p h t e")[:, :, g * 4 + i, :],
                  vb[:, :].rearrange("p (h e) -> p h e", e=128), [kvb], [("vscr", g * 4 + i)], "vscr")
        if own is None:
            continue
        for h in range(6):
            pr = k.ps[2 + cnt % 2]
            kpr = "ps%d" % (2 + cnt % 2)
            cnt += 1
            k.mm(pr[:, :], [(win[:, c, h * 128:(h + 1) * 128], hT[:, c, :]) for c in range(NCH)], ["win"] + h_rd, [kpr])
            headnorm(k, pr[:, :], 128, TT, gq8, k.bd_b, QT[:, h, dsl], [kpr], [("QT", h, own)], tmp_next())
        for hm in range(4):
            pr = k.ps[2 + cnt % 2]
            kpr = "ps%d" % (2 + cnt % 2)
            cnt += 1
            k.mm(pr[0:64, :], [(win[:, c, 2304 + hm * 64:2304 + (hm + 1) * 64], hT[:, c, :]) for c in range(NCH)],
                 ["win"] + h_rd, [kpr])
            headnorm(k, pr[0:64, :], 64, TT, gmq8[0:64, :], k.ones_b[0:64, 0:64], mqT[0:64, hm, dsl], [kpr],
                     [("mqT", hm, own)], tmp_next())
    m.release(mk)


def attn_core(k, QT, tokT, kscr, vscr, neglam, subgs, flagb, nprev):
    m = k.m
    mk = m.mark()
    nctx = nprev + NTT
    nkt = nctx * 4
    kbuf = [m.alloc("kbuf", [128, nctx * TT], BF16) for _ in range(2)]
    vbuf = [m.alloc("vbuf", [128, nkt, 128], BF16) for _ in range(2)]
    pb = [m.alloc("pp", [128, 2, TT], BF16) for _ in range(2)]
    acc = m.alloc("acc", [128, 2, TT], F32)
    rs1 = m.alloc("rs1", [128, TT], F32)
    rs2 = m.alloc("rs2", [128, TT], F32)
    t1 = m.alloc("t1", [128, TT], F32)
    t2 = m.alloc("t2", [128, TT], F32)
    sqb = m.alloc("asq", [128, TT], BF16)
    rsn = m.alloc("rsn", [128, TT], F32)
    steps = []
    for h in range(6):
        for qt in range(NTT):
            kts = [(g, 0, True) for g in range(nprev * 4)] + [(nprev * 4 + j, 0, False) for j in range(qt * 4)] + \
                  [(nprev * 4 + qt * 4 + j, 128 * j, False) for j in range(4)]
            for idx, (g, n0, isprev) in enumerate(kts):
                steps.append((h, qt, idx, len(kts), g, n0, isprev))
    loaded = set()

    def load_head(h):
        if h in loaded or h >= 6:
            return
        loaded.add(h)
        kb, vb = kbuf[h % 2], vbuf[h % 2]
        kkb, kvb = "kbuf%d" % (h % 2), "vbuf%d" % (h % 2)
        k.dma("sp", kb[:, :], kscr[h, :, 0:nctx * TT], [("kscr", h, g) for g in range(nctx)], [kkb], kkb)
        k.dma("sp", vb[:, :, :], vscr[h, :, 0:nkt, :], [("vscr", g) for g in range(nkt)], [kvb], kvb)

    def S(i):
        h, qt, idx, nk, g, n0, isprev = steps[i]
        load_head(h)
        b = i % 2
        kb = kbuf[h % 2]
        kkb = "kbuf%d" % (h % 2)
        k.mm1(k.ps[2 * b][:, n0:TT], kb[0:64, g * 128:(g + 1) * 128], QT[0:64, h, qt * TT + n0:(qt + 1) * TT], True, True,
              [kkb, ("QT", h, qt)], ["ps%d" % (2 * b)])
        k.mm1(k.ps[2 * b + 1][:, n0:TT], kb[64:128, g * 128:(g + 1) * 128], QT[64:128, h, qt * TT + n0:(qt + 1) * TT], True, True,
              [kkb, ("QT", h, qt)], ["ps%d" % (2 * b + 1)])

    load_head(0)
    S(0)
    for i in range(len(steps)):
        h, qt, idx, nk, g, n0, isprev = steps[i]
        if i + 1 < len(steps):
            S(i + 1)
        if idx == 0 and qt == 0:
            load_head(h + 1)
        b = i % 2
        vb = vbuf[h % 2]
        kvb = "vbuf%d" % (h % 2)
        P = pb[b]
        kp = "pp%d" % b
        sview = k.psall[:, 2 * b * 512:(2 * b + 2) * 512].rearrange("p (a c) -> p a c", c=512)
        diag = n0 > 0 or (g >= nprev * 4 + qt * 4)
        bias = flagb if isprev else None
        rdb = ["gains"] if isprev else []
        k.act(P[:, :, n0:TT], sview[:, :, n0:TT], AF.Exp, ["ps%d" % (2 * b), "ps%d" % (2 * b + 1)] + rdb, [kp],
              bias=bias, scale=0.125)
        if diag:
            k.memset(P[64:128, :, n0:n0 + 64], 0.0, [kp], eng="pool")
        if idx == 0:
            k.copy(acc[:, 0, :], P[:, 0, :], [kp], ["acc0"], eng="dve")
            k.copy(acc[:, 1, :], P[:, 1, :], [kp], ["acc1"], eng="pool")
        else:
            k.tt(acc[:, 0, n0:TT], P[:, 0, n0:TT], acc[:, 0, n0:TT], ALU.add, [kp, "acc0"], ["acc0"], eng="dve")
            k.tt(acc[:, 1, n0:TT], P[:, 1, n0:TT], acc[:, 1, n0:TT], ALU.add, [kp, "acc1"], ["acc1"], eng="pool")
        k.mm1(k.ps[4][:, n0:TT], vb[:, g, :], P[:, 0, n0:TT], idx == 0, idx == nk - 1, [kvb, kp], ["ps4"])
        k.mm1(k.ps[5][:, n0:TT], vb[:, g, :], P[:, 1, n0:TT], idx == 0, idx == nk - 1, [kvb, kp], ["ps5"])
        if idx != nk - 1:
            continue
        qsl = slice(qt * TT, (qt + 1) * TT)
        k.mm(k.ps[6][:, :], [(k.ones_f, acc[:, 0, :])], ["acc0", "cf32"], ["ps6"])
        k.mm(k.ps[7][:, :], [(k.ones_f, acc[:, 1, :])], ["acc1", "cf32"], ["ps7"])
        k.s.add("dve", lambda e, o=rs1[:, :], i_=k.ps[6][:, :]: e.reciprocal(o, i_), ["ps6"], ["rs1"])
        k.s.add("dve", lambda e, o=rs2[:, :], i_=k.ps[7][:, :]: e.reciprocal(o, i_), ["ps7"], ["rs2"])
        k.tt(t1[:, :], k.ps[4][:, :], rs1[:, :], ALU.mult, ["ps4", "rs1"], ["t1"])
        k.tt(t2[:, :], k.ps[5][:, :], rs2[:, :], ALU.mult, ["ps5", "rs2"], ["t2"])
        k.stt(t1[:, :], t2[:, :], neglam, t1[:, :], ALU.mult, ALU.add, ["t1", "t2", "gains"], ["t1"])
        k.act(sqb[:, :], t1[:, :], AF.Square, ["t1"], ["asq"])
        k.mm(k.ps[6][:, :], [(k.ones_b, sqb[:, :])], ["asq", "cbf"], ["ps6"])
        k.act(rsn[:, :], k.ps[6][:, :], AF.Ln, ["ps6", "epsc"], ["rsn"], bias=k.eps_ap(128 * EPS))
        k.act(rsn[:, :], rsn[:, :], AF.Exp, ["rsn"], ["rsn"], scale=-0.5)
        k.stt(tokT[:, h, qsl], t1[:, :], subgs, rsn[:, :], ALU.mult, ALU.mult, ["t1", "rsn", "gains"],
              [("tokT", h, qt), ("QT", h, qt)])
    m.release(mk)


def mem_attn(k, mqT, memT, KmT, Vm, lname):
    m = k.m
    mk = m.mark()
    p1 = [m.alloc("pm", [128, TT], BF16) for _ in range(2)]
    rs1 = m.alloc("rsm", [128, TT], F32)
    it = 0
    for qt in range(NTT):
        qsl = slice(qt * TT, (qt + 1) * TT)
        for hm in range(4):
            for mt in range(2):
                b = it % 2
                it += 1
                s1 = k.ps[b]
                ks1 = "ps%d" % b
                P1 = p1[b]
                kp1 = "pm_%d" % b
                k.mm1(s1[:, :], KmT[0:64, hm, mt * 128:(mt + 1) * 128], mqT[0:64, hm, qsl], True, True,
                      [("KmT" + lname, hm), ("mqT", hm, qt)], [ks1])
                k.act(P1[:, :], s1[:, :], AF.Exp, [ks1], [kp1], scale=0.125)
                k.mm1(k.ps[4][0:64, :], Vm[:, mt, hm * 64:(hm + 1) * 64], P1[:, :], mt == 0, mt == 1,
                      [("Vm" + lname, mt), kp1], ["ps4"])
                k.mm1(k.ps[5][0:64, :], k.ones_b[:, 0:64], P1[:, :], mt == 0, mt == 1, ["cbf", kp1], ["ps5"])
            k.s.add("dve", lambda e, o=rs1[0:64, :], i=k.ps[5][0:64, :]: e.reciprocal(o, i), ["ps5"], ["rsm"])
            k.tt(memT[0:64, hm, qsl], k.ps[4][0:64, :], rs1[0:64, :], ALU.mult, ["ps4", "rsm"],
                 [("memT", hm, qt), ("mqT", hm, qt)])
    m.release(mk)


def attn_outproj(k, xT, xname, tokT, tokname, nk, memT, wo_d, lname):
    m = k.m
    mk = m.mark()
    wo = m.alloc("wo", [128, 6, D], BF16)
    wom = m.alloc("wom", [64, 4, D], BF16)
    k.dma("pool", wo[:], wo_d[0:768, :].rearrange("(c p) d -> p c d", p=128), (), ["wo"], "wo")
    k.dma("pool", wom[:], wo_d[768:1024, :].rearrange("(h p) d -> p h d", p=64), (), ["wom"], "wom")
    cnt = 0
    for qt in range(NTT):
        qsl = slice(qt * TT, (qt + 1) * TT)
        rd = [(tokname, kc, qt) for kc in range(nk)] + [("memT", hm, qt) for hm in range(4)]
        for ds in range(NCH):
            pb = k.ps[cnt % 2]
            kpb = "ps%d" % (cnt % 2)
            cnt += 1
            pairs = [(wo[:, kc, ds * 128:(ds + 1) * 128], tokT[:, kc, qsl]) for kc in range(nk)] + \
                    [(wom[0:64, hm, ds * 128:(ds + 1) * 128], memT[0:64, hm, qsl]) for hm in range(4)]
            k.mm(pb[:, :], pairs, ["wo", "wom"] + rd, [kpb])
            k.tt(xT[:, ds, qsl], pb[:, :], xT[:, ds, qsl], ALU.add, [kpb] + k.xtok(xname, qt, [ds]), k.xtok(xname, qt, [ds]))
    m.release(mk)


def attn_gains(k, par_d):
    m = k.m
    par = m.alloc("par", [128, PA_W], F32)
    k.dma("sp", par[:], par_d[:, :], (), ["parraw"], "par")
    gn = m.alloc("gains", [128, 32], F32)
    k.ts(gn[:, 0:16], par[:, 0:16], 32.0, None, ALU.mult, None, ["parraw"], ["par"])
    k.ts(gn[:, 16:20], par[:, 16:20], 8.0, None, ALU.mult, None, ["parraw"], ["gains"])
    lam_init = 0.8 - 0.6 * math.exp(-0.3 * 0)
    k.ts(gn[:, 20:21], par[:, 20:21], float(math.sqrt(128.0) * (1.0 - lam_init)), None, ALU.mult, None, ["parraw"], ["g20"])
    k.copy(gn[:, 21:23], par[:, 21:23], ["parraw"], ["g21"])
    pr = m.alloc("lprod", [128, 2, 64], F32)
    k.tt(pr[:, 0, :], par[:, 24:88], par[:, 88:152], ALU.mult, ["parraw"], ["lprod0"])
    k.tt(pr[:, 1, :], par[:, 152:216], par[:, 216:280], ALU.mult, ["parraw"], ["lprod1"])
    k.s.add("dve", lambda e: e.tensor_reduce(gn[:, 24:26], pr[:, :, :], AX.X, ALU.add), ["lprod0", "lprod1"], ["g24"])
    k.act(gn[:, 24:26], gn[:, 24:26], AF.Exp, ["g24"], ["g24"])
    k.tt(gn[:, 26:27], gn[:, 25:26], gn[:, 24:25], ALU.subtract, ["g24"], ["g26"])
    k.ts(gn[:, 27:28], gn[:, 26:27], float(-lam_init), None, ALU.add, None, ["g26"], ["g27"])
    k.copy(gn[:, 28:29], gn[:, 27:28], ["g27", "g20", "g21", "par", "gains"], ["gains"])
    return dict(g32=gn[:, 0:8], gmem32=gn[:, 8:16], gq8=gn[:, 16:17], gk8=gn[:, 17:18], gmq8=gn[:, 18:19],
                gmk8=gn[:, 19:20], subgs=gn[:, 20:21], flag=gn[:, 21:22], flagb=gn[:, 22:23], neglam=gn[:, 27:28])


def build_attn0():
    nc = bass.Bass("TRN2", target_bir_lowering=False)
    xo = nc.dram_tensor("xo", [SEQH, D], F32, kind="ExternalInput").ap()
    xp = nc.dram_tensor("xp", [SEQH, D], F32, kind="ExternalInput").ap()
    mem = nc.dram_tensor("mem", [MEM_LEN, D], F32, kind="ExternalInput").ap()
    cst = nc.dram_tensor("cst", [128, CONST_W], F32, kind="ExternalInput").ap()
    par = nc.dram_tensor("par", [128, PA_W], F32, kind="ExternalInput").ap()
    win_d = nc.dram_tensor("win", [D, 2560], F32, kind="ExternalInput").ap()
    wkv_d = nc.dram_tensor("wkv", [D, 512], F32, kind="ExternalInput").ap()
    wo_d = nc.dram_tensor("wo", [D, D], F32, kind="ExternalInput").ap()
    y = nc.dram_tensor("y", [SEQH, D], F32, kind="ExternalOutput").ap()
    kscr = nc.dram_tensor("kscr", [6, 128, 2 * SEQH], BF16).ap()
    vscr = nc.dram_tensor("vscr", [6, 128, 32, 128], BF16).ap()
    k = K(nc)
    with k.st:
        m = k.m
        k.setup_consts(cst)
        gd = attn_gains(k, par)
        xT = m.alloc("xT", [128, NCH, SEQH], F32)
        KmT = m.alloc("KmT", [64, 4, MEM_LEN], BF16)
        Vm = m.alloc("Vm", [128, 2, 256], BF16)
        mem_kv_setup(k, mem, wkv_d, gd["gmem32"], gd["gmk8"], KmT, Vm, "0")
        QT = m.alloc("QT", [128, 6, SEQH], BF16)
        mqT = m.alloc("mqT", [64, 4, SEQH], BF16)
        mk1 = m.mark()
        win = m.alloc("win", [128, NCH, 2560], BF16)
        winv = win_d.rearrange("(c p) f -> p c f", p=128)
        for j in range(5):
            k.dma("pool", win[:, :, j * 512:(j + 1) * 512], winv[:, :, j * 512:(j + 1) * 512], (), ["win"], "win")
        tiles = [(xp[i * TT:(i + 1) * TT, :], None) for i in range(NTT)] + [(xo[i * TT:(i + 1) * TT, :], i) for i in range(NTT)]
        attn_inproj(k, tiles, xT, gd["g32"], win, gd["gq8"], gd["gk8"], gd["gmq8"], QT, mqT, kscr, vscr, 0)
        m.release(mk1)
        tokT = m.alloc("tokT", [128, 6, SEQH], BF16)
        memT = m.alloc("memTo", [64, 4, SEQH], BF16)
        attn_core(k, QT, tokT, kscr, vscr, gd["neglam"], gd["subgs"], gd["flagb"], NTT)
        mem_attn(k, mqT, memT, KmT, Vm, "0")
        attn_outproj(k, xT, "xT", tokT, "tokT", 6, memT, wo_d, "0")
        k.store_xT(xT, y, "xT")
        k.s.emit()
    return nc


def run_attn0(inp, xo_shards, xp_shards, flags, mems, trace=False):
    if "attn0" not in _cache:
        _cache["attn0"] = build_attn0()
    nc = _cache["attn0"]
    cst = make_consts()
    in_maps = []
    for i in range(8):
        in_maps.append({"xo": np.ascontiguousarray(xo_shards[i]), "xp": np.ascontiguousarray(xp_shards[i]),
                        "mem": np.ascontiguousarray(mems[i]), "cst": cst, "par": pack_par_attn(inp, 0, flags[i]),
                        "win": np.asarray(inp["da_w_in"][0]), "wkv": np.asarray(inp["mem_w_kv"][0]),
                        "wo": np.asarray(inp["w_out"][0])})
    res = run_bass_kernel_spmd(nc, in_maps, core_ids=list(range(8)), trace=trace)
    return [r["y"] for r in res.results], res


PC_W = 880
TC = 256
NTC = SEQH // TC
SSM_IN = 2316


def pack_par_ssd(inp, flag):
    p = np.zeros((128, PC_W), np.float32)
    p[:, 0:8] = pack_pp(inp["ln1_g"][1])
    p[:, 8:16] = pack_pp(inp["mem_norm_g"])
    p[:, 16] = np.tile(np.asarray(inp["mem_qn_g"][1], np.float32), 2)
    p[:, 17] = np.tile(np.asarray(inp["mem_kn_g"][1], np.float32), 2)
    p[:, 18] = flag
    cw = np.asarray(inp["ssm_conv_w"][0], np.float32)
    p[:, 20:60] = cw.reshape(4, 10, 128).transpose(2, 1, 0).reshape(128, 40)
    p[:, 60:70] = np.asarray(inp["ssm_conv_b"][0], np.float32).reshape(10, 128).T
    p[:, 70:82] = np.asarray(inp["ssm_dt_bias"][0], np.float32)[None, :]
    p[:, 82:94] = np.asarray(inp["ssm_a_log"][0], np.float32)[None, :]
    p[:, 94:106] = np.asarray(inp["ssm_d"][0], np.float32)[None, :]
    p[:, 106:874] = np.asarray(inp["ssm_norm_g"][0], np.float32)[None, :]
    return p


def ssd_gains(k, par_d):
    m = k.m
    par = m.alloc("parc", [128, PC_W], F32)
    k.dma("sp", par[:], par_d[:, :], (), ["parraw"], "par")
    gn = m.alloc("gainc", [128, 48], F32)
    k.ts(gn[:, 0:16], par[:, 0:16], 32.0, None, ALU.mult, None, ["parraw"], ["par"])
    k.ts(gn[:, 16:18], par[:, 16:18], 8.0, None, ALU.mult, None, ["parraw"], ["gains"])
    k.act(gn[:, 20:32], par[:, 82:94], AF.Exp, ["parraw"], ["g20"])
    k.ts(gn[:, 20:32], gn[:, 20:32], -1.0, None, ALU.mult, None, ["g20"], ["g20"])
    k.copy(gn[:, 32:33], par[:, 18:19], ["parraw", "g20", "par", "gains"], ["gains"])
    return dict(g32=gn[:, 0:8], gmem32=gn[:, 8:16], gmq8=gn[:, 16:17], gmk8=gn[:, 17:18], a_bc=gn[:, 20:32],
                flag=gn[:, 32:33], cw=par[:, 20:60], cb=par[:, 60:70], dtb=par[:, 70:82], dsk=par[:, 94:106],
                ng=par[:, 106:874])


def ssd_pass(k, xT, xname, gd, win, own, H, Hbf, halo, tokT, mqT):
    m = k.m
    mk = m.mark()
    h_off = m.mark()
    dabc = m.alloc("dabc", [128, 12, 128], F32)
    hT = m.overlay("hT1", [128, NCH, TC], BF16, h_off)
    sq_off = m.mark()
    expE = m.alloc("expE", [128, 12, 128], F32)
    sq = m.overlay("sq1", [128, NCH, TC], BF16, sq_off)
    ytmp = m.overlay("ytmp", [128, TOK_W], F32, sq_off)
    rstd = m.alloc("rstd1", [128, TC], F32)
    raw = [m.alloc("raw", [128, TC + 3], F32) for _ in range(2)]
    cacc = [m.alloc("cacc", [128, TC], F32) for _ in range(2)]
    xbcT = m.alloc("xbcT", [128, 10, TC], BF16)
    dtt = m.alloc("dtt", [128, 2, 12], F32)
    dAt = m.alloc("dAt", [128, 2, 12], F32)
    xB = m.alloc("xB", [128, 1024], BF16)
    cs = m.alloc("cs", [128, 24], F32)
    ed = m.alloc("ed", [128, 24], F32)
    wend = m.alloc("wend", [128, 12], F32)
    xend = m.alloc("xend", [128, TOK_W], BF16)
    tmp = dict(sq=m.alloc("hsq", [128, 384], BF16), ksq="hsq", rs=m.alloc("hrs", [128, 384], F32), krs="hrs",
               pb=k.ps[7], kpb="ps7")
    if own:
        zs = m.alloc("zs", [128, 2, TOK_W], BF16)
        cb = m.alloc("cb", [128, 2, 128], F32)
        Wt = m.alloc("Wt", [128, 12, 128], BF16)
        xdt = m.alloc("xdt", [128, TOK_W], BF16)
        xD = m.alloc("xD", [128, TOK_W], BF16)
        yn = m.alloc("yn", [128, TOK_W], BF16)
        ss = m.alloc("ss", [128, 4], F32)
        junk = tmp["rs"]
    cw, cbias = gd["cw"], gd["cb"]
    ps = k.ps
    rc = 0
    for ti in range(NTC):
        tsl = slice(ti * TC, (ti + 1) * TC)
        xrd = [(xname, c, ti * 2 + j) for c in range(NCH) for j in range(2)]
        k.act(sq[:, :, :], xT[:, :, tsl], AF.Square, xrd, ["sq1", "ytmp"] + [("expE", q) for q in range(3)])
        k.mm(ps[6][:, 0:TC], [(k.ones_b, sq[:, c, :]) for c in range(NCH)], ["sq1", "cbf"], ["ps6"])
        k.act(rstd[:, :], ps[6][:, 0:TC], AF.Ln, ["ps6", "epsc"], ["rstd1"], bias=k.eps_ap(D * EPS))
        k.act(rstd[:, :], rstd[:, :], AF.Exp, ["rstd1"], ["rstd1"], scale=-0.5)
        for c in range(NCH):
            k.stt(hT[:, c, :], xT[:, c, tsl], gd["g32"][:, c:c + 1], rstd[:, :], ALU.mult, ALU.mult,
                  [(xname, c, ti * 2), (xname, c, ti * 2 + 1), "rstd1", "par"], [("hT1", c), "dabc"])
        h_rd = [("hT1", c) for c in range(NCH)]
        for cc in range(10):
            b = rc % 2
            rc += 1
            pr, kpr = ps[b], "ps%d" % b
            rw, krw = raw[b], "raw%d" % b
            ca, kca = cacc[b], "cacc%d" % b
            k.mm(pr[:, 0:TC], [(win[:, c, 768 + cc * 128:768 + (cc + 1) * 128], hT[:, c, :]) for c in range(NCH)],
                 ["win1"] + h_rd, [kpr])
            k.copy(rw[:, 3:TC + 3], pr[:, 0:TC], [kpr], [krw], eng="act")
            k.copy(rw[:, 0:3], halo[:, cc, :], [("halo", cc)], [krw], eng="pool")
            k.copy(halo[:, cc, :], rw[:, TC:TC + 3], [krw], [("halo", cc)], eng="pool")
            k.ts(ca[:, :], rw[:, 0:TC], cw[:, cc * 4:cc * 4 + 1], cbias[:, cc:cc + 1], ALU.mult, ALU.add,
                 [krw, "parraw"], [kca])
            for j in range(1, 4):
                k.stt(ca[:, :], rw[:, j:j + TC], cw[:, cc * 4 + j:cc * 4 + j + 1], ca[:, :], ALU.mult, ALU.add,
                      [krw, kca, "parraw"], [kca])
            k.act(xbcT[:, cc, :], ca[:, :], AF.Silu, [kca], [("xbcT", cc)])
        for j in range(2):
            k.mm(ps[2][:, j * 12:(j + 1) * 12], [(hT[:, c, j * 128:(j + 1) * 128], win[:, c, 2048:2060]) for c in range(NCH)],
                 ["win1"] + h_rd, ["ps2"])
        k.tt(dtt[:, :, :], ps[2][:, 0:24].rearrange("p (a b) -> p a b", b=12),
             gd["dtb"].unsqueeze(1).broadcast_to([128, 2, 12]), ALU.add, ["ps2", "parraw"], ["dtt"])
        k.act(dtt[:, :, :], dtt[:, :, :], AF.Exp, ["dtt"], ["dtt"])
        k.act(dtt[:, :, :], dtt[:, :, :], AF.Ln, ["dtt", "epsc"], ["dtt"], bias=k.eps_ap(1.0))
        k.tt(dAt[:, :, :], dtt[:, :, :], gd["a_bc"].unsqueeze(1).broadcast_to([128, 2, 12]), ALU.mult, ["dtt", "gains"], ["dAt"])
        if own:
            for j in range(2):
                for (c0, cwid, bank) in ((0, 512, 3), (512, 256, 4)):
                    k.mm(ps[bank][:, 0:cwid], [(hT[:, c, j * 128:(j + 1) * 128], win[:, c, c0:c0 + cwid]) for c in range(NCH)],
                         ["win1"] + h_rd, ["ps%d" % bank])
                    k.act(zs[:, j, c0:c0 + cwid], ps[bank][:, 0:cwid], AF.Silu, ["ps%d" % bank], [("zs", j)])
            for hm in range(4):
                k.mm(ps[5][0:64, 0:TC], [(win[:, c, 2060 + hm * 64:2060 + (hm + 1) * 64], hT[:, c, :]) for c in range(NCH)],
                     ["win1"] + h_rd, ["ps5"])
                headnorm(k, ps[5][0:64, 0:TC], 64, TC, gd["gmq8"][0:64, :], k.ones_b[0:64, 0:64], mqT[0:64, hm, tsl],
                         ["ps5"], [("mqT", hm, ti // 2)], tmp)
        for w in range(2):
            wsl = slice(w * 128, (w + 1) * 128)
            gw = ti * 2 + w
            pbf = ps[0].bitcast(BF16)
            for cc in range(8):
                k.tr(pbf[:, cc * 128:(cc + 1) * 128], xbcT[:, cc, wsl], k.ident_b, [("xbcT", cc), "cbf"], ["ps0"])
            k.copy(xB[:, :], pbf[:, 0:1024], ["ps0"], ["xB"], eng="act")
            k.mm(ps[1][:, 0:12], [(k.utri, dAt[:, w, :])], ["dAt", "cf32"], ["ps1"])
            k.mm(ps[1][:, 12:24], [(k.ones_f, dAt[:, w, :])], ["dAt", "cf32"], ["ps1"])
            k.copy(cs[:, :], ps[1][:, 0:24], ["ps1"], ["cs"], eng="dve")
            k.act(ed[:, :], ps[1][:, 0:24], AF.Exp, ["ps1"], ["ed"])
            k.tt(wend[:, :], cs[:, 12:24], cs[:, 0:12], ALU.subtract, ["cs"], ["wend"])
            k.act(wend[:, :], wend[:, :], AF.Exp, ["wend"], ["wend"])
            k.tt(wend[:, :], wend[:, :], dtt[:, w, :], ALU.mult, ["wend", "dtt"], ["wend"])
            x3 = xB[:, 0:TOK_W].rearrange("p (h d) -> p h d", d=64)
            k.tt(xend[:, :].rearrange("p (h d) -> p h d", d=64), x3, wend[:, :].unsqueeze(2).broadcast_to([128, 12, 64]),
                 ALU.mult, ["xB", "wend"], ["xend"])
            if own:
                k.tt(xdt[:, :].rearrange("p (h d) -> p h d", d=64), x3, dtt[:, w, :].unsqueeze(2).broadcast_to([128, 12, 64]),
                     ALU.mult, ["xB", "dtt"], ["xdt"], eng="dve")
                k.tt(xD[:, :].rearrange("p (h d) -> p h d", d=64), x3, gd["dsk"].unsqueeze(2).broadcast_to([128, 12, 64]),
                     ALU.mult, ["xB", "parraw"], ["xD"], eng="dve")
                k.copy(dabc[:, :, :], dAt[:, w, :].unsqueeze(2).broadcast_to([128, 12, 128]), ["dAt"], ["dabc"] + [("hT1", c) for c in range(NCH)], eng="pool")
                for h in range(12):
                    bank = 2 + h // 4
                    o = ps[bank][:, (h % 4) * 128:(h % 4 + 1) * 128]
                    kb = "ps%d" % bank
                    k.mm1(o, dabc[:, h, :], k.utri, True, False, ["dabc", "cf32"], [kb])
                    k.mm1(o, k.negutri, dabc[:, h, :], False, False, ["dabc", "cf32"], [kb])
                    k.mm1(o, k.ident, k.maskneg, False, True, ["cf32"], [kb])
                for q in range(3):
                    k.act(expE[:, q * 4:(q + 1) * 4, :].rearrange("p a b -> p (a b)"), ps[2 + q][:, :], AF.Exp,
                          ["ps%d" % (2 + q), "sq1", "ytmp"], [("expE", q)])
                for g in range(2):
                    k.mm1(ps[1][:, 256 + g * 128:256 + (g + 1) * 128], xbcT[:, 6 + g, wsl], xbcT[:, 8 + g, wsl], True, True,
                          [("xbcT", 6 + g), ("xbcT", 8 + g)], ["ps1"])
                k.copy(cb[:, :, :].rearrange("p a b -> p (a b)"), ps[1][:, 256:512], ["ps1"], ["cb"], eng="dve")
                for g in range(2):
                    k.tt(Wt[:, 6 * g:6 * g + 6, :], expE[:, 6 * g:6 * g + 6, :], cb[:, g, :].unsqueeze(1).broadcast_to([128, 6, 128]),
                         ALU.mult, [("expE", q) for q in range(3)] + ["cb"], [("Wt", g)])
                k.mm1(ps[2][:, 0:512], k.ident_b, xD[:, 0:512], True, False, ["xD", "cbf"], ["ps2"])
                for h in range(8):
                    k.mm1(ps[2][:, h * 64:(h + 1) * 64], Wt[:, h, :], xdt[:, h * 64:(h + 1) * 64], False, h == 7,
                          [("Wt", h // 6), "xdt"], ["ps2"])
                k.mm1(ps[3][:, 0:256], k.ident_b, xD[:, 512:768], True, False, ["xD", "cbf"], ["ps3"])
                for h in range(8, 12):
                    k.mm1(ps[3][:, (h - 8) * 64:(h - 7) * 64], Wt[:, h, :], xdt[:, h * 64:(h + 1) * 64], False, h == 11,
                          [("Wt", h // 6), "xdt"], ["ps3"])
                for g in range(2):
                    k.mm1(ps[5 + g][:, 0:384], xbcT[:, 8 + g, wsl], Hbf[:, g * 384:(g + 1) * 384], True, True,
                          [("xbcT", 8 + g), "Hbf"], ["ps%d" % (5 + g)])
                for g in range(2):
                    k.tt(ytmp[:, g * 384:(g + 1) * 384].rearrange("p (h d) -> p h d", d=64),
                         ps[5 + g][:, 0:384].rearrange("p (h d) -> p h d", d=64),
                         ed[:, 6 * g:6 * g + 6].unsqueeze(2).broadcast_to([128, 6, 64]), ALU.mult, ["ps%d" % (5 + g), "ed"], ["ytmp", "sq1"] + [("expE", q) for q in range(3)])
                k.tt(ytmp[:, 0:512], ps[2][:, 0:512], ytmp[:, 0:512], ALU.add, ["ps2", "ytmp"], ["ytmp"])
                k.tt(ytmp[:, 512:768], ps[3][:, 0:256], ytmp[:, 512:768], ALU.add, ["ps3", "ytmp"], ["ytmp"])
                k.tt(ytmp[:, :], ytmp[:, :], zs[:, w, :], ALU.mult, ["ytmp", ("zs", w)], ["ytmp"])
                for g in range(2):
                    k.act(junk[:, :], ytmp[:, g * 384:(g + 1) * 384], AF.Square, ["ytmp"], ["hrs", ("ss", g)],
                          accum_out=ss[:, g:g + 1])
                k.act(ss[:, 2:4], ss[:, 0:2], AF.Ln, [("ss", 0), ("ss", 1), "epsc"], ["ss2"], bias=k.eps_ap(EPS), scale=1.0 / 384.0)
                k.act(ss[:, 2:4], ss[:, 2:4], AF.Exp, ["ss2"], ["ss2"], scale=-0.5)
                for g in range(2):
                    k.stt(yn[:, g * 384:(g + 1) * 384], ytmp[:, g * 384:(g + 1) * 384], ss[:, 2 + g:3 + g],
                          gd["ng"][:, g * 384:(g + 1) * 384], ALU.mult, ALU.mult, ["ytmp", "ss2", "parraw"], [("yn", g)])
                pbf4 = ps[4].bitcast(BF16)
                for kc in range(6):
                    k.tr(pbf4[:, kc * 128:(kc + 1) * 128], yn[:, kc * 128:(kc + 1) * 128], k.ident_b,
                         [("yn", kc // 3), "cbf"], ["ps4"])
                k.copy(tokT[:, :, gw * 128:(gw + 1) * 128], pbf4[:, 0:768].rearrange("p (a b) -> p a b", b=128), ["ps4"],
                       [("tokT1", kc, gw // 4) for kc in range(6)], eng="act")
            for g in range(2):
                k.mm1(ps[7 - g][:, 0:384], xB[:, 768 + g * 128:768 + (g + 1) * 128], xend[:, g * 384:(g + 1) * 384], True, True,
                      ["xB", "xend"], ["ps%d" % (7 - g)])
            for g in range(2):
                Hg = H[:, g * 384:(g + 1) * 384].rearrange("p (h d) -> p h d", d=64)
                k.tt(Hg, Hg, ed[:, 12 + 6 * g:12 + 6 * g + 6].unsqueeze(2).broadcast_to([128, 6, 64]), ALU.mult,
                     ["H", "ed", "Hbf"], ["H"])
                k.tt(H[:, g * 384:(g + 1) * 384], ps[7 - g][:, 0:384], H[:, g * 384:(g + 1) * 384], ALU.add,
                     ["ps%d" % (7 - g), "H"], ["H"])
            k.copy(Hbf[:, :], H[:, :], ["H"], ["Hbf"], eng="act")
    m.release(mk)


def build_ssd():
    nc = bass.Bass("TRN2", target_bir_lowering=False)
    xo = nc.dram_tensor("xo", [SEQH, D], F32, kind="ExternalInput").ap()
    xp = nc.dram_tensor("xp", [SEQH, D], F32, kind="ExternalInput").ap()
    mem = nc.dram_tensor("mem", [MEM_LEN, D], F32, kind="ExternalInput").ap()
    cst = nc.dram_tensor("cst", [128, CONST_W], F32, kind="ExternalInput").ap()
    par = nc.dram_tensor("par", [128, PC_W], F32, kind="ExternalInput").ap()
    win_d = nc.dram_tensor("win", [D, SSM_IN], F32, kind="ExternalInput").ap()
    wkv_d = nc.dram_tensor("wkv", [D, 512], F32, kind="ExternalInput").ap()
    wo_d = nc.dram_tensor("wo", [D, D], F32, kind="ExternalInput").ap()
    y = nc.dram_tensor("y", [SEQH, D], F32, kind="ExternalOutput").ap()
    k = K(nc)
    with k.st:
        m = k.m
        k.setup_consts(cst)
        gd = ssd_gains(k, par)
        xT = m.alloc("xT", [128, NCH, SEQH], F32)
        KmT = m.alloc("KmT", [64, 4, MEM_LEN], BF16)
        Vm = m.alloc("Vm", [128, 2, 256], BF16)
        mem_kv_setup(k, mem, wkv_d, gd["gmem32"], gd["gmk8"], KmT, Vm, "1")
        H = m.alloc("H", [128, TOK_W], F32)
        Hbf = m.alloc("Hbf", [128, TOK_W], BF16)
        halo = m.alloc("halo", [128, 10, 3], F32)
        k.memset(H[:, :], 0.0, ["H"], eng="dve")
        k.memset(Hbf[:, :], 0.0, ["Hbf"], eng="pool")
        k.memset(halo[:, :, :], 0.0, [("halo", cc) for cc in range(10)], eng="pool")
        mqT = m.alloc("mqT", [64, 4, SEQH], BF16)
        tokT = m.alloc("tokT1", [128, 6, SEQH], BF16)
        mk1 = m.mark()
        win = m.alloc("win1", [128, NCH, SSM_IN], BF16)
        winv = win_d.rearrange("(c p) f -> p c f", p=128)
        for (a, b) in ((0, 512), (512, 1024), (1024, 1536), (1536, 2048), (2048, SSM_IN)):
            k.dma("pool", win[:, :, a:b], winv[:, :, a:b], (), ["win1"], "win1")
        k.load_xT(xp, xT, "xT")
        ssd_pass(k, xT, "xT", gd, win, False, H, Hbf, halo, None, None)
        k.ts(H[:, :], H[:, :], gd["flag"], None, ALU.mult, None, ["H", "gains"], ["H"])
        k.copy(Hbf[:, :], H[:, :], ["H"], ["Hbf"], eng="pool")
        for cc in range(10):
            k.ts(halo[:, cc, :], halo[:, cc, :], gd["flag"], None, ALU.mult, None, [("halo", cc), "gains"], [("halo", cc)])
        k.s.barrier()
        k.load_xT(xo, xT, "xT")
        ssd_pass(k, xT, "xT", gd, win, True, H, Hbf, halo, tokT, mqT)
        m.release(mk1)
        memT = m.alloc("memTo", [64, 4, SEQH], BF16)
        mem_attn(k, mqT, memT, KmT, Vm, "1")
        attn_outproj(k, xT, "xT", tokT, "tokT1", 6, memT, wo_d, "1")
        k.store_xT(xT, y, "xT")
        k.s.emit()
    return nc


def run_ssd(inp, xo_shards, xp_shards, flags, mems, trace=False):
    if "ssd" not in _cache:
        _cache["ssd"] = build_ssd()
    nc = _cache["ssd"]
    cst = make_consts()
    in_maps = []
    for i in range(8):
        in_maps.append({"xo": np.ascontiguousarray(xo_shards[i]), "xp": np.ascontiguousarray(xp_shards[i]),
                        "mem": np.ascontiguousarray(mems[i]), "cst": cst, "par": pack_par_ssd(inp, flags[i]),
                        "win": np.asarray(inp["ssm_w_in"][0]), "wkv": np.asarray(inp["mem_w_kv"][1]),
                        "wo": np.asarray(inp["w_out"][1])})
    res = run_bass_kernel_spmd(nc, in_maps, core_ids=list(range(8)), trace=trace)
    return [r["y"] for r in res.results], res


def build_fused():
    nc = bass.Bass("TRN2", target_bir_lowering=False)
    dt = lambda name, shape: nc.dram_tensor(name, shape, F32, kind="ExternalInput").ap()
    xo = dt("xo", [SEQH, D])
    xp = dt("xp", [SEQH, D])
    mem = dt("mem", [MEM_LEN, D])
    cst = dt("cst", [128, CONST_W])
    parA = dt("parA", [128, PA_W])
    parC = dt("parC", [128, PC_W])
    parF = dt("parF", [128, 16])
    win0_d = dt("win0", [D, 2560])
    win1_d = dt("win1", [D, SSM_IN])
    wkv0_d = dt("wkv0", [D, 512])
    wkv1_d = dt("wkv1", [D, 512])
    wo0_d = dt("wo0", [D, D])
    wo1_d = dt("wo1", [D, D])
    fg = dt("fg", [D, FFN_DIM])
    fu = dt("fu", [D, FFN_DIM])
    fd = dt("fd", [FFN_DIM, D])
    wr = dt("wr", [D, N_EXPERTS])
    eg = dt("eg", [N_EXPERTS, D, EXPERT_DIM])
    eu = dt("eu", [N_EXPERTS, D, EXPERT_DIM])
    ed = dt("ed", [N_EXPERTS, EXPERT_DIM, D])
    y = nc.dram_tensor("y", [SEQH, D], F32, kind="ExternalOutput").ap()
    kscr = nc.dram_tensor("kscr", [6, 128, 2 * SEQH], BF16).ap()
    vscr = nc.dram_tensor("vscr", [6, 128, 32, 128], BF16).ap()
    hc_d = nc.dram_tensor("hcd", [N_EXPERTS * CAP, D], BF16).ap()
    yc_d = nc.dram_tensor("ycd", [N_EXPERTS * CAP, D], F32).ap()
    cnt_d = nc.dram_tensor("cntd", [1, 8], I32).ap()
    k = K(nc)
    with k.st:
        m = k.m
        k.setup_consts(cst)
        xT_off = m.off
        xT = m.alloc("xT", [128, NCH, SEQH], F32)
        gF = m.alloc("gF", [128, 16], F32)
        gF32 = m.alloc("gF32", [128, 16], F32)
        zt = m.alloc("zt", [128, D], BF16)
        k.dma("sp", gF[:], parF[:, :], (), ["gFraw"], "parF")
        k.ts(gF32[:], gF[:], 32.0, None, ALU.mult, None, ["gFraw"], ["par"])
        mk_layers = m.mark()
        gA = attn_gains(k, parA)
        gC = ssd_gains(k, parC)
        KmT0 = m.alloc("KmT0", [64, 4, MEM_LEN], BF16)
        Vm0 = m.alloc("Vm0", [128, 2, 256], BF16)
        KmT1 = m.alloc("KmT1", [64, 4, MEM_LEN], BF16)
        Vm1 = m.alloc("Vm1", [128, 2, 256], BF16)
        mem_kv_setup(k, mem, wkv0_d, gA["gmem32"], gA["gmk8"], KmT0, Vm0, "0")
        mem_kv_setup(k, mem, wkv1_d, gC["gmem32"], gC["gmk8"], KmT1, Vm1, "1")
        H = m.alloc("H", [128, TOK_W], F32)
        Hbf = m.alloc("Hbf", [128, TOK_W], BF16)
        halo = m.alloc("halo", [128, 10, 3], F32)
        k.memset(H[:, :], 0.0, ["H"], eng="dve")
        k.memset(Hbf[:, :], 0.0, ["Hbf"], eng="pool")
        k.memset(halo[:, :, :], 0.0, [("halo", cc) for cc in range(10)], eng="pool")

        def layer0(xd, nprev):
            mk = m.mark()
            QT = m.alloc("QT", [128, 6, SEQH], BF16)
            mqT = m.alloc("mqT", [64, 4, SEQH], BF16)
            mk1 = m.mark()
            win = m.alloc("win", [128, NCH, 2560], BF16)
            winv = win0_d.rearrange("(c p) f -> p c f", p=128)
            for j in range(5):
                k.dma("pool", win[:, :, j * 512:(j + 1) * 512], winv[:, :, j * 512:(j + 1) * 512], (), ["win"], "win")
            tiles = [(xd[i * TT:(i + 1) * TT, :], i) for i in range(NTT)]
            attn_inproj(k, tiles, xT, gA["g32"], win, gA["gq8"], gA["gk8"], gA["gmq8"], QT, mqT, kscr, vscr, nprev)
            m.release(mk1)
            mem_attn(k, mqT, mqT, KmT0, Vm0, "0")
            attn_core(k, QT, QT, kscr, vscr, gA["neglam"], gA["subgs"], gA["flagb"], nprev)
            attn_outproj(k, xT, "xT", QT, "tokT", 6, mqT, wo0_d, "0")
            m.release(mk)
            mk = m.mark()
            h2T = m.alloc("h2T", [128, NCH, SEQH], BF16)
            mk2 = m.mark()
            sq = m.alloc("sq", [128, NCH, TT], BF16)
            rstd = m.alloc("rstd", [128, TT], F32)
            for tt in range(NTT):
                k.rmsnorm_T(xT, "xT", gF32[:, 0:8], h2T, "h2T", slice(tt * TT, (tt + 1) * TT), tt, sq, "sq", rstd, "rstd")
            m.release(mk2)
            k.ffn_bufs()
            k.ffn(xT, "xT", h2T, "h2T", fg, fu, fd, FFN_DIM)
            m.release(mk)

        layer0(xp, 0)
        hc_zero_fill(k, zt, hc_d)
        mk = m.mark()
        win1 = m.alloc("win1", [128, NCH, SSM_IN], BF16)
        win1v = win1_d.rearrange("(c p) f -> p c f", p=128)
        for (a, b) in ((768, 1280), (1280, 1792), (1792, 2060)):
            k.dma("pool", win1[:, :, a:b], win1v[:, :, a:b], (), ["win1"], "win1")
        ssd_pass(k, xT, "xT", gC, win1, False, H, Hbf, halo, None, None)
        m.release(mk)
        k.ts(H[:, :], H[:, :], gC["flag"], None, ALU.mult, None, ["H", "gains"], ["H"])
        k.copy(Hbf[:, :], H[:, :], ["H"], ["Hbf"], eng="pool")
        for cc in range(10):
            k.ts(halo[:, cc, :], halo[:, cc, :], gC["flag"], None, ALU.mult, None, [("halo", cc), "gains"], [("halo", cc)])
        k.s.barrier()
        layer0(xo, NTT)
        mk = m.mark()
        mqT = m.alloc("mqT", [64, 4, SEQH], BF16)
        tokT = m.alloc("tokT1", [128, 6, SEQH], BF16)
        mk1 = m.mark()
        win1 = m.alloc("win1", [128, NCH, SSM_IN], BF16)
        for (a, b) in ((0, 512), (512, 1024), (1024, 1536), (1536, 2048), (2048, SSM_IN)):
            k.dma("pool", win1[:, :, a:b], win1v[:, :, a:b], (), ["win1"], "win1")
        ssd_pass(k, xT, "xT", gC, win1, True, H, Hbf, halo, tokT, mqT)
        m.release(mk1)
        mem_attn(k, mqT, mqT, KmT1, Vm1, "1")
        attn_outproj(k, xT, "xT", tokT, "tokT1", 6, mqT, wo1_d, "1")
        m.release(mk)
        k.s.freeze_weights = True
        m.release(mk_layers)
        moe_sparse(k, xT, "xT", xT_off, gF32[:, 8:16], wr, eg, eu, ed, y, hc_d, yc_d, cnt_d)
        k.s.emit()
    return nc


def kernel(**inputs):
    inp = {k: np.asarray(v) for k, v in inputs.items()}
    x = inp["x"].astype(np.float32, copy=False)
    ncore = 8
    if "fused" not in _cache:
        _cache["fused"] = build_fused()
    nc = _cache["fused"]
    cst = make_consts()
    parF = np.concatenate([pack_pp(inp["ln2_g"][0]), pack_pp(inp["ln2_g"][1])], axis=1)
    in_maps = []
    for i in range(ncore):
        b, hf = i // 2, i % 2
        in_maps.append({
            "xo": np.ascontiguousarray(x[b, hf * SEQH:(hf + 1) * SEQH]),
            "xp": np.ascontiguousarray(x[b, 0:SEQH]),
            "mem": np.ascontiguousarray(inp["mem"][b]),
            "cst": cst, "parA": pack_par_attn(inp, 0, float(hf)), "parC": pack_par_ssd(inp, float(hf)), "parF": parF,
            "win0": inp["da_w_in"][0], "win1": inp["ssm_w_in"][0], "wkv0": inp["mem_w_kv"][0], "wkv1": inp["mem_w_kv"][1],
            "wo0": inp["w_out"][0], "wo1": inp["w_out"][1],
            "fg": inp["ffn_w_gate"][0], "fu": inp["ffn_w_up"][0], "fd": inp["ffn_w_down"][0],
            "wr": inp["moe_w_router"][0], "eg": inp["moe_w_gate"][0], "eu": inp["moe_w_up"][0], "ed": inp["moe_w_down"][0],
        })
    res = run_bass_kernel_spmd(nc, in_maps, core_ids=list(range(ncore)))
    out = np.empty((4, 2 * SEQH, D), np.float32)
    for i in range(ncore):
        out[i // 2, (i % 2) * SEQH:(i % 2 + 1) * SEQH] = res.results[i]["y"]
    return out


def kernel_unfused(**inputs):
    inp = {k: np.asarray(v) for k, v in inputs.items()}
    x = inp["x"].astype(np.float32, copy=False)
    ncore = 8
    bs = [i // 2 for i in range(ncore)]
    hf = [i % 2 for i in range(ncore)]
    flags = [float(h) for h in hf]
    mems = [inp["mem"][b] for b in bs]
    xo = [x[bs[i], hf[i] * SEQH:(hf[i] + 1) * SEQH] for i in range(ncore)]
    xp = [x[bs[i], 0:SEQH] for i in range(ncore)]
    x1, _ = run_attn0(inp, xo, xp, flags, mems)
    x2 = run_ffn0(x1, inp["ln2_g"][0], inp["ffn_w_gate"][0], inp["ffn_w_up"][0], inp["ffn_w_down"][0])
    xp2 = [x2[2 * bs[i]] for i in range(ncore)]
    x3, _ = run_ssd(inp, x2, xp2, flags, mems)
    x4, _ = run_moe(x3, inp["ln2_g"][1], inp["moe_w_router"][0], inp["moe_w_gate"][0], inp["moe_w_up"][0],
                    inp["moe_w_down"][0])
    out = np.empty((4, 2 * SEQH, D), np.float32)
    for i in range(ncore):
        out[bs[i], hf[i] * SEQH:(hf[i] + 1) * SEQH] = x4[i]
    return out
```
